# Optimizing a Trainium2 kernel written in Bass

```python
import jax, jax.numpy as jnp
from jax import lax
import numpy as np

D_MODEL = 2048
BATCH = 8
SEQ = 2048
DEPTH = 1

PLE_DIM = 256
BLK = 128
EPS = 1e-6

A_HEAD_DIM = 128
A_WIDTH = D_MODEL // 2
A_HEADS = A_WIDTH // A_HEAD_DIM
A_BRANCHES = ((128, 1), (512, 4), (2048, 16))

B_HEAD_DIM = 64
B_WIDTH = D_MODEL - A_WIDTH
B_HEADS = B_WIDTH // B_HEAD_DIM
B_GROUP = 8
B_KV_HEADS = B_HEADS // B_GROUP
B_WINDOW = 128

MIX_WIDTH = A_WIDTH + B_WIDTH
IN_COLS = 3 * A_WIDTH + B_WIDTH + 2 * B_KV_HEADS * B_HEAD_DIM

N_GROUPS = 4
EXPERTS_PER_GROUP = 8
N_EXPERTS = N_GROUPS * EXPERTS_PER_GROUP
TOP_K = 2
D_EXPERT = D_MODEL // 8

kernel_name = "hymba_dilated_sinkswa_hiermoe_ple"


def rms_norm(x, g):
    xf = x.astype(jnp.float32)
    y = xf * lax.rsqrt(jnp.mean(xf * xf, axis=-1, keepdims=True) + EPS)
    return (y * g.astype(jnp.float32)).astype(x.dtype)


def alibi_slopes(n):
    return jnp.asarray(np.array([2.0 ** (-8.0 * (i + 1) / n) for i in range(n)], dtype=np.float32))


def _with_prev_block(xb):
    prev = jnp.pad(xb[:, :, :-1], ((0, 0), (0, 0), (1, 0), (0, 0), (0, 0)))
    return jnp.concatenate([prev, xb], axis=3)


def banded_attention(q, k, v, slopes, max_delta, stride, sinks=None):
    n, hkv, g, L, hd = q.shape
    nb = L // BLK
    qb = q.reshape(n, hkv, g, nb, BLK, hd)
    kb = _with_prev_block(k.reshape(n, hkv, nb, BLK, hd))
    vb = _with_prev_block(v.reshape(n, hkv, nb, BLK, hd))
    s = jnp.einsum('nkgbqd,nkbsd->nkgbqs', qb, kb,
                   preferred_element_type=jnp.float32) * (hd ** -0.5)
    qi = jnp.arange(L).reshape(nb, BLK)
    ki = qi[:, :1] - BLK + jnp.arange(2 * BLK)[None, :]
    delta = qi[:, :, None] - ki[:, None, :]
    valid = (delta >= 0) & (delta <= max_delta) & (ki[:, None, :] >= 0)
    s = s - slopes.astype(jnp.float32)[:, :, None, None, None] * (stride * delta).astype(jnp.float32)
    s = jnp.where(valid, s, -jnp.inf)
    m = jnp.max(s, axis=-1)
    if sinks is not None:
        sk = sinks.astype(jnp.float32)[:, :, None, None]
        m = jnp.maximum(m, sk)
    pr = jnp.exp(s - m[..., None])
    den = jnp.sum(pr, axis=-1)
    if sinks is not None:
        den = den + jnp.exp(sk - m)
    o = jnp.einsum('nkgbqs,nkbsd->nkgbqd', pr.astype(v.dtype), vb,
                   preferred_element_type=jnp.float32) / den[..., None]
    lse = m + jnp.log(den)
    return o.reshape(n, hkv, g, L, hd).astype(q.dtype), lse.reshape(n, hkv, g, L)


def dilated_branch(q, k, v, slopes, window, dilation):
    b, h, s, hd = q.shape
    span = dilation * BLK
    sp = -(-s // span) * span
    L = sp // dilation

    def to_residues(t):
        t = jnp.pad(t, ((0, 0), (0, 0), (0, sp - s), (0, 0)))
        t = t.reshape(b, h, L, dilation, hd).transpose(0, 3, 1, 2, 4)
        return t.reshape(b * dilation, h, L, hd)

    qr, kr, vr = to_residues(q), to_residues(k), to_residues(v)
    o, lse = banded_attention(qr[:, :, None], kr, vr, slopes[:, None], window // dilation, dilation)
    o = o[:, :, 0].reshape(b, dilation, h, L, hd).transpose(0, 2, 3, 1, 4).reshape(b, h, sp, hd)[:, :, :s]
    lse = lse[:, :, 0].reshape(b, dilation, h, L).transpose(0, 2, 3, 1).reshape(b, h, sp)[:, :, :s]
    return o, lse


def dilated_attention(q, k, v, slopes):
    outs, lses = [], []
    for window, dilation in A_BRANCHES:
        o, lse = dilated_branch(q, k, v, slopes, window, dilation)
        outs.append(o)
        lses.append(lse)
    w = jax.nn.softmax(jnp.stack(lses), axis=0)
    return jnp.einsum('rbhs,rbhsd->bhsd', w.astype(q.dtype), jnp.stack(outs))


def sink_window_attention(q, k, v, slopes, sinks):
    b, hq, s, hd = q.shape
    qg = q.reshape(b, B_KV_HEADS, B_GROUP, s, hd)
    o, _ = banded_attention(qg, k, v, slopes.reshape(B_KV_HEADS, B_GROUP), B_WINDOW - 1, 1,
                            sinks.reshape(B_KV_HEADS, B_GROUP))
    return o.reshape(b, hq, s, hd)


def hier_moe(x, w_grp, b_grp, w_exp, b_exp, w_gate, w_up, w_down):
    t = x.shape[0]
    grp_logits = jnp.dot(x, w_grp, preferred_element_type=jnp.float32) + b_grp.astype(jnp.float32)
    grp_prob = jax.nn.softmax(grp_logits, axis=-1)
    grp_idx = jnp.argmax(grp_logits, axis=-1)
    grp_w = jnp.take_along_axis(grp_prob, grp_idx[:, None], axis=-1)
    exp_logits = jnp.einsum('td,dge->tge', x, w_exp,
                            preferred_element_type=jnp.float32) + b_exp.astype(jnp.float32)
    in_grp = jnp.take_along_axis(exp_logits, grp_idx[:, None, None], axis=1)[:, 0]
    top_val, top_idx = lax.top_k(in_grp, TOP_K)
    top_w = jax.nn.softmax(top_val, axis=-1) * grp_w
    eid = grp_idx[:, None] * EXPERTS_PER_GROUP + top_idx
    combine = jnp.sum(jax.nn.one_hot(eid, N_EXPERTS, dtype=jnp.float32) * top_w[..., None], axis=1)
    hg = jnp.einsum('td,edf->tef', x, w_gate)
    hu = jnp.einsum('td,edf->tef', x, w_up)
    hid = jax.nn.silu(hg) * hu * combine.astype(x.dtype)[..., None]
    return jnp.einsum('tef,efd->td', hid, w_down)


def setup_inputs(seed: int = 0) -> dict:
    key = jax.random.key(seed)
    ks = jax.random.split(key, 20)

    def nrm(k, shape, scale):
        return jax.random.normal(k, shape, jnp.float32) * scale

    return {
        "x": nrm(ks[0], (BATCH, SEQ, D_MODEL), 1.0),
        "p": nrm(ks[1], (DEPTH, BATCH, SEQ, PLE_DIM), 1.0),
        "w_in": nrm(ks[2], (DEPTH, D_MODEL, IN_COLS), D_MODEL ** -0.5),
        "w_out": nrm(ks[3], (DEPTH, MIX_WIDTH, D_MODEL), MIX_WIDTH ** -0.5),
        "sinks": nrm(ks[4], (DEPTH, B_HEADS), 1.0),
        "g_mix": 1.0 + nrm(ks[5], (DEPTH, D_MODEL), 0.02),
        "g_moe": 1.0 + nrm(ks[6], (DEPTH, D_MODEL), 0.02),
        "g_ple": 1.0 + nrm(ks[7], (DEPTH, D_MODEL), 0.02),
        "g_final": 1.0 + nrm(ks[8], (D_MODEL,), 0.02),
        "w_grp": nrm(ks[9], (DEPTH, D_MODEL, N_GROUPS), D_MODEL ** -0.5),
        "b_grp": nrm(ks[10], (DEPTH, N_GROUPS), 0.01),
        "w_exp": nrm(ks[11], (DEPTH, D_MODEL, N_GROUPS, EXPERTS_PER_GROUP), D_MODEL ** -0.5),
        "b_exp": nrm(ks[12], (DEPTH, N_GROUPS, EXPERTS_PER_GROUP), 0.01),
        "w_gate": nrm(ks[13], (DEPTH, N_EXPERTS, D_MODEL, D_EXPERT), D_MODEL ** -0.5),
        "w_up": nrm(ks[14], (DEPTH, N_EXPERTS, D_MODEL, D_EXPERT), D_MODEL ** -0.5),
        "w_down": nrm(ks[15], (DEPTH, N_EXPERTS, D_EXPERT, D_MODEL), D_EXPERT ** -0.5),
        "w_ple": nrm(ks[16], (DEPTH, PLE_DIM, D_MODEL), PLE_DIM ** -0.5),
        "w_ple_gate": nrm(ks[17], (DEPTH, D_MODEL, D_MODEL), D_MODEL ** -0.5),
    }


def reference(x, p, w_in, w_out, sinks, g_mix, g_moe, g_ple, g_final, w_grp, b_grp,
              w_exp, b_exp, w_gate, w_up, w_down, w_ple, w_ple_gate):
    b, s, d = x.shape
    slopes_a = alibi_slopes(A_HEADS)
    slopes_b = alibi_slopes(B_HEADS)
    kv_w = B_KV_HEADS * B_HEAD_DIM
    splits = [A_WIDTH, 2 * A_WIDTH, 3 * A_WIDTH, 3 * A_WIDTH + B_WIDTH, 3 * A_WIDTH + B_WIDTH + kv_w]

    def heads(t, n, hd):
        return t.reshape(b, s, n, hd).transpose(0, 2, 1, 3)

    h = x
    for i in range(DEPTH):
        a = rms_norm(h, g_mix[i])
        proj = a @ w_in[i]
        qa, ka, va, qb, kb, vb = jnp.split(proj, splits, axis=-1)
        oa = dilated_attention(heads(qa, A_HEADS, A_HEAD_DIM), heads(ka, A_HEADS, A_HEAD_DIM),
                               heads(va, A_HEADS, A_HEAD_DIM), slopes_a)
        ob = sink_window_attention(heads(qb, B_HEADS, B_HEAD_DIM), heads(kb, B_KV_HEADS, B_HEAD_DIM),
                                   heads(vb, B_KV_HEADS, B_HEAD_DIM), slopes_b, sinks[i])
        mixed = jnp.concatenate([oa.transpose(0, 2, 1, 3).reshape(b, s, A_WIDTH),
                                 ob.transpose(0, 2, 1, 3).reshape(b, s, B_WIDTH)], axis=-1)
        h = h + mixed @ w_out[i]
        m = rms_norm(h, g_moe[i]).reshape(b * s, d)
        h = h + hier_moe(m, w_grp[i], b_grp[i], w_exp[i], b_exp[i],
                         w_gate[i], w_up[i], w_down[i]).reshape(b, s, d)
        n = rms_norm(h, g_ple[i])
        h = h + jax.nn.sigmoid(n @ w_ple_gate[i]) * (p[i] @ w_ple[i])
    return rms_norm(h, g_final)
```

```python
import numpy as np
from contextlib import ExitStack
import concourse.bass as bass
import concourse.mybir as mybir
from concourse.bass_utils import run_bass_kernel_spmd

F32 = mybir.dt.float32
BF16 = mybir.dt.bfloat16
I32 = mybir.dt.int32
AF = mybir.ActivationFunctionType
ALU = mybir.AluOpType
AX = mybir.AxisListType

S = 2048
D = 2048
NT = 16
NE = 32
CAP = 256
NEGV = -30000.0
BIGV = 1.0e4
EPS = 1e-6


class Eng:
    def __init__(self, nc, es, eng, name, chain=False):
        self._e = eng
        self.sem = es.enter_context(nc.semaphore("sem_" + name))
        self.cnt = 0
        self.seen = {}
        self.chain = chain

    @property
    def e(self):
        if self.chain and self.cnt > self.seen.get(id(self.sem), 0):
            self._e.wait_ge(self.sem, self.cnt)
            self.seen[id(self.sem)] = self.cnt
        return self._e

    def wait(self, *toks):
        for t in toks:
            if t is None:
                continue
            if isinstance(t, list):
                self.wait(*t)
                continue
            sem, val = t
            k = id(sem)
            if self.seen.get(k, 0) >= val:
                continue
            self._e.wait_ge(sem, val)
            self.seen[k] = val

    def sig(self, inst):
        self.cnt += 1
        inst.then_inc(self.sem, 1)
        return (self.sem, self.cnt)


class DSem:
    def __init__(self, nc, es, name, reg):
        self.sem = es.enter_context(nc.semaphore("dma_" + name))
        self.cnt = 0
        reg.append(self)

    def add(self, inst):
        self.cnt += 16
        inst.then_inc(self.sem, 16)
        return (self.sem, self.cnt)

    def tok(self):
        return (self.sem, self.cnt) if self.cnt else None


def build(stage=99):
    nc = bass.Bass("TRN2", target_bir_lowering=False)

    def din(n, s, d=F32):
        return nc.dram_tensor(n, s, d, kind="ExternalInput").ap()

    x = din("x", [S, D])
    p_in = din("p", [S, 256])
    w_in = din("w_in", [D, 4352])
    w_out = din("w_out", [D, D])
    sinks = din("sinks", [1, 16])
    g_mix = din("g_mix", [1, D])
    g_moe = din("g_moe", [1, D])
    g_ple = din("g_ple", [1, D])
    g_final = din("g_final", [1, D])
    w_grp = din("w_grp", [D, 4])
    b_grp = din("b_grp", [1, 4])
    w_exp = din("w_exp", [D, 32])
    b_exp = din("b_exp", [1, 32])
    w_gate = din("w_gate", [NE, D, 256])
    w_up = din("w_up", [NE, D, 256])
    w_down = din("w_down", [NE, 256, D])
    w_ple = din("w_ple", [256, D])
    w_pg = din("w_ple_gate", [D, D])
    out = nc.dram_tensor("out", [S, D], F32, kind="ExternalOutput").ap()
    H1 = nc.dram_tensor("H1", [S, D], F32, kind="Internal").ap()
    Mscr = nc.dram_tensor("Mscr", [S, D], BF16, kind="Internal").ap()
    Xg = nc.dram_tensor("Xg", [NE * CAP, D], BF16, kind="Internal").ap()
    Y = nc.dram_tensor("Y", [NE * CAP, D], BF16, kind="Internal").ap()
    WGU = nc.dram_tensor("WGU", [NE, 128, 16 * 512], BF16, kind="Internal").ap()
    WDN = nc.dram_tensor("WDN", [NE, 128, 2 * D], BF16, kind="Internal").ap()
    WO = nc.dram_tensor("WO", [128, 16, D], BF16, kind="Internal").ap()
    WPG = nc.dram_tensor("WPG", [128, 16, D], BF16, kind="Internal").ap()

    es = ExitStack()
    with es:
        PE = Eng(nc, es, nc.tensor, "pe")
        ACT = Eng(nc, es, nc.scalar, "act", chain=True)
        DVE = Eng(nc, es, nc.vector, "dve", chain=True)
        POOL = Eng(nc, es, nc.gpsimd, "pool", chain=True)
        SP = Eng(nc, es, nc.sync, "sp")
        ENGS = [PE, ACT, DVE, POOL, SP]
        dsems = []
        bar_sem = es.enter_context(nc.semaphore("bar"))
        bar_cnt = [0]

        def mk_dsem(name):
            return DSem(nc, es, name, dsems)

        def barrier():
            for ds in dsems:
                SP.wait(ds.tok())
            bar_cnt[0] += len(ENGS)
            for E in ENGS:
                E.e.drain().then_inc(bar_sem, 1)
            for E in ENGS:
                E.e.wait_ge(bar_sem, bar_cnt[0])

        d_pre_wo = DSem(nc, es, "pre_wo", [])
        d_pre_ex = [DSem(nc, es, "pre_ex%d" % g_, []) for g_ in range(8)]
        d_pre_pg = DSem(nc, es, "pre_pg", [])
        bg_list = []
        for kc in range(16):
            bg_list.append((d_pre_wo, WO[:, kc, :], w_out[128 * kc:128 * kc + 128, :]))
        for kc in range(16):
            bg_list.append((d_pre_pg, WPG[:, kc, :], w_pg[128 * kc:128 * kc + 128, :]))
        for e in range(NE):
            wv = WGU[e].rearrange("p (c f) -> p c f", c=16)
            bg_list.append((d_pre_ex[e // 4], wv[:, :, 0:256], w_gate[e].rearrange("(c p) f -> p c f", p=128)))
            bg_list.append((d_pre_ex[e // 4], wv[:, :, 256:512], w_up[e].rearrange("(c p) f -> p c f", p=128)))
            bg_list.append((d_pre_ex[e // 4], WDN[e].rearrange("p (c d) -> p c d", c=2),
                            w_down[e].rearrange("(c p) d -> p c d", p=128)))
        bg_pos = [0]

        def bg_issue(n):
            for _ in range(n):
                if bg_pos[0] >= len(bg_list):
                    return
                ds, o, i = bg_list[bg_pos[0]]
                bg_pos[0] += 1
                ds.add(POOL.e.dma_start(out=o, in_=i))

        def sb(name, shape, dt, stack=es):
            return stack.enter_context(nc.sbuf_tensor(name, shape, dt))

        def pst(name, shape, dt, stack):
            return stack.enter_context(nc.psum_tensor(name, shape, dt))

        ident_f = sb("ident_f", [128, 128], F32)
        ident_b = sb("ident_b", [128, 128], BF16)
        ones_b = sb("ones_b", [128, 128], BF16)
        Utri = sb("Utri", [128, 128], BF16)
        DT = sb("DT", [128, 256], F32)
        NEG_A = sb("NEG_A", [128, 256], F32)
        NEG_B = sb("NEG_B", [128, 256], F32)
        es_t = sb("es_t", [128, 16], F32)
        sk_t = sb("sk_t", [128, 16], F32)
        ss = sb("ss", [128, 64], F32)
        sd = sb("sd", [128, 64], F32)
        rs = sb("rs", [128, 64], F32)
        d_c = mk_dsem("const")
        lg_all = sb("lg_all", [128, NT, 36], F32)
        dst_i = sb("dst_i", [128, 2, NT], I32)
        wts = sb("wts", [128, 2, NT], F32)

        POOL.sig(POOL.e.memset(ident_f[:], 1.0))
        POOL.sig(POOL.e.affine_select(out=ident_f[:], in_=ident_f[:], pattern=[[-1, 128]], compare_op=ALU.is_equal,
                        fill=0.0, base=0, channel_multiplier=1))
        POOL.sig(POOL.e.memset(ones_b[:], 1.0))
        POOL.sig(POOL.e.memset(Utri[:], 1.0))
        POOL.sig(POOL.e.affine_select(out=Utri[:], in_=Utri[:], pattern=[[1, 128]], compare_op=ALU.is_gt,
                        fill=0.0, base=0, channel_multiplier=-1))
        POOL.sig(POOL.e.iota(DT[:, 0:128], pattern=[[1, 128]], base=128, channel_multiplier=-1,
               allow_small_or_imprecise_dtypes=True))
        POOL.sig(POOL.e.iota(DT[:, 128:256], pattern=[[1, 128]], base=0, channel_multiplier=-1,
               allow_small_or_imprecise_dtypes=True))
        POOL.sig(POOL.e.memset(NEG_A[:], 0.0))
        POOL.sig(POOL.e.memset(NEG_B[:], 0.0))
        POOL.sig(POOL.e.affine_select(out=NEG_A[:, 0:128], in_=NEG_A[:, 0:128], pattern=[[-1, 128]], compare_op=ALU.is_ge,
                        fill=NEGV, base=0, channel_multiplier=1))
        POOL.sig(POOL.e.affine_select(out=NEG_A[:, 128:256], in_=NEG_A[:, 128:256], pattern=[[1, 128]], compare_op=ALU.is_ge,
                        fill=NEGV, base=0, channel_multiplier=-1))
        POOL.sig(POOL.e.affine_select(out=NEG_B[:, 0:128], in_=NEG_B[:, 0:128], pattern=[[-1, 128]], compare_op=ALU.is_ge,
                        fill=NEGV, base=-1, channel_multiplier=1))
        t_pc = POOL.sig(POOL.e.affine_select(out=NEG_B[:, 128:256], in_=NEG_B[:, 128:256], pattern=[[1, 128]],
                                        compare_op=ALU.is_ge, fill=NEGV, base=0, channel_multiplier=-1))
        d_sk = mk_dsem("sk")
        bc_reg = POOL.e.to_reg(NE * CAP - 1)
        t_sk = d_sk.add(SP.e.dma_start(out=sk_t[:], in_=sinks.to_broadcast([128, 16])))
        DVE.wait(t_pc)
        t_idb = DVE.sig(DVE.e.tensor_copy(out=ident_b[:], in_=ident_f[:]))
        ACT.wait(t_sk)
        t_es = ACT.sig(ACT.e.activation(out=es_t[:], in_=sk_t[:], func=AF.Exp))
        t_const = [t_pc, t_idb, t_es]

        def rms_stats(col, src, junk, src_tok):
            ACT.wait(src_tok)
            t1 = ACT.sig(ACT.e.activation(out=junk, in_=src, func=AF.Square, accum_out=ss[:, col:col + 1]))
            ACT.wait(t1)
            t2 = ACT.sig(ACT.e.activation(out=sd[:, col:col + 1], in_=ss[:, col:col + 1], func=AF.Sqrt,
                                          scale=1.0 / D, bias=EPS))
            DVE.wait(t2)
            t3 = DVE.sig(DVE.e.reciprocal(out=rs[:, col:col + 1], in_=sd[:, col:col + 1]))
            return t3

        dbg = None
        if stage < 99:
            dbg = nc.dram_tensor("dbg", [S, D], F32, kind="ExternalOutput").ap()

        st_mixed = ExitStack()
        mixedT = sb("mixedT", [128, 16, S], BF16, st_mixed)
        st_aT = ExitStack()
        aT = sb("aT", [128, 16, S], BF16, st_aT)

        with ExitStack() as s1:
            xt = [sb("xt%d" % i, [128, D], F32, s1) for i in range(2)]
            at = [sb("at%d" % i, [128, D], BF16, s1) for i in range(2)]
            gm = sb("gm", [128, D], F32, s1)
            junk = sb("junk", [128, D], BF16, s1)
            psT = pst("psT1", [128, 2, 1024], BF16, s1)
            dx = [mk_dsem("x%d" % i) for i in range(2)]
            d_g = mk_dsem("gmix")
            t_g = d_g.add(SP.e.dma_start(out=gm[:], in_=g_mix.to_broadcast([128, D])))
            xt_free = [None, None]
            at_free = [None, None]
            psT_free = [None, None]
            p1 = {}

            def pre1(j):
                b = j % 2
                SP.wait(xt_free[b])
                t_x = dx[b].add(SP.e.dma_start(out=xt[b][:], in_=x[128 * j:128 * j + 128, :]))
                t_rs = rms_stats(j, xt[b][:], junk[:], t_x)
                DVE.wait(t_rs, t_x, t_g, at_free[b])
                t_a = DVE.sig(DVE.e.scalar_tensor_tensor(out=at[b][:], in0=xt[b][:], scalar=rs[:, j:j + 1],
                                                         in1=gm[:], op0=ALU.mult, op1=ALU.mult))
                xt_free[b] = t_a
                p1[j] = t_a

            def post1(j):
                b = j % 2
                PE.wait(p1[j], t_idb)
                t_tr = [None, None]
                for c in range(16):
                    if c % 8 == 0:
                        PE.wait(psT_free[c // 8])
                    ins = PE.e.transpose(out=psT[:, c // 8, (c % 8) * 128:(c % 8) * 128 + 128],
                                         in_=at[b][:, c * 128:c * 128 + 128], identity=ident_b[:])
                    if c % 8 == 7:
                        t_tr[c // 8] = PE.sig(ins)
                at_free[b] = t_tr[1]
                ACT.wait(t_tr[0])
                psT_free[0] = ACT.sig(ACT.e.activation(
                    out=aT[:, 0:8, 128 * j:128 * j + 128],
                    in_=psT[:, 0, :].rearrange("p (c t) -> p c t", c=8), func=AF.Copy))
                DVE.wait(t_tr[1])
                psT_free[1] = DVE.sig(DVE.e.tensor_copy(
                    out=aT[:, 8:16, 128 * j:128 * j + 128],
                    in_=psT[:, 1, :].rearrange("p (c t) -> p c t", c=8)))

            pre1(0)
            for j in range(NT):
                if j + 1 < NT:
                    pre1(j + 1)
                post1(j)
            t_aT = [psT_free[0], psT_free[1]]
            barrier()

        if stage == 1:
            with ExitStack() as sd_:
                tmp = sb("dbgtmp", [128, S], F32, sd_)
                dd = mk_dsem("dbg")
                for c in range(16):
                    DVE.wait(dd.tok())
                    t = DVE.sig(DVE.e.tensor_copy(out=tmp[:], in_=aT[:, c, :]))
                    SP.wait(t)
                    dd.add(SP.e.dma_start(out=dbg[128 * c:128 * c + 128, :], in_=tmp[:]))
                SP.wait(dd.tok())
                barrier()
            st_aT.close()
            st_mixed.close()
            return nc

        with ExitStack() as s2:
            slab = [sb("slab%d" % i, [128, 16, 128], BF16, s2) for i in range(4)]
            ztile = sb("ztile", [128, D], BF16, s2)
            d_xz = mk_dsem("xgzero")
            t_z0 = POOL.sig(POOL.e.memset(ztile[:], 0.0))
            xz_pos = [0]

            def xz_issue():
                i = xz_pos[0]
                if i >= 8:
                    return
                xz_pos[0] += 1
                SP.wait(t_z0)
                d_xz.add(SP.e.dma_start(
                    out=Xg[1024 * i:1024 * i + 1024, :].rearrange("(b p) f -> p b f", p=128),
                    in_=ztile[:].rearrange("p (b f) -> p b f", b=1).to_broadcast([128, 8, D])))

            dW = [mk_dsem("slab%d" % i) for i in range(4)]
            QT = sb("QT", [128, S], BF16, s2)
            KT = sb("KT", [128, S], BF16, s2)
            VT = sb("VT", [128, S], BF16, s2)
            Vs = [sb("Vs%d" % i, [128, 16, 128], BF16, s2) for i in range(2)]
            ODacc = sb("ODacc", [128, 2, S], F32, s2)
            PT = [sb("PT%d" % i, [128, 512], BF16, s2) for i in range(2)]
            MB2 = [[sb("MB_%d_%d" % (i, r), [128, 512], BF16, s2) for r in range(3)] for i in range(2)]
            btmp = [sb("btmp%d" % i, [128, 2, 128], F32, s2) for i in range(2)]
            psP = pst("psP", [128, 2, 512], F32, s2)
            psT = pst("psT2", [128, 1, 1024], BF16, s2)
            psS = pst("psS", [128, 2, 512], F32, s2)
            psO = pst("psO", [128, 2, 512], F32, s2)

            st = dict(slab_n=0, slab_free=[None] * 4, psP_n=0, psP_free=[None, None],
                      psT_free=None, vs_n=0, vs_free=[None, None], mb_n=0, mb_free=[None, None])
            buf_tok = {}

            def mat_job(pieces, dst, scale, dst_free):
                k = st["slab_n"] % 4
                st["slab_n"] += 1
                POOL.wait(st["slab_free"][k])
                off = 0
                tok = None
                for (c0, n) in pieces:
                    tok = dW[k].add(POOL.e.dma_start(
                        out=slab[k][:, :, off:off + n],
                        in_=w_in[:, c0:c0 + n].rearrange("(c p) n -> p c n", p=128)))
                    off += n
                assert off == 128
                bg_issue(3)
                if st["slab_n"] >= 8 and st["slab_n"] % 3 == 2:
                    xz_issue()
                last = None
                for t4 in range(4):
                    bk = st["psP_n"] % 2
                    st["psP_n"] += 1
                    PE.wait(tok, st["psP_free"][bk], t_aT)
                    for kc in range(16):
                        ins = PE.e.matmul(psP[:, bk, :], lhsT=slab[k][:, kc, :], rhs=aT[:, kc, 512 * t4:512 * t4 + 512],
                                          start=(kc == 0), stop=(kc == 15))
                    t_mm = PE.sig(ins)
                    if bk == 0:
                        ACT.wait(t_mm, dst_free)
                        last = ACT.sig(ACT.e.activation(out=dst[:, 512 * t4:512 * t4 + 512], in_=psP[:, bk, :],
                                                        func=AF.Copy, scale=float(scale)))
                    else:
                        DVE.wait(t_mm, dst_free)
                        last = DVE.sig(DVE.e.tensor_scalar(out=dst[:, 512 * t4:512 * t4 + 512], in0=psP[:, bk, :],
                                                           scalar1=float(scale), scalar2=None, op0=ALU.mult))
                    st["psP_free"][bk] = last
                    if t4 == 2:
                        prev = last
                st["slab_free"][k] = t_mm
                return [prev, last]

            def v_layout(src, src_tok, s):
                vi = st["vs_n"] % 2
                st["vs_n"] += 1
                V = Vs[vi]
                toks = []
                for half in range(2):
                    PE.wait(src_tok, st["psT_free"], t_idb)
                    for i8 in range(8):
                        slot = half * 8 + i8
                        b_, rho = slot // s, slot % s
                        start = s * 128 * b_ + rho
                        ins = PE.e.transpose(out=psT[:, 0, i8 * 128:i8 * 128 + 128],
                                             in_=src[:, start:start + 127 * s + 1:s], identity=ident_b[:])
                    t_tr = PE.sig(ins)
                    buf_tok["vt_free"] = t_tr
                    E = ACT if half == 0 else DVE
                    E.wait(t_tr, st["vs_free"][vi])
                    if half == 0:
                        t_e = ACT.sig(ACT.e.activation(out=V[:, 0:8, :],
                                                       in_=psT[:, 0, :].rearrange("p (c t) -> p c t", c=8),
                                                       func=AF.Copy))
                    else:
                        t_e = DVE.sig(DVE.e.tensor_copy(out=V[:, 8:16, :],
                                                        in_=psT[:, 0, :].rearrange("p (c t) -> p c t", c=8)))
                    st["psT_free"] = t_e
                    toks.append(t_e)
                return vi, toks

            def make_mask(coefs, negt):
                par = st["mb_n"] % 2
                st["mb_n"] += 1
                DVE.wait(st["mb_free"][par], t_const)
                t = None
                for r, c in enumerate(coefs):
                    for u in range(2):
                        t = DVE.sig(DVE.e.scalar_tensor_tensor(out=MB2[par][r][:, 256 * u:256 * u + 256], in0=DT[:],
                                                               scalar=-float(c), in1=negt[:], op0=ALU.mult,
                                                               op1=ALU.add))
                return par, t

            batches = []

            def unit_cols(s, b_, rho):
                start = s * 128 * b_ + rho
                return slice(start, start + 127 * s + 1, s)

            for h in range(8):
                slope = 2.0 ** (-(h + 1))
                hstate = {}

                def pre_head(h=h, hstate=hstate, slope=slope):
                    qk_free = buf_tok.get("qk_free")
                    vt_free = buf_tok.get("vt_free")
                    hstate["q"] = mat_job([(h * 128, 128)], QT, 128.0 ** -0.5, qk_free)
                    hstate["k"] = mat_job([(1024 + h * 128, 128)], KT, 1.0, qk_free)
                    hstate["v"] = mat_job([(2048 + h * 128, 128)], VT, 1.0, vt_free)
                    hstate["mb"] = make_mask([slope * 1, slope * 4, slope * 16], NEG_A)

                for r, s in enumerate((1, 4, 16)):
                    units = [(b_, rho) for b_ in range(16 // s) for rho in range(s)]
                    bstate = {}

                    def pre_branch(s=s, hstate=hstate, bstate=bstate):
                        bstate["v"] = v_layout(VT, hstate["v"], s)

                    for ib in range(8):
                        us = units[2 * ib:2 * ib + 2]
                        pres = []
                        if r == 0 and ib == 0:
                            pres.append(pre_head)
                        if ib == 0:
                            pres.append(pre_branch)
                        batches.append(dict(kind="A", h=h, r=r, s=s, units=us, pres=pres, hstate=hstate,
                                            bstate=bstate, first=(r == 0), last_of_head=(r == 2 and ib == 7),
                                            last_of_branch=(ib == 7), rows=slice(0, 128)))

            kvstates = [{}, {}]
            for c in range(8):
                gkv = c // 4
                hstate = {}
                kvstate = kvstates[gkv]

                def pre_kv(gkv=gkv, kvstate=kvstate):
                    qk_free = buf_tok.get("qk_free")
                    vt_free = buf_tok.get("vt_free")
                    k0 = 4096 + gkv * 64
                    v0 = 4224 + gkv * 64
                    kvstate["k"] = mat_job([(k0, 64), (k0, 64)], KT, 1.0, qk_free)
                    kvstate["vT"] = mat_job([(v0, 64), (v0, 64)], VT, 1.0, vt_free)
                    kvstate["v"] = v_layout(VT, kvstate["vT"], 1)

                def pre_chunk(c=c, hstate=hstate, kvstate=kvstate):
                    qk_free = buf_tok.get("qk_free")
                    hstate["q"] = mat_job([(3072 + c * 128, 128)], QT, 64.0 ** -0.5, qk_free)
                    hstate["k"] = kvstate["k"]
                    sl = [2.0 ** (-8.0 * (2 * c + hh + 1) / 16.0) for hh in range(2)]
                    hstate["mb"] = make_mask(sl, NEG_B)

                for hh in range(2):
                    for ib in range(8):
                        us = [(2 * ib, 0), (2 * ib + 1, 0)]
                        pres = []
                        if hh == 0 and ib == 0:
                            if c % 4 == 0:
                                pres.append(pre_kv)
                            pres.append(pre_chunk)
                        batches.append(dict(kind="B", h=2 * c + hh, c=c, hh=hh, r=hh, s=1, units=us, pres=pres,
                                            hstate=hstate, bstate=kvstate, first=True, last_of_head=False,
                                            last_of_branch=(c % 4 == 3 and hh == 1 and ib == 7),
                                            last_q=(hh == 1 and ib == 7),
                                            rows=slice(64 * hh, 64 * hh + 64)))

            if stage == 2:
                batches = [bb for bb in batches if bb["kind"] == "A" and bb["h"] < 1]

            NB = len(batches)
            S_free = [None, None]
            PT_free = [None, None]
            O_free = [None, None]
            tS = [None] * NB
            tP = [None] * NB

            def emit_qk(n):
                bb = batches[n]
                for f in bb["pres"]:
                    f()
                pb = n % 2
                hs = bb["hstate"]
                rows = bb["rows"]
                par, t_mb = hs["mb"]
                PE.wait(S_free[pb], hs["q"], hs["k"], t_mb, t_idb)
                first = True
                for u, (b_, rho) in enumerate(bb["units"]):
                    qc = unit_cols(bb["s"], b_, rho)
                    if b_ > 0:
                        kc = unit_cols(bb["s"], b_ - 1, rho)
                        PE.e.matmul(psS[:, pb, 256 * u:256 * u + 128], lhsT=KT[rows, kc], rhs=QT[rows, qc],
                                    start=first, stop=False, skip_group_check=True)
                        first = False
                    PE.e.matmul(psS[:, pb, 256 * u + 128:256 * u + 256], lhsT=KT[rows, qc], rhs=QT[rows, qc],
                                start=first, stop=False, skip_group_check=True)
                    first = False
                ins = PE.e.matmul(psS[:, pb, :], lhsT=ident_b[:], rhs=MB2[par][bb["r"]][:], start=False, stop=True,
                                  skip_group_check=True)
                tS[n] = PE.sig(ins)
                if bb["last_of_head"] or bb.get("last_q"):
                    buf_tok["qk_free"] = tS[n]
                if bb.get("last_q") or bb["last_of_head"]:
                    st["mb_free"][par] = tS[n]

            def emit_exp(n):
                pb = n % 2
                ACT.wait(tS[n], PT_free[pb])
                tP[n] = ACT.sig(ACT.e.activation(out=PT[pb][:], in_=psS[:, pb, :], func=AF.Exp))
                S_free[pb] = tP[n]

            def emit_pv(n):
                bb = batches[n]
                pb = n % 2
                vi, vtoks = bb["bstate"]["v"]
                V = Vs[vi]
                s = bb["s"]
                PE.wait(tP[n], O_free[pb], vtoks)
                first = True
                ins = None
                for u, (b_, rho) in enumerate(bb["units"]):
                    slot = b_ * s + rho
                    parts = []
                    if b_ > 0:
                        parts.append(((b_ - 1) * s + rho, PT[pb][:, 256 * u:256 * u + 128]))
                    parts.append((slot, PT[pb][:, 256 * u + 128:256 * u + 256]))
                    for i, (sl, pt) in enumerate(parts):
                        PE.e.matmul(psO[:, pb, 256 * u:256 * u + 128], lhsT=V[:, sl, :], rhs=pt,
                                    start=first, stop=False, skip_group_check=True)
                        first = False
                    for i, (sl, pt) in enumerate(parts):
                        ins = PE.e.matmul(psO[:, pb, 256 * u + 128:256 * u + 256], lhsT=ones_b[:], rhs=pt,
                                          start=False, stop=(u == 1 and i == len(parts) - 1),
                                          skip_group_check=True)
                t_o = PE.sig(ins)
                PT_free[pb] = t_o
                if bb["last_of_branch"]:
                    st["vs_free"][vi] = t_o
                return t_o

            def emit_evac(n, t_o):
                bb = batches[n]
                pb = n % 2
                s = bb["s"]
                (b0, r0), (b1, r1) = bb["units"]
                if bb["kind"] == "A":
                    if s == 1:
                        oap = ODacc[:, :, 128 * b0:128 * b0 + 256].rearrange("p o (u q) -> p o u q", u=2)
                    else:
                        lo = s * 128 * b0
                        oap = ODacc[:, :, lo:lo + 128 * s].rearrange("p o (q r) -> p o r q", r=s)[:, :, r0:r0 + 2, :]
                    iap = psO[:, pb, :].rearrange("p (u o q) -> p o u q", u=2, o=2)
                    DVE.wait(t_o, buf_tok.get("od_free"), buf_tok.get("od_last"))
                    if bb["first"]:
                        t_e = DVE.sig(DVE.e.tensor_copy(out=oap, in_=iap))
                    else:
                        t_e = DVE.sig(DVE.e.tensor_tensor(out=oap, in0=iap, in1=oap, op=ALU.add))
                    buf_tok["od_last"] = t_e
                    O_free[pb] = t_e
                    if bb["last_of_head"]:
                        h = bb["h"]
                        ACT.wait(t_e)
                        t0 = ACT.sig(ACT.e.activation(out=ODacc[:, 1, :], in_=ODacc[:, 1, :], func=AF.Ln))
                        ACT.wait(t0)
                        t1 = ACT.sig(ACT.e.activation(out=ODacc[:, 1, :], in_=ODacc[:, 1, :], func=AF.Exp, scale=-1.0))
                        DVE.wait(t1)
                        t2 = DVE.sig(DVE.e.tensor_tensor(out=mixedT[:, h, :], in0=ODacc[:, 0, :], in1=ODacc[:, 1, :],
                                                         op=ALU.mult))
                        buf_tok["od_free"] = t2
                        buf_tok["mixed_last"] = t2
                else:
                    rows = bb["rows"]
                    h = bb["h"]
                    c = bb["c"]
                    iv = psO[rows, pb, :].rearrange("p (u o q) -> p o u q", u=2, o=2)
                    tb = btmp[pb]
                    ACT.wait(t_o, t_es, buf_tok.get("btmp_free%d" % pb))
                    ta = ACT.sig(ACT.e.activation(out=tb[rows, :, :], in_=iv[:, 1, :, :], func=AF.Ln,
                                                  bias=es_t[rows, h:h + 1], scale=1.0))
                    ACT.wait(ta)
                    tb_ = ACT.sig(ACT.e.activation(out=tb[rows, :, :], in_=tb[rows, :, :], func=AF.Exp, scale=-1.0))
                    DVE.wait(tb_)
                    t_e = DVE.sig(DVE.e.tensor_tensor(
                        out=mixedT[rows, 8 + c, 128 * b0:128 * b0 + 256].rearrange("p (u q) -> p u q", u=2),
                        in0=iv[:, 0, :, :], in1=tb[rows, :, :], op=ALU.mult))
                    O_free[pb] = t_e
                    buf_tok["btmp_free%d" % pb] = t_e
                    buf_tok["mixed_last"] = t_e

            emit_qk(0)
            emit_exp(0)
            for n in range(NB):
                if n + 1 < NB:
                    emit_qk(n + 1)
                    emit_exp(n + 1)
                t_o = emit_pv(n)
                emit_evac(n, t_o)
            barrier()

        if stage in (2, 22):
            with ExitStack() as sd_:
                tmp = sb("dbgtmp", [128, S], F32, sd_)
                dd = mk_dsem("dbg")
                for c in range(16):
                    DVE.wait(dd.tok())
                    t = DVE.sig(DVE.e.tensor_copy(out=tmp[:], in_=mixedT[:, c, :]))
                    SP.wait(t)
                    dd.add(SP.e.dma_start(out=dbg[128 * c:128 * c + 128, :], in_=tmp[:]))
                SP.wait(dd.tok())
                barrier()
            st_aT.close()
            st_mixed.close()
            return nc

        st_aT.close()
        st_wout = ExitStack()
        wout = sb("wout", [128, 16, D], BF16, st_wout)
        with ExitStack() as s3:
            xt = [sb("xt3_%d" % i, [128, D], F32, s3) for i in range(2)]
            h1 = [sb("h1_%d" % i, [128, D], F32, s3) for i in range(2)]
            mf = sb("mf", [128, D], F32, s3)
            mhl = sb("mhl", [128, 2, D], BF16, s3)
            mhlT = sb("mhlT", [128, 2, 16, 128], BF16, s3)
            Whl = sb("Whl", [128, 2, 16, 36], BF16, s3)
            mb1 = sb("mb1", [128, D], BF16, s3)
            mb = [mb1, mb1]
            gmo = sb("gmo", [128, D], F32, s3)
            junk = mb1
            Wr = mf[:, 1024:1024 + 16 * 36].rearrange("p (c n) -> p c n", c=16)
            bias_bc = sb("bias_bc", [128, 36], F32, s3)
            psH = pst("psH", [128, 2, 2, 512], F32, s3)
            psF = pst("psF", [128, 2, 1024], BF16, s3)
            psZ = pst("psZ", [128, 512], F32, s3)
            d_w = mk_dsem("wout")
            dx = [mk_dsem("x3_%d" % i) for i in range(2)]
            dh = [mk_dsem("h1s%d" % i) for i in range(2)]
            dm1 = mk_dsem("ms")
            dm = [dm1, dm1]
            SP.wait(d_pre_wo.tok())
            for kc4 in range(4):
                t_w = d_w.add(SP.e.dma_start(out=wout[:, 4 * kc4:4 * kc4 + 4, :], in_=WO[:, 4 * kc4:4 * kc4 + 4, :]))
            t_c3 = d_c.add(SP.e.dma_start(out=gmo[:], in_=g_moe.to_broadcast([128, D])))
            t_c3 = d_c.add(SP.e.dma_start(out=Wr[:, :, 0:4], in_=w_grp.rearrange("(c p) n -> p c n", p=128)))
            t_c3 = d_c.add(SP.e.dma_start(out=Wr[:, :, 4:36], in_=w_exp.rearrange("(c p) n -> p c n", p=128)))
            t_c3 = d_c.add(SP.e.dma_start(out=bias_bc[:, 0:4], in_=b_grp.to_broadcast([128, 4])))
            t_c3 = d_c.add(SP.e.dma_start(out=bias_bc[:, 4:36], in_=b_exp.to_broadcast([128, 32])))

            DVE.wait(t_c3)
            t_ = DVE.sig(DVE.e.tensor_copy(out=Whl[:, 0, :, :], in_=Wr))
            DVE.wait(t_)
            Wtmp = mf[:, 0:16 * 36].rearrange("p (c n) -> p c n", c=16)
            t_ = DVE.sig(DVE.e.tensor_tensor(out=Wtmp, in0=Wr, in1=Whl[:, 0, :, :], op=ALU.subtract))
            DVE.wait(t_)
            t_whl = DVE.sig(DVE.e.tensor_copy(out=Whl[:, 1, :, :], in_=Wtmp))
            mf_free0 = t_whl
            psH_free = [None, None]
            psF_free = [None, None]
            psZ_free = None
            mhl_free = None
            xt_free = [None, None]
            h1_free = [None, None]
            mb_free = [None, None]
            mf_free = t_whl
            mfT_free = None
            t_hmm = {}

            def op_mm(j):
                b = j % 2
                bg_issue(4)
                SP.wait(xt_free[b])
                t_x = dx[b].add(SP.e.dma_start(out=xt[b][:], in_=x[128 * j:128 * j + 128, :]))
                mm = []
                for half in range(2):
                    PE.wait(psH_free[half], t_w)
                    for nn in range(2):
                        for kc in range(16):
                            ins = PE.e.matmul(psH[:, half, nn, :], lhsT=mixedT[:, kc, 128 * j:128 * j + 128],
                                              rhs=wout[:, kc, 1024 * half + 512 * nn:1024 * half + 512 * nn + 512],
                                              start=(kc == 0), stop=(kc == 15))
                    mm.append(PE.sig(ins))
                t_hmm[j] = dict(t_x=t_x, mm=mm)

            def op_add(j):
                b = j % 2
                a = t_hmm[j]
                toks = []
                for half in range(2):
                    DVE.wait(a["mm"][half], a["t_x"], h1_free[b])
                    t_h = DVE.sig(DVE.e.tensor_tensor(out=h1[b][:, 1024 * half:1024 * half + 1024],
                                                      in0=psH[:, half, :, :].rearrange("p n f -> p (n f)"),
                                                      in1=xt[b][:, 1024 * half:1024 * half + 1024], op=ALU.add))
                    psH_free[half] = t_h
                    toks.append(t_h)
                xt_free[b] = toks[1]
                a["toks"] = toks

            def rest_a(j):
                nonlocal mf_free, mfT_free, psZ_free
                b = j % 2
                toks = t_hmm[j]["toks"]
                SP.wait(toks)
                t_hs = dh[b].add(SP.e.dma_start(out=H1[128 * j:128 * j + 128, :], in_=h1[b][:]))
                ACT.wait(mb_free)
                t_rs = rms_stats(16 + j, h1[b][:], junk[:], toks)
                DVE.wait(toks, t_c3, mf_free)
                t_mf = DVE.sig(DVE.e.tensor_tensor(out=mf[:], in0=h1[b][:], in1=gmo[:], op=ALU.mult))
                ACT.wait(t_mf, t_rs, mb_free)
                t_m = ACT.sig(ACT.e.activation(out=mb[b][:], in_=mf[:], func=AF.Copy, scale=rs[:, 16 + j:17 + j]))
                SP.wait(t_m)
                mb_free[b] = dm[b].add(SP.e.dma_start(out=Mscr[128 * j:128 * j + 128, :], in_=mb[b][:]))
                DVE.wait(t_mf, mhl_free)
                t_hi = DVE.sig(DVE.e.tensor_copy(out=mhl[:, 0, :], in_=mf[:]))
                DVE.wait(t_hi)
                t_lo = DVE.sig(DVE.e.tensor_tensor(out=mhl[:, 1, :], in0=mf[:], in1=mhl[:, 0, :], op=ALU.subtract))
                t_hmm[j].update(t_hs=t_hs, t_rs=t_rs, t_mf=t_mf, t_m=t_m, t_lo=t_lo)

            def rest_b(j):
                nonlocal mf_free, mfT_free, psZ_free, mhl_free
                b = j % 2
                a = t_hmm[j]
                t_hs, t_rs, t_mf, t_m, t_lo = a["t_hs"], a["t_rs"], a["t_mf"], a["t_m"], a["t_lo"]
                evs = []
                for q4 in range(4):
                    fb = q4 % 2
                    hl, ch = q4 // 2, q4 % 2
                    PE.wait(t_lo, psF_free[fb], t_idb)
                    for i8 in range(8):
                        c = 8 * ch + i8
                        ins = PE.e.transpose(out=psF[:, fb, 128 * i8:128 * i8 + 128], in_=mhl[:, hl, 128 * c:128 * c + 128],
                                             identity=ident_b[:])
                    t_tr = PE.sig(ins)
                    E = ACT if fb == 0 else DVE
                    E.wait(t_tr, mfT_free)
                    if fb == 0:
                        t_e = ACT.sig(ACT.e.activation(out=mhlT[:, hl, 8 * ch:8 * ch + 8, :],
                                                       in_=psF[:, fb, :].rearrange("p (c t) -> p c t", c=8),
                                                       func=AF.Copy))
                    else:
                        t_e = DVE.sig(DVE.e.tensor_copy(out=mhlT[:, hl, 8 * ch:8 * ch + 8, :],
                                                        in_=psF[:, fb, :].rearrange("p (c t) -> p c t", c=8)))
                    psF_free[fb] = t_e
                    evs.append(t_e)
                mhl_free = t_tr
                mf_free = [t_lo, t_m]
                h1_free[b] = [t_hs, t_mf, t_rs]
                PE.wait(evs, psZ_free, t_whl)
                n_mm = 0
                for (xa, wa_) in ((0, 0), (1, 0), (0, 1)):
                    for c in range(16):
                        ins = PE.e.matmul(psZ[:, 0:36], lhsT=mhlT[:, xa, c, :], rhs=Whl[:, wa_, c, :],
                                          start=(n_mm == 0), stop=(n_mm == 47))
                        n_mm += 1
                t_z = PE.sig(ins)
                mfT_free = t_z
                DVE.wait(t_z, t_rs)
                psZ_free = DVE.sig(DVE.e.scalar_tensor_tensor(out=lg_all[:, j, :], in0=psZ[:, 0:36],
                                                              scalar=rs[:, 16 + j:17 + j], in1=bias_bc[:],
                                                              op0=ALU.mult, op1=ALU.add))

            op_mm(0)
            op_add(0)
            for j in range(NT):
                if j + 1 < NT:
                    op_mm(j + 1)
                rest_a(j)
                if j + 1 < NT:
                    op_add(j + 1)
                rest_b(j)
            barrier()
        st_wout.close()
        st_mixed.close()

        if stage == 3:
            with ExitStack() as sd_:
                tmp = sb("dbgtmp", [128, S], F32, sd_)
                dd = mk_dsem("dbg")
                for c in range(16):
                    SP.wait(dd.tok())
                    dd.add(SP.e.dma_start(out=tmp[:], in_=H1[128 * c:128 * c + 128, :]))
                    SP.wait(dd.tok())
                    dd.add(SP.e.dma_start(out=dbg[128 * c:128 * c + 128, :], in_=tmp[:]))
                SP.wait(dd.tok())
                barrier()
            return nc

        bg_issue(1000)
        st_wpg = ExitStack()
        wpg = sb("wpg", [128, 16, D], BF16, st_wpg)
        d_wpg = mk_dsem("wpg")
        with ExitStack() as s5:
            NMT = 4
            mt = [sb("mt%d" % i, [128, D], BF16, s5) for i in range(NMT)]
            wgu = [sb("wgu%d" % i, [128, 16, 512], BF16, s5) for i in range(2)]
            wdn = [sb("wdn%d" % i, [128, 2, D], BF16, s5) for i in range(2)]
            xs = [[sb("xs%d_%d" % (i, s_), [128, D], BF16, s5) for s_ in range(2)] for i in range(2)]
            XT = [sb("XT%d" % i, [128, 16, 256], BF16, s5) for i in range(2)]
            sg = sb("sg", [128, 2, 256], F32, s5)
            hidT = sb("hidT", [128, 2, 256], BF16, s5)
            yb = [sb("yb%d" % i, [128, D], BF16, s5) for i in range(2)]
            dmt = [mk_dsem("mt%d" % i) for i in range(NMT)]
            dsc = [mk_dsem("sc%d" % i) for i in range(NMT)]
            dwe = [mk_dsem("we%d" % i) for i in range(2)]
            dxs = [mk_dsem("xs%d" % i) for i in range(2)]
            dys = [mk_dsem("ys%d" % i) for i in range(2)]
            mt_tok = {}
            for j in range(NMT):
                mt_tok[j] = dmt[j].add(SP.e.dma_start(out=mt[j][:], in_=Mscr[128 * j:128 * j + 128, :]))

            with ExitStack() as s4:
                def t3(name, shape, dt=F32):
                    return sb(name, shape, dt, s4)
                gmax = t3("gmax", [128, NT]); goh = t3("goh", [128, NT, 4]); tmp4 = t3("tmp4", [128, NT, 4])
                sume = t3("sume", [128, NT]); grpw = t3("grpw", [128, NT]); pen4 = t3("pen4", [128, NT, 4])
                Lm = t3("Lm", [128, NT, 32]); Lm2 = t3("Lm2", [128, NT, 32])
                m1 = t3("m1", [128, NT]); m2 = t3("m2", [128, NT])
                oh = [t3("oh%d" % k, [128, NT, 32]) for k in range(2)]
                Mk = t3("Mk", [128, NT, 32], BF16)
                pos = t3("pos", [128, NT, 32]); tmp32 = t3("tmp32", [128, NT, 32])
                iota3 = t3("iota3", [128, NT, 32])
                pk = t3("pk", [128, NT]); ek = t3("ek", [128, NT]); dk = t3("dk", [128, NT]); ok = t3("ok", [128, NT])
                d21 = t3("d21", [128, NT]); e21 = t3("e21", [128, NT]); wa = t3("wa", [128, NT]); wb_ = t3("wb_", [128, NT])
                psR = pst("psR", [128, 512], F32, s4)
                t_io = POOL.sig(POOL.e.iota(iota3[:], pattern=[[0, NT], [1, 32]], base=0, channel_multiplier=0,
                                            allow_small_or_imprecise_dtypes=True))
                last = [None]

                def dv(inst_fn, extra=None):
                    DVE.wait(last[0], extra)
                    last[0] = DVE.sig(inst_fn())
                    return last[0]

                Lg = lg_all[:, :, 0:4]
                Le4 = lg_all[:, :, 4:36].rearrange("p j (g e) -> p j g e", g=4)
                V = DVE.e
                dv(lambda: V.tensor_reduce(out=gmax[:], in_=Lg, axis=AX.X, op=ALU.max))
                dv(lambda: V.tensor_tensor(out=goh[:], in0=Lg, in1=gmax[:].to_broadcast([128, NT, 4]), op=ALU.is_equal))
                t = dv(lambda: V.tensor_tensor(out=tmp4[:], in0=Lg, in1=gmax[:].to_broadcast([128, NT, 4]), op=ALU.subtract))
                ACT.wait(t)
                t = ACT.sig(ACT.e.activation(out=tmp4[:], in_=tmp4[:], func=AF.Exp))
                dv(lambda: V.tensor_reduce(out=sume[:], in_=tmp4[:], axis=AX.X, op=ALU.add), t)
                dv(lambda: V.reciprocal(out=grpw[:], in_=sume[:]))
                dv(lambda: V.tensor_scalar(out=pen4[:], in0=goh[:], scalar1=BIGV, scalar2=-BIGV, op0=ALU.mult, op1=ALU.add))
                dv(lambda: V.tensor_tensor(out=Lm[:].rearrange("p j (g e) -> p j g e", g=4), in0=Le4,
                                           in1=pen4[:].to_broadcast([128, NT, 4, 8]), op=ALU.add))
                dv(lambda: V.tensor_reduce(out=m1[:], in_=Lm[:], axis=AX.X, op=ALU.max))
                dv(lambda: V.tensor_tensor(out=oh[0][:], in0=Lm[:], in1=m1[:].to_broadcast([128, NT, 32]), op=ALU.is_equal))
                dv(lambda: V.scalar_tensor_tensor(out=Lm2[:], in0=oh[0][:], scalar=-BIGV, in1=Lm[:], op0=ALU.mult, op1=ALU.add))
                dv(lambda: V.tensor_reduce(out=m2[:], in_=Lm2[:], axis=AX.X, op=ALU.max))
                dv(lambda: V.tensor_tensor(out=oh[1][:], in0=Lm2[:], in1=m2[:].to_broadcast([128, NT, 32]), op=ALU.is_equal))
                t_mk = dv(lambda: V.tensor_tensor(out=Mk[:], in0=oh[0][:], in1=oh[1][:], op=ALU.add))
                t = dv(lambda: V.tensor_tensor(out=d21[:], in0=m2[:], in1=m1[:], op=ALU.subtract))
                ACT.wait(t)
                t = ACT.sig(ACT.e.activation(out=e21[:], in_=d21[:], func=AF.Exp))
                dv(lambda: V.tensor_scalar(out=wa[:], in0=e21[:], scalar1=1.0, scalar2=None, op0=ALU.add), t)
                dv(lambda: V.reciprocal(out=wa[:], in_=wa[:]))
                dv(lambda: V.tensor_tensor(out=wb_[:], in0=wa[:], in1=e21[:], op=ALU.mult))
                dv(lambda: V.tensor_tensor(out=wa[:], in0=wa[:], in1=grpw[:], op=ALU.mult))
                dv(lambda: V.tensor_tensor(out=wb_[:], in0=wb_[:], in1=grpw[:], op=ALU.mult))
                PE.wait(t_mk, t_pc)
                for j in range(NT):
                    PE.e.matmul(psR[:, 32 * j:32 * j + 32], lhsT=Utri[:], rhs=Mk[:, j, :], start=(j == 0), stop=False,
                                skip_group_check=True)
                for j in range(NT - 1):
                    for j2 in range(j + 1, NT):
                        ins = PE.e.matmul(psR[:, 32 * j2:32 * j2 + 32], lhsT=ones_b[:], rhs=Mk[:, j, :], start=False,
                                          stop=(j == NT - 2), skip_group_check=True)
                t_pos = PE.sig(ins)
                dv(lambda: V.tensor_copy(out=pos[:].rearrange("p j e -> p (j e)"), in_=psR[:]), t_pos)
                for k in range(2):
                    wk = wa if k == 0 else wb_
                    dv(lambda: V.tensor_tensor(out=tmp32[:], in0=oh[k][:], in1=pos[:], op=ALU.mult))
                    dv(lambda: V.tensor_reduce(out=pk[:], in_=tmp32[:], axis=AX.X, op=ALU.add))
                    dv(lambda: V.tensor_tensor(out=tmp32[:], in0=oh[k][:], in1=iota3[:], op=ALU.mult), t_io)
                    dv(lambda: V.tensor_reduce(out=ek[:], in_=tmp32[:], axis=AX.X, op=ALU.add))
                    dv(lambda: V.scalar_tensor_tensor(out=dk[:], in0=ek[:], scalar=float(CAP), in1=pk[:], op0=ALU.mult, op1=ALU.add))
                    dv(lambda: V.tensor_scalar(out=ok[:], in0=pk[:], scalar1=float(CAP), scalar2=1.0e6, op0=ALU.is_ge, op1=ALU.mult))
                    dv(lambda: V.tensor_tensor(out=dk[:], in0=dk[:], in1=ok[:], op=ALU.add))
                    dv(lambda: V.tensor_copy(out=dst_i[:, k, :], in_=dk[:]))
                    dv(lambda: V.tensor_scalar(out=ok[:], in0=pk[:], scalar1=float(CAP), scalar2=None, op0=ALU.is_lt))
                    dv(lambda: V.tensor_tensor(out=wts[:, k, :], in0=wk[:], in1=ok[:], op=ALU.mult))
                t_route = last[0]


            s5p = ExitStack()
            psT = pst("psT5", [128, 2, 1024], BF16, s5p)
            psG = pst("psG", [128, 2, 2, 256], F32, s5p)
            psY = pst("psY", [128, 2, 2, 512], F32, s5p)

            ACT.wait(d_pre_pg.tok())
            for kc4 in range(4):
                t_wpg = d_wpg.add(ACT.e.dma_start(out=wpg[:, 4 * kc4:4 * kc4 + 4, :], in_=WPG[:, 4 * kc4:4 * kc4 + 4, :]))
            sc_tok = [None] * NMT
            for j in range(NT):
                b = j % NMT
                if j >= NMT:
                    SP.wait(sc_tok[b])
                    mt_tok[j] = dmt[b].add(SP.e.dma_start(out=mt[b][:], in_=Mscr[128 * j:128 * j + 128, :]))
                POOL.wait(mt_tok[j], t_route, d_xz.tok())
                for k in range(2):
                    sc_tok[b] = dsc[b].add(POOL.e.indirect_dma_start(
                        out=Xg, out_offset=bass.IndirectOffsetOnAxis(ap=dst_i[:, k, j:j + 1], axis=0),
                        in_=mt[b][:], in_offset=None, bounds_check=bc_reg, oob_is_err=False))
            t_scatter = list(sc_tok)

            def load_w(e):
                b = e % 2
                ACT.wait(w_free[b], d_pre_ex[e // 4].tok())
                dwe[b].add(ACT.e.dma_start(out=wgu[b][:].rearrange("p c f -> p (c f)"), in_=WGU[e]))
                return dwe[b].add(ACT.e.dma_start(out=wdn[b][:].rearrange("p c d -> p (c d)"), in_=WDN[e]))

            def load_x(e):
                b = e % 2
                SP.wait(t_scatter, xs_free[b])
                for s_ in range(2):
                    t = dxs[b].add(SP.e.dma_start(out=xs[b][s_][:], in_=Xg[e * CAP + 128 * s_:e * CAP + 128 * s_ + 128, :]))
                return t

            w_free = [None, None]
            xs_free = [None, None]
            psT_free = [None, None]
            psG_free = [None, None]
            psY_free = [None, None]
            yb_free = [None, None]
            XT_free = [None, None]
            hid_free = None
            sg_free = None
            xt_evs = {}
            t_w = {0: load_w(0)}
            t_x = {0: load_x(0)}
            if NE > 1:
                t_x[1] = load_x(1)

            def tr_round(e, r):
                b = e % 2
                s_, ch = r // 2, r % 2
                tb = r % 2
                PE.wait(t_x[e], psT_free[tb], t_idb)
                for i8 in range(8):
                    c = 8 * ch + i8
                    ins = PE.e.transpose(out=psT[:, tb, 128 * i8:128 * i8 + 128],
                                         in_=xs[b][s_][:, 128 * c:128 * c + 128], identity=ident_b[:])
                t_tr = PE.sig(ins)
                E = ACT if tb == 0 else DVE
                E.wait(t_tr, XT_free[b])
                if tb == 0:
                    t_e = ACT.sig(ACT.e.activation(out=XT[b][:, 8 * ch:8 * ch + 8, 128 * s_:128 * s_ + 128],
                                                   in_=psT[:, tb, :].rearrange("p (c t) -> p c t", c=8), func=AF.Copy))
                else:
                    t_e = DVE.sig(DVE.e.tensor_copy(out=XT[b][:, 8 * ch:8 * ch + 8, 128 * s_:128 * s_ + 128],
                                                    in_=psT[:, tb, :].rearrange("p (c t) -> p c t", c=8)))
                psT_free[tb] = t_e
                xt_evs.setdefault(e, []).append(t_e)
                if r == 3:
                    xs_free[b] = t_tr

            for r in range(4):
                tr_round(0, r)
            for e in range(NE):
                b = e % 2
                if e + 1 < NE:
                    t_w[e + 1] = load_w(e + 1)
                if e + 2 < NE:
                    t_x[e + 2] = load_x(e + 2)
                hts = []
                for fc in range(2):
                    PE.wait(xt_evs[e], t_w[e], psG_free[fc])
                    for gu in range(2):
                        for kc in range(16):
                            ins = PE.e.matmul(psG[:, fc, gu, :], lhsT=wgu[b][:, kc, 256 * gu + 128 * fc:256 * gu + 128 * fc + 128],
                                              rhs=XT[b][:, kc, :], start=(kc == 0), stop=(kc == 15))
                        if gu == 1:
                            t_g = PE.sig(ins)
                        if e + 1 < NE:
                            tr_round(e + 1, 2 * fc + gu)
                    ACT.wait(t_g, sg_free)
                    t_s = ACT.sig(ACT.e.activation(out=sg[:, fc, :], in_=psG[:, fc, 0, :], func=AF.Silu))
                    DVE.wait(t_s, hid_free)
                    t_h = DVE.sig(DVE.e.tensor_tensor(out=hidT[:, fc, :], in0=sg[:, fc, :], in1=psG[:, fc, 1, :], op=ALU.mult))
                    psG_free[fc] = t_h
                    hts.append(t_h)
                XT_free[b] = t_g
                sg_free = hts[1]
                ev_s = []
                for u in range(4):
                    s_, half = u // 2, u % 2
                    yi = u % 2
                    PE.wait(hts, psY_free[yi])
                    for nn in range(2):
                        for fc in range(2):
                            ins = PE.e.matmul(psY[:, yi, nn, :], lhsT=hidT[:, fc, 128 * s_:128 * s_ + 128],
                                              rhs=wdn[b][:, fc, 1024 * half + 512 * nn:1024 * half + 512 * nn + 512],
                                              start=(fc == 0), stop=(fc == 1))
                    t_y = PE.sig(ins)
                    if yi == 0:
                        ACT.wait(t_y, yb_free[s_])
                        t_c = ACT.sig(ACT.e.activation(out=yb[s_][:, 1024 * half:1024 * half + 1024],
                                                       in_=psY[:, yi, :, :].rearrange("p n f -> p (n f)"), func=AF.Copy))
                    else:
                        DVE.wait(t_y, yb_free[s_])
                        t_c = DVE.sig(DVE.e.tensor_copy(out=yb[s_][:, 1024 * half:1024 * half + 1024],
                                                        in_=psY[:, yi, :, :].rearrange("p n f -> p (n f)")))
                    psY_free[yi] = t_c
                    ev_s.append(t_c)
                    if half == 1:
                        SP.wait(ev_s)
                        ev_s = []
                        yb_free[s_] = dys[s_].add(SP.e.dma_start(out=Y[e * CAP + 128 * s_:e * CAP + 128 * s_ + 128, :], in_=yb[s_][:]))
                hid_free = t_y
                w_free[b] = t_y
            barrier()
            s5p.close()

        with ExitStack() as s6:
            h2 = [sb("h2_%d" % i, [128, D], F32, s6) for i in range(3)]
            y1 = [sb("y1_%d" % i, [128, D], BF16, s6) for i in range(2)]
            y2 = [sb("y2_%d" % i, [128, D], BF16, s6) for i in range(2)]
            nb = [sb("nb%d" % i, [128, D], BF16, s6) for i in range(2)]
            nT = [sb("nT%d" % i, [128, 16, 128], BF16, s6) for i in range(2)]
            ptl = [sb("ptl%d" % i, [128, 256], F32, s6) for i in range(2)]
            pbf = [sb("pbf%d" % i, [128, 256], BF16, s6) for i in range(2)]
            pT = [sb("pT%d" % i, [128, 2, 128], BF16, s6) for i in range(2)]
            wple = sb("wple", [128, 2, D], BF16, s6)
            gpl = sb("gpl", [128, D], F32, s6)
            gfn = sb("gfn", [128, D], F32, s6)
            sgm = [sb("sgm%d" % i, [128, 512], F32, s6) for i in range(2)]
            ot = [sb("ot%d" % i, [128, D], F32, s6) for i in range(2)]
            junk = sb("junk6", [128, D], BF16, s6)
            psT = pst("psT6", [128, 2, 1024], BF16, s6)
            psGt = pst("psGt", [128, 2, 512], F32, s6)
            psPe = pst("psPe", [128, 2, 512], F32, s6)
            d_c6 = mk_dsem("c6")
            dh2 = [mk_dsem("h2l%d" % i) for i in range(3)]
            dy = [mk_dsem("yg%d" % i) for i in range(2)]
            dp = [mk_dsem("pl%d" % i) for i in range(2)]
            dout = [mk_dsem("out%d" % i) for i in range(2)]
            t_c6 = d_c6.add(POOL.e.dma_start(out=wple[:], in_=w_ple.rearrange("(c p) d -> p c d", p=128)))
            t_c6 = d_c6.add(SP.e.dma_start(out=gpl[:], in_=g_ple.to_broadcast([128, D])))
            t_c6 = d_c6.add(SP.e.dma_start(out=gfn[:], in_=g_final.to_broadcast([128, D])))
            h2_free = [None, None, None]
            y_free = [None, None]
            nb_free = [None, None]
            nT_free = [None, None]
            p_free = [None, None]
            pbf_free = [None, None]
            pT_free = [None, None]
            psT_free = [None, None]
            psQ_free = [None, None]
            sgm_free = [None, None]
            ot_free = [None, None]
            stA = {}

            def A_pre(j):
                b = j % 2
                SP.wait(h2_free[j % 3])
                t_h = dh2[j % 3].add(SP.e.dma_start(out=h2[j % 3][:], in_=H1[128 * j:128 * j + 128, :]))
                SP.wait(p_free[b])
                t_p = dp[b].add(SP.e.dma_start(out=ptl[b][:], in_=p_in[128 * j:128 * j + 128, :]))
                POOL.wait(y_free[b])
                dy[b].add(POOL.e.indirect_dma_start(out=y1[b][:], out_offset=None, in_=Y,
                                                    in_offset=bass.IndirectOffsetOnAxis(ap=dst_i[:, 0, j:j + 1], axis=0),
                                                    bounds_check=bc_reg, oob_is_err=False))
                t_y = dy[b].add(POOL.e.indirect_dma_start(out=y2[b][:], out_offset=None, in_=Y,
                                                          in_offset=bass.IndirectOffsetOnAxis(ap=dst_i[:, 1, j:j + 1], axis=0),
                                                          bounds_check=bc_reg, oob_is_err=False))
                DVE.wait(t_h, t_y)
                t1 = DVE.sig(DVE.e.scalar_tensor_tensor(out=h2[j % 3][:], in0=y1[b][:], scalar=wts[:, 0, j:j + 1], in1=h2[j % 3][:],
                                                        op0=ALU.mult, op1=ALU.add))
                DVE.wait(t1)
                t2 = DVE.sig(DVE.e.scalar_tensor_tensor(out=h2[j % 3][:], in0=y2[b][:], scalar=wts[:, 1, j:j + 1], in1=h2[j % 3][:],
                                                        op0=ALU.mult, op1=ALU.add))
                y_free[b] = t2
                t_rs = rms_stats(32 + j, h2[j % 3][:], junk[:], t2)
                DVE.wait(t_rs, t_c6, nb_free[b])
                t_n = DVE.sig(DVE.e.scalar_tensor_tensor(out=nb[b][:], in0=h2[j % 3][:], scalar=rs[:, 32 + j:33 + j], in1=gpl[:],
                                                         op0=ALU.mult, op1=ALU.mult))
                ACT.wait(t_p, pbf_free[b])
                t_pb = ACT.sig(ACT.e.activation(out=pbf[b][:], in_=ptl[b][:], func=AF.Copy))
                p_free[b] = t_pb
                stA[j] = dict(t_n=t_n, t_pb=t_pb)

            def A_tr(j):
                b = j % 2
                a = stA[j]
                evs = []
                for ch in range(2):
                    PE.wait(a["t_n"], psT_free[ch])
                    for i8 in range(8):
                        c = 8 * ch + i8
                        ins = PE.e.transpose(out=psT[:, ch, 128 * i8:128 * i8 + 128], in_=nb[b][:, 128 * c:128 * c + 128],
                                             identity=ident_b[:])
                    if ch == 0:
                        t_tr = PE.sig(ins)
                        ACT.wait(t_tr, nT_free[b])
                        t_e = ACT.sig(ACT.e.activation(out=nT[b][:, 0:8, :],
                                                       in_=psT[:, 0, :].rearrange("p (c t) -> p c t", c=8), func=AF.Copy))
                        psT_free[0] = t_e
                        evs.append(t_e)
                    else:
                        t_tr = PE.sig(ins)
                        ACT.wait(t_tr, nT_free[b])
                        t_e = ACT.sig(ACT.e.activation(out=nT[b][:, 8:16, :],
                                                       in_=psT[:, 1, :].rearrange("p (c t) -> p c t", c=8), func=AF.Copy))
                        psT_free[1] = t_e
                        evs.append(t_e)
                nb_free[b] = t_tr
                PE.wait(a["t_pb"], psT_free[0])
                for c in range(2):
                    ins = PE.e.transpose(out=psT[:, 0, 128 * c:128 * c + 128], in_=pbf[b][:, 128 * c:128 * c + 128],
                                         identity=ident_b[:])
                t_tr = PE.sig(ins)
                pbf_free[b] = t_tr
                ACT.wait(t_tr, pT_free[b])
                t_e = ACT.sig(ACT.e.activation(out=pT[b][:], in_=psT[:, 0, 0:256].rearrange("p (c t) -> p c t", c=2),
                                               func=AF.Copy))
                psT_free[0] = t_e
                evs.append(t_e)
                a["evs"] = evs

            def B_mm_q(j, q):
                b = j % 2
                a = stA[j]
                qb = q % 2
                PE.wait(a["evs"], t_wpg, t_c6, psQ_free[qb])
                for kc in range(16):
                    PE.e.matmul(psGt[:, qb, :], lhsT=nT[b][:, kc, :], rhs=wpg[:, kc, 512 * q:512 * q + 512],
                                start=(kc == 0), stop=(kc == 15))
                for kc in range(2):
                    ins = PE.e.matmul(psPe[:, qb, :], lhsT=pT[b][:, kc, :], rhs=wple[:, kc, 512 * q:512 * q + 512],
                                      start=(kc == 0), stop=(kc == 1))
                t_mm = PE.sig(ins)
                ACT.wait(t_mm, sgm_free[qb])
                t_s = ACT.sig(ACT.e.activation(out=sgm[qb][:], in_=psGt[:, qb, :], func=AF.Sigmoid))
                DVE.wait(t_s)
                t_a = DVE.sig(DVE.e.tensor_tensor(out=sgm[qb][:], in0=sgm[qb][:], in1=psPe[:, qb, :], op=ALU.mult))
                psQ_free[qb] = t_a
                DVE.wait(t_a)
                tq = DVE.sig(DVE.e.tensor_tensor(out=h2[j % 3][:, 512 * q:512 * q + 512],
                                                 in0=h2[j % 3][:, 512 * q:512 * q + 512], in1=sgm[qb][:], op=ALU.add))
                sgm_free[qb] = tq
                a["tq"] = tq
                if q == 3:
                    nT_free[b] = t_mm
                    pT_free[b] = t_mm

            def B_fin(j):
                b = j % 2
                a = stA[j]
                t_rs = rms_stats(48 + j, h2[j % 3][:], junk[:], a["tq"])
                DVE.wait(t_rs, ot_free[b])
                t_o = DVE.sig(DVE.e.scalar_tensor_tensor(out=ot[b][:], in0=h2[j % 3][:], scalar=rs[:, 48 + j:49 + j], in1=gfn[:],
                                                         op0=ALU.mult, op1=ALU.mult))
                h2_free[j % 3] = t_o
                ACT.wait(t_o)
                ot_free[b] = dout[b].add(ACT.e.dma_start(out=out[128 * j:128 * j + 128, :], in_=ot[b][:]))

            A_pre(0)
            A_tr(0)
            for j in range(NT):
                B_mm_q(j, 0)
                B_mm_q(j, 1)
                if j + 1 < NT:
                    A_pre(j + 1)
                B_mm_q(j, 2)
                B_mm_q(j, 3)
                if j + 1 < NT:
                    A_tr(j + 1)
                B_fin(j)
            barrier()
        st_wpg.close()
    return nc


_IN_NAMES = ["x", "p", "w_in", "w_out", "sinks", "g_mix", "g_moe", "g_ple", "g_final", "w_grp", "b_grp",
             "w_exp", "b_exp", "w_gate", "w_up", "w_down", "w_ple", "w_ple_gate"]


def make_in_maps(inputs, n_cores=8):
    f = lambda a: np.ascontiguousarray(np.asarray(a, dtype=np.float32))
    shared = {
        "w_in": f(inputs["w_in"][0]), "w_out": f(inputs["w_out"][0]),
        "sinks": f(inputs["sinks"]).reshape(1, 16),
        "g_mix": f(inputs["g_mix"]).reshape(1, D), "g_moe": f(inputs["g_moe"]).reshape(1, D),
        "g_ple": f(inputs["g_ple"]).reshape(1, D), "g_final": f(inputs["g_final"]).reshape(1, D),
        "w_grp": f(inputs["w_grp"][0]), "b_grp": f(inputs["b_grp"]).reshape(1, 4),
        "w_exp": f(inputs["w_exp"][0]).reshape(D, 32), "b_exp": f(inputs["b_exp"]).reshape(1, 32),
        "w_gate": f(inputs["w_gate"][0]), "w_up": f(inputs["w_up"][0]), "w_down": f(inputs["w_down"][0]),
        "w_ple": f(inputs["w_ple"][0]), "w_ple_gate": f(inputs["w_ple_gate"][0]),
    }
    xs = f(inputs["x"])
    ps = f(inputs["p"])
    maps = []
    for c in range(n_cores):
        m = dict(shared)
        m["x"] = xs[c]
        m["p"] = ps[0, c]
        maps.append(m)
    return maps


def kernel(**inputs):
    nc = build()
    in_maps = make_in_maps(inputs)
    res = run_bass_kernel_spmd(nc, in_maps, core_ids=list(range(8)))
    return np.stack([r["out"] for r in res.results], axis=0).astype(np.float32)
```

```python
import numpy as np
from contextlib import ExitStack
import concourse.bass as bass
import concourse.mybir as mybir
from concourse.bass_utils import run_bass_kernel_spmd

F32 = mybir.dt.float32
BF16 = mybir.dt.bfloat16
I32 = mybir.dt.int32
AF = mybir.ActivationFunctionType
ALU = mybir.AluOpType
AX = mybir.AxisListType

S = 2048
D = 2048
NT = 16
NE = 32
CAP = 256
NEGV = -30000.0
BIGV = 1.0e4
EPS = 1e-6


class Eng:
    def __init__(self, nc, es, eng, name):
        self.e = eng
        self.sem = es.enter_context(nc.semaphore("sem_" + name))
        self.cnt = 0
        self.seen = {}

    def wait(self, *toks):
        for t in toks:
            if t is None:
                continue
            if isinstance(t, list):
                self.wait(*t)
                continue
            sem, val = t
            k = id(sem)
            if self.seen.get(k, 0) >= val:
                continue
            self.e.wait_ge(sem, val)
            self.seen[k] = val

    def sig(self, inst):
        self.cnt += 1
        inst.then_inc(self.sem, 1)
        return (self.sem, self.cnt)


class DSem:
    def __init__(self, nc, es, name, reg):
        self.sem = es.enter_context(nc.semaphore("dma_" + name))
        self.cnt = 0
        reg.append(self)

    def add(self, inst):
        self.cnt += 16
        inst.then_inc(self.sem, 16)
        return (self.sem, self.cnt)

    def tok(self):
        return (self.sem, self.cnt) if self.cnt else None


def build(stage=99):
    nc = bass.Bass("TRN2", target_bir_lowering=False)

    def din(n, s, d=F32):
        return nc.dram_tensor(n, s, d, kind="ExternalInput").ap()

    x = din("x", [S, D])
    p_in = din("p", [S, 256])
    w_in = din("w_in", [D, 4352])
    w_out = din("w_out", [D, D])
    sinks = din("sinks", [1, 16])
    g_mix = din("g_mix", [1, D])
    g_moe = din("g_moe", [1, D])
    g_ple = din("g_ple", [1, D])
    g_final = din("g_final", [1, D])
    w_grp = din("w_grp", [D, 4])
    b_grp = din("b_grp", [1, 4])
    w_exp = din("w_exp", [D, 32])
    b_exp = din("b_exp", [1, 32])
    w_gate = din("w_gate", [NE, D, 256])
    w_up = din("w_up", [NE, D, 256])
    w_down = din("w_down", [NE, 256, D])
    w_ple = din("w_ple", [256, D])
    w_pg = din("w_ple_gate", [D, D])
    out = nc.dram_tensor("out", [S, D], F32, kind="ExternalOutput").ap()
    H1 = nc.dram_tensor("H1", [S, D], F32, kind="Internal").ap()
    Mscr = nc.dram_tensor("Mscr", [S, D], BF16, kind="Internal").ap()
    Xg = nc.dram_tensor("Xg", [NE * CAP, D], BF16, kind="Internal").ap()
    Y = nc.dram_tensor("Y", [NE * CAP, D], BF16, kind="Internal").ap()
    WGU = nc.dram_tensor("WGU", [NE, 128, 16 * 512], BF16, kind="Internal").ap()
    WDN = nc.dram_tensor("WDN", [NE, 128, 2 * D], BF16, kind="Internal").ap()
    WO = nc.dram_tensor("WO", [128, 16, D], BF16, kind="Internal").ap()
    WPG = nc.dram_tensor("WPG", [128, 16, D], BF16, kind="Internal").ap()

    es = ExitStack()
    with es:
        PE = Eng(nc, es, nc.tensor, "pe")
        ACT = Eng(nc, es, nc.scalar, "act")
        DVE = Eng(nc, es, nc.vector, "dve")
        POOL = Eng(nc, es, nc.gpsimd, "pool")
        SP = Eng(nc, es, nc.sync, "sp")
        ENGS = [PE, ACT, DVE, POOL, SP]
        dsems = []
        bar_sem = es.enter_context(nc.semaphore("bar"))
        bar_cnt = [0]

        def mk_dsem(name):
            return DSem(nc, es, name, dsems)

        def barrier():
            for ds in dsems:
                SP.wait(ds.tok())
            bar_cnt[0] += len(ENGS)
            for E in ENGS:
                E.e.drain().then_inc(bar_sem, 1)
            for E in ENGS:
                E.e.wait_ge(bar_sem, bar_cnt[0])

        d_pre_wo = DSem(nc, es, "pre_wo", [])
        d_pre_ex = [DSem(nc, es, "pre_ex%d" % g_, []) for g_ in range(8)]
        d_pre_pg = DSem(nc, es, "pre_pg", [])
        bg_list = []
        for kc in range(16):
            bg_list.append((d_pre_wo, WO[:, kc, :], w_out[128 * kc:128 * kc + 128, :]))
        for kc in range(16):
            bg_list.append((d_pre_pg, WPG[:, kc, :], w_pg[128 * kc:128 * kc + 128, :]))
        for e in range(NE):
            wv = WGU[e].rearrange("p (c f) -> p c f", c=16)
            bg_list.append((d_pre_ex[e // 4], wv[:, :, 0:256], w_gate[e].rearrange("(c p) f -> p c f", p=128)))
            bg_list.append((d_pre_ex[e // 4], wv[:, :, 256:512], w_up[e].rearrange("(c p) f -> p c f", p=128)))
            bg_list.append((d_pre_ex[e // 4], WDN[e].rearrange("p (c d) -> p c d", c=2),
                            w_down[e].rearrange("(c p) d -> p c d", p=128)))
        bg_pos = [0]

        def bg_issue(n):
            for _ in range(n):
                if bg_pos[0] >= len(bg_list):
                    return
                ds, o, i = bg_list[bg_pos[0]]
                bg_pos[0] += 1
                ds.add(POOL.e.dma_start(out=o, in_=i))

        def sb(name, shape, dt, stack=es):
            return stack.enter_context(nc.sbuf_tensor(name, shape, dt))

        def pst(name, shape, dt, stack):
            return stack.enter_context(nc.psum_tensor(name, shape, dt))

        ident_f = sb("ident_f", [128, 128], F32)
        ident_b = sb("ident_b", [128, 128], BF16)
        ones_b = sb("ones_b", [128, 128], BF16)
        Utri = sb("Utri", [128, 128], BF16)
        DT = sb("DT", [128, 256], F32)
        NEG_A = sb("NEG_A", [128, 256], F32)
        NEG_B = sb("NEG_B", [128, 256], F32)
        es_t = sb("es_t", [128, 16], F32)
        sk_t = sb("sk_t", [128, 16], F32)
        ss = sb("ss", [128, 64], F32)
        sd = sb("sd", [128, 64], F32)
        rs = sb("rs", [128, 64], F32)
        d_c = mk_dsem("const")
        lg_all = sb("lg_all", [128, NT, 36], F32)
        dst_i = sb("dst_i", [128, 2, NT], I32)
        wts = sb("wts", [128, 2, NT], F32)

        g = POOL.e
        g.memset(ident_f[:], 1.0)
        g.affine_select(out=ident_f[:], in_=ident_f[:], pattern=[[-1, 128]], compare_op=ALU.is_equal,
                        fill=0.0, base=0, channel_multiplier=1)
        g.memset(ones_b[:], 1.0)
        g.memset(Utri[:], 1.0)
        g.affine_select(out=Utri[:], in_=Utri[:], pattern=[[1, 128]], compare_op=ALU.is_gt,
                        fill=0.0, base=0, channel_multiplier=-1)
        g.iota(DT[:, 0:128], pattern=[[1, 128]], base=128, channel_multiplier=-1,
               allow_small_or_imprecise_dtypes=True)
        g.iota(DT[:, 128:256], pattern=[[1, 128]], base=0, channel_multiplier=-1,
               allow_small_or_imprecise_dtypes=True)
        g.memset(NEG_A[:], 0.0)
        g.memset(NEG_B[:], 0.0)
        g.affine_select(out=NEG_A[:, 0:128], in_=NEG_A[:, 0:128], pattern=[[-1, 128]], compare_op=ALU.is_ge,
                        fill=NEGV, base=0, channel_multiplier=1)
        g.affine_select(out=NEG_A[:, 128:256], in_=NEG_A[:, 128:256], pattern=[[1, 128]], compare_op=ALU.is_ge,
                        fill=NEGV, base=0, channel_multiplier=-1)
        g.affine_select(out=NEG_B[:, 0:128], in_=NEG_B[:, 0:128], pattern=[[-1, 128]], compare_op=ALU.is_ge,
                        fill=NEGV, base=-1, channel_multiplier=1)
        t_pc = POOL.sig(g.affine_select(out=NEG_B[:, 128:256], in_=NEG_B[:, 128:256], pattern=[[1, 128]],
                                        compare_op=ALU.is_ge, fill=NEGV, base=0, channel_multiplier=-1))
        d_sk = mk_dsem("sk")
        bc_reg = POOL.e.to_reg(NE * CAP - 1)
        t_sk = d_sk.add(SP.e.dma_start(out=sk_t[:], in_=sinks.to_broadcast([128, 16])))
        DVE.wait(t_pc)
        t_idb = DVE.sig(DVE.e.tensor_copy(out=ident_b[:], in_=ident_f[:]))
        ACT.wait(t_sk)
        t_es = ACT.sig(ACT.e.activation(out=es_t[:], in_=sk_t[:], func=AF.Exp))
        t_const = [t_pc, t_idb, t_es]

        def rms_stats(col, src, junk, src_tok):
            ACT.wait(src_tok)
            t1 = ACT.sig(ACT.e.activation(out=junk, in_=src, func=AF.Square, accum_out=ss[:, col:col + 1]))
            ACT.wait(t1)
            t2 = ACT.sig(ACT.e.activation(out=sd[:, col:col + 1], in_=ss[:, col:col + 1], func=AF.Sqrt,
                                          scale=1.0 / D, bias=EPS))
            DVE.wait(t2)
            t3 = DVE.sig(DVE.e.reciprocal(out=rs[:, col:col + 1], in_=sd[:, col:col + 1]))
            return t3

        dbg = None
        if stage < 99:
            dbg = nc.dram_tensor("dbg", [S, D], F32, kind="ExternalOutput").ap()

        st_mixed = ExitStack()
        mixedT = sb("mixedT", [128, 16, S], BF16, st_mixed)
        st_aT = ExitStack()
        aT = sb("aT", [128, 16, S], BF16, st_aT)

        with ExitStack() as s1:
            xt = [sb("xt%d" % i, [128, D], F32, s1) for i in range(2)]
            at = [sb("at%d" % i, [128, D], BF16, s1) for i in range(2)]
            gm = sb("gm", [128, D], F32, s1)
            junk = sb("junk", [128, D], BF16, s1)
            psT = pst("psT1", [128, 2, 1024], BF16, s1)
            dx = [mk_dsem("x%d" % i) for i in range(2)]
            d_g = mk_dsem("gmix")
            t_g = d_g.add(SP.e.dma_start(out=gm[:], in_=g_mix.to_broadcast([128, D])))
            xt_free = [None, None]
            at_free = [None, None]
            psT_free = [None, None]
            p1 = {}

            def pre1(j):
                b = j % 2
                SP.wait(xt_free[b])
                t_x = dx[b].add(SP.e.dma_start(out=xt[b][:], in_=x[128 * j:128 * j + 128, :]))
                t_rs = rms_stats(j, xt[b][:], junk[:], t_x)
                DVE.wait(t_rs, t_x, t_g, at_free[b])
                t_a = DVE.sig(DVE.e.scalar_tensor_tensor(out=at[b][:], in0=xt[b][:], scalar=rs[:, j:j + 1],
                                                         in1=gm[:], op0=ALU.mult, op1=ALU.mult))
                xt_free[b] = t_a
                p1[j] = t_a

            def post1(j):
                b = j % 2
                PE.wait(p1[j], t_idb)
                t_tr = [None, None]
                for c in range(16):
                    if c % 8 == 0:
                        PE.wait(psT_free[c // 8])
                    ins = PE.e.transpose(out=psT[:, c // 8, (c % 8) * 128:(c % 8) * 128 + 128],
                                         in_=at[b][:, c * 128:c * 128 + 128], identity=ident_b[:])
                    if c % 8 == 7:
                        t_tr[c // 8] = PE.sig(ins)
                at_free[b] = t_tr[1]
                ACT.wait(t_tr[0])
                psT_free[0] = ACT.sig(ACT.e.activation(
                    out=aT[:, 0:8, 128 * j:128 * j + 128],
                    in_=psT[:, 0, :].rearrange("p (c t) -> p c t", c=8), func=AF.Copy))
                DVE.wait(t_tr[1])
                psT_free[1] = DVE.sig(DVE.e.tensor_copy(
                    out=aT[:, 8:16, 128 * j:128 * j + 128],
                    in_=psT[:, 1, :].rearrange("p (c t) -> p c t", c=8)))

            pre1(0)
            for j in range(NT):
                if j + 1 < NT:
                    pre1(j + 1)
                post1(j)
            t_aT = [psT_free[0], psT_free[1]]
            barrier()

        if stage == 1:
            with ExitStack() as sd_:
                tmp = sb("dbgtmp", [128, S], F32, sd_)
                dd = mk_dsem("dbg")
                for c in range(16):
                    DVE.wait(dd.tok())
                    t = DVE.sig(DVE.e.tensor_copy(out=tmp[:], in_=aT[:, c, :]))
                    SP.wait(t)
                    dd.add(SP.e.dma_start(out=dbg[128 * c:128 * c + 128, :], in_=tmp[:]))
                SP.wait(dd.tok())
                barrier()
            st_aT.close()
            st_mixed.close()
            return nc

        with ExitStack() as s2:
            slab = [sb("slab%d" % i, [128, 16, 128], BF16, s2) for i in range(4)]
            dW = [mk_dsem("slab%d" % i) for i in range(4)]
            QT = sb("QT", [128, S], BF16, s2)
            KT = sb("KT", [128, S], BF16, s2)
            VT = sb("VT", [128, S], BF16, s2)
            Vs = [sb("Vs%d" % i, [128, 16, 128], BF16, s2) for i in range(2)]
            ODacc = sb("ODacc", [128, 2, S], F32, s2)
            PT = [sb("PT%d" % i, [128, 512], BF16, s2) for i in range(2)]
            MB2 = [[sb("MB_%d_%d" % (i, r), [128, 512], BF16, s2) for r in range(3)] for i in range(2)]
            btmp = [sb("btmp%d" % i, [128, 2, 128], F32, s2) for i in range(2)]
            psP = pst("psP", [128, 2, 512], F32, s2)
            psT = pst("psT2", [128, 1, 1024], BF16, s2)
            psS = pst("psS", [128, 2, 512], F32, s2)
            psO = pst("psO", [128, 2, 512], F32, s2)

            st = dict(slab_n=0, slab_free=[None] * 4, psP_n=0, psP_free=[None, None],
                      psT_free=None, vs_n=0, vs_free=[None, None], mb_n=0, mb_free=[None, None])
            buf_tok = {}

            def mat_job(pieces, dst, scale, dst_free):
                k = st["slab_n"] % 4
                st["slab_n"] += 1
                POOL.wait(st["slab_free"][k])
                off = 0
                tok = None
                for (c0, n) in pieces:
                    tok = dW[k].add(POOL.e.dma_start(
                        out=slab[k][:, :, off:off + n],
                        in_=w_in[:, c0:c0 + n].rearrange("(c p) n -> p c n", p=128)))
                    off += n
                assert off == 128
                bg_issue(3)
                last = None
                for t4 in range(4):
                    bk = st["psP_n"] % 2
                    st["psP_n"] += 1
                    PE.wait(tok, st["psP_free"][bk], t_aT)
                    for kc in range(16):
                        ins = PE.e.matmul(psP[:, bk, :], lhsT=slab[k][:, kc, :], rhs=aT[:, kc, 512 * t4:512 * t4 + 512],
                                          start=(kc == 0), stop=(kc == 15))
                    t_mm = PE.sig(ins)
                    if bk == 0:
                        ACT.wait(t_mm, dst_free)
                        last = ACT.sig(ACT.e.activation(out=dst[:, 512 * t4:512 * t4 + 512], in_=psP[:, bk, :],
                                                        func=AF.Copy, scale=float(scale)))
                    else:
                        DVE.wait(t_mm, dst_free)
                        last = DVE.sig(DVE.e.tensor_scalar(out=dst[:, 512 * t4:512 * t4 + 512], in0=psP[:, bk, :],
                                                           scalar1=float(scale), scalar2=None, op0=ALU.mult))
                    st["psP_free"][bk] = last
                    if t4 == 2:
                        prev = last
                st["slab_free"][k] = t_mm
                return [prev, last]

            def v_layout(src, src_tok, s):
                vi = st["vs_n"] % 2
                st["vs_n"] += 1
                V = Vs[vi]
                toks = []
                for half in range(2):
                    PE.wait(src_tok, st["psT_free"], t_idb)
                    for i8 in range(8):
                        slot = half * 8 + i8
                        b_, rho = slot // s, slot % s
                        start = s * 128 * b_ + rho
                        ins = PE.e.transpose(out=psT[:, 0, i8 * 128:i8 * 128 + 128],
                                             in_=src[:, start:start + 127 * s + 1:s], identity=ident_b[:])
                    t_tr = PE.sig(ins)
                    buf_tok["vt_free"] = t_tr
                    E = ACT if half == 0 else DVE
                    E.wait(t_tr, st["vs_free"][vi])
                    if half == 0:
                        t_e = ACT.sig(ACT.e.activation(out=V[:, 0:8, :],
                                                       in_=psT[:, 0, :].rearrange("p (c t) -> p c t", c=8),
                                                       func=AF.Copy))
                    else:
                        t_e = DVE.sig(DVE.e.tensor_copy(out=V[:, 8:16, :],
                                                        in_=psT[:, 0, :].rearrange("p (c t) -> p c t", c=8)))
                    st["psT_free"] = t_e
                    toks.append(t_e)
                return vi, toks

            def make_mask(coefs, negt):
                par = st["mb_n"] % 2
                st["mb_n"] += 1
                DVE.wait(st["mb_free"][par], t_const)
                t = None
                for r, c in enumerate(coefs):
                    for u in range(2):
                        t = DVE.sig(DVE.e.scalar_tensor_tensor(out=MB2[par][r][:, 256 * u:256 * u + 256], in0=DT[:],
                                                               scalar=-float(c), in1=negt[:], op0=ALU.mult,
                                                               op1=ALU.add))
                return par, t

            batches = []

            def unit_cols(s, b_, rho):
                start = s * 128 * b_ + rho
                return slice(start, start + 127 * s + 1, s)

            for h in range(8):
                slope = 2.0 ** (-(h + 1))
                hstate = {}

                def pre_head(h=h, hstate=hstate, slope=slope):
                    qk_free = buf_tok.get("qk_free")
                    vt_free = buf_tok.get("vt_free")
                    hstate["q"] = mat_job([(h * 128, 128)], QT, 128.0 ** -0.5, qk_free)
                    hstate["k"] = mat_job([(1024 + h * 128, 128)], KT, 1.0, qk_free)
                    hstate["v"] = mat_job([(2048 + h * 128, 128)], VT, 1.0, vt_free)
                    hstate["mb"] = make_mask([slope * 1, slope * 4, slope * 16], NEG_A)

                for r, s in enumerate((1, 4, 16)):
                    units = [(b_, rho) for b_ in range(16 // s) for rho in range(s)]
                    bstate = {}

                    def pre_branch(s=s, hstate=hstate, bstate=bstate):
                        bstate["v"] = v_layout(VT, hstate["v"], s)

                    for ib in range(8):
                        us = units[2 * ib:2 * ib + 2]
                        pres = []
                        if r == 0 and ib == 0:
                            pres.append(pre_head)
                        if ib == 0:
                            pres.append(pre_branch)
                        batches.append(dict(kind="A", h=h, r=r, s=s, units=us, pres=pres, hstate=hstate,
                                            bstate=bstate, first=(r == 0), last_of_head=(r == 2 and ib == 7),
                                            last_of_branch=(ib == 7), rows=slice(0, 128)))

            kvstates = [{}, {}]
            for c in range(8):
                gkv = c // 4
                hstate = {}
                kvstate = kvstates[gkv]

                def pre_kv(gkv=gkv, kvstate=kvstate):
                    qk_free = buf_tok.get("qk_free")
                    vt_free = buf_tok.get("vt_free")
                    k0 = 4096 + gkv * 64
                    v0 = 4224 + gkv * 64
                    kvstate["k"] = mat_job([(k0, 64), (k0, 64)], KT, 1.0, qk_free)
                    kvstate["vT"] = mat_job([(v0, 64), (v0, 64)], VT, 1.0, vt_free)
                    kvstate["v"] = v_layout(VT, kvstate["vT"], 1)

                def pre_chunk(c=c, hstate=hstate, kvstate=kvstate):
                    qk_free = buf_tok.get("qk_free")
                    hstate["q"] = mat_job([(3072 + c * 128, 128)], QT, 64.0 ** -0.5, qk_free)
                    hstate["k"] = kvstate["k"]
                    sl = [2.0 ** (-8.0 * (2 * c + hh + 1) / 16.0) for hh in range(2)]
                    hstate["mb"] = make_mask(sl, NEG_B)

                for hh in range(2):
                    for ib in range(8):
                        us = [(2 * ib, 0), (2 * ib + 1, 0)]
                        pres = []
                        if hh == 0 and ib == 0:
                            if c % 4 == 0:
                                pres.append(pre_kv)
                            pres.append(pre_chunk)
                        batches.append(dict(kind="B", h=2 * c + hh, c=c, hh=hh, r=hh, s=1, units=us, pres=pres,
                                            hstate=hstate, bstate=kvstate, first=True, last_of_head=False,
                                            last_of_branch=(c % 4 == 3 and hh == 1 and ib == 7),
                                            last_q=(hh == 1 and ib == 7),
                                            rows=slice(64 * hh, 64 * hh + 64)))

            if stage == 2:
                batches = [bb for bb in batches if bb["kind"] == "A" and bb["h"] < 1]

            NB = len(batches)
            S_free = [None, None]
            PT_free = [None, None]
            O_free = [None, None]
            tS = [None] * NB
            tP = [None] * NB

            def emit_qk(n):
                bb = batches[n]
                for f in bb["pres"]:
                    f()
                pb = n % 2
                hs = bb["hstate"]
                rows = bb["rows"]
                par, t_mb = hs["mb"]
                PE.wait(S_free[pb], hs["q"], hs["k"], t_mb, t_idb)
                first = True
                for u, (b_, rho) in enumerate(bb["units"]):
                    qc = unit_cols(bb["s"], b_, rho)
                    if b_ > 0:
                        kc = unit_cols(bb["s"], b_ - 1, rho)
                        PE.e.matmul(psS[:, pb, 256 * u:256 * u + 128], lhsT=KT[rows, kc], rhs=QT[rows, qc],
                                    start=first, stop=False)
                        first = False
                    PE.e.matmul(psS[:, pb, 256 * u + 128:256 * u + 256], lhsT=KT[rows, qc], rhs=QT[rows, qc],
                                start=first, stop=False)
                    first = False
                ins = PE.e.matmul(psS[:, pb, :], lhsT=ident_b[:], rhs=MB2[par][bb["r"]][:], start=False, stop=True)
                tS[n] = PE.sig(ins)
                if bb["last_of_head"] or bb.get("last_q"):
                    buf_tok["qk_free"] = tS[n]
                if bb.get("last_q") or bb["last_of_head"]:
                    st["mb_free"][par] = tS[n]

            def emit_exp(n):
                pb = n % 2
                ACT.wait(tS[n], PT_free[pb])
                tP[n] = ACT.sig(ACT.e.activation(out=PT[pb][:], in_=psS[:, pb, :], func=AF.Exp))
                S_free[pb] = tP[n]

            def emit_pv(n):
                bb = batches[n]
                pb = n % 2
                vi, vtoks = bb["bstate"]["v"]
                V = Vs[vi]
                s = bb["s"]
                PE.wait(tP[n], O_free[pb], vtoks)
                first = True
                ins = None
                for u, (b_, rho) in enumerate(bb["units"]):
                    slot = b_ * s + rho
                    parts = []
                    if b_ > 0:
                        parts.append(((b_ - 1) * s + rho, PT[pb][:, 256 * u:256 * u + 128]))
                    parts.append((slot, PT[pb][:, 256 * u + 128:256 * u + 256]))
                    for i, (sl, pt) in enumerate(parts):
                        PE.e.matmul(psO[:, pb, 256 * u:256 * u + 128], lhsT=V[:, sl, :], rhs=pt,
                                    start=first, stop=False)
                        first = False
                    for i, (sl, pt) in enumerate(parts):
                        ins = PE.e.matmul(psO[:, pb, 256 * u + 128:256 * u + 256], lhsT=ones_b[:], rhs=pt,
                                          start=False, stop=(u == 1 and i == len(parts) - 1))
                t_o = PE.sig(ins)
                PT_free[pb] = t_o
                if bb["last_of_branch"]:
                    st["vs_free"][vi] = t_o
                return t_o

            def emit_evac(n, t_o):
                bb = batches[n]
                pb = n % 2
                s = bb["s"]
                (b0, r0), (b1, r1) = bb["units"]
                if bb["kind"] == "A":
                    if s == 1:
                        oap = ODacc[:, :, 128 * b0:128 * b0 + 256].rearrange("p o (u q) -> p o u q", u=2)
                    else:
                        lo = s * 128 * b0
                        oap = ODacc[:, :, lo:lo + 128 * s].rearrange("p o (q r) -> p o r q", r=s)[:, :, r0:r0 + 2, :]
                    iap = psO[:, pb, :].rearrange("p (u o q) -> p o u q", u=2, o=2)
                    DVE.wait(t_o, buf_tok.get("od_free"), buf_tok.get("od_last"))
                    if bb["first"]:
                        t_e = DVE.sig(DVE.e.tensor_copy(out=oap, in_=iap))
                    else:
                        t_e = DVE.sig(DVE.e.tensor_tensor(out=oap, in0=iap, in1=oap, op=ALU.add))
                    buf_tok["od_last"] = t_e
                    O_free[pb] = t_e
                    if bb["last_of_head"]:
                        h = bb["h"]
                        ACT.wait(t_e)
                        t0 = ACT.sig(ACT.e.activation(out=ODacc[:, 1, :], in_=ODacc[:, 1, :], func=AF.Ln))
                        ACT.wait(t0)
                        t1 = ACT.sig(ACT.e.activation(out=ODacc[:, 1, :], in_=ODacc[:, 1, :], func=AF.Exp, scale=-1.0))
                        DVE.wait(t1)
                        t2 = DVE.sig(DVE.e.tensor_tensor(out=mixedT[:, h, :], in0=ODacc[:, 0, :], in1=ODacc[:, 1, :],
                                                         op=ALU.mult))
                        buf_tok["od_free"] = t2
                        buf_tok["mixed_last"] = t2
                else:
                    rows = bb["rows"]
                    h = bb["h"]
                    c = bb["c"]
                    iv = psO[rows, pb, :].rearrange("p (u o q) -> p o u q", u=2, o=2)
                    tb = btmp[pb]
                    ACT.wait(t_o, t_es, buf_tok.get("btmp_free%d" % pb))
                    ta = ACT.sig(ACT.e.activation(out=tb[rows, :, :], in_=iv[:, 1, :, :], func=AF.Ln,
                                                  bias=es_t[rows, h:h + 1], scale=1.0))
                    ACT.wait(ta)
                    tb_ = ACT.sig(ACT.e.activation(out=tb[rows, :, :], in_=tb[rows, :, :], func=AF.Exp, scale=-1.0))
                    DVE.wait(tb_)
                    t_e = DVE.sig(DVE.e.tensor_tensor(
                        out=mixedT[rows, 8 + c, 128 * b0:128 * b0 + 256].rearrange("p (u q) -> p u q", u=2),
                        in0=iv[:, 0, :, :], in1=tb[rows, :, :], op=ALU.mult))
                    O_free[pb] = t_e
                    buf_tok["btmp_free%d" % pb] = t_e
                    buf_tok["mixed_last"] = t_e

            emit_qk(0)
            emit_exp(0)
            for n in range(NB):
                if n + 1 < NB:
                    emit_qk(n + 1)
                    emit_exp(n + 1)
                t_o = emit_pv(n)
                emit_evac(n, t_o)
            barrier()

        if stage in (2, 22):
            with ExitStack() as sd_:
                tmp = sb("dbgtmp", [128, S], F32, sd_)
                dd = mk_dsem("dbg")
                for c in range(16):
                    DVE.wait(dd.tok())
                    t = DVE.sig(DVE.e.tensor_copy(out=tmp[:], in_=mixedT[:, c, :]))
                    SP.wait(t)
                    dd.add(SP.e.dma_start(out=dbg[128 * c:128 * c + 128, :], in_=tmp[:]))
                SP.wait(dd.tok())
                barrier()
            st_aT.close()
            st_mixed.close()
            return nc

        st_aT.close()
        st_wout = ExitStack()
        wout = sb("wout", [128, 16, D], BF16, st_wout)
        with ExitStack() as s3:
            xt = [sb("xt3_%d" % i, [128, D], F32, s3) for i in range(2)]
            h1 = [sb("h1_%d" % i, [128, D], F32, s3) for i in range(2)]
            mf = sb("mf", [128, D], F32, s3)
            mhl = sb("mhl", [128, 2, D], BF16, s3)
            mhlT = sb("mhlT", [128, 2, 16, 128], BF16, s3)
            Whl = sb("Whl", [128, 2, 16, 36], BF16, s3)
            mb1 = sb("mb1", [128, D], BF16, s3)
            mb = [mb1, mb1]
            gmo = sb("gmo", [128, D], F32, s3)
            junk = mb1
            Wr = mf[:, 1024:1024 + 16 * 36].rearrange("p (c n) -> p c n", c=16)
            bias_bc = sb("bias_bc", [128, 36], F32, s3)
            psH = pst("psH", [128, 2, 2, 512], F32, s3)
            psF = pst("psF", [128, 2, 1024], BF16, s3)
            psZ = pst("psZ", [128, 512], F32, s3)
            d_w = mk_dsem("wout")
            dx = [mk_dsem("x3_%d" % i) for i in range(2)]
            dh = [mk_dsem("h1s%d" % i) for i in range(2)]
            dm1 = mk_dsem("ms")
            dm = [dm1, dm1]
            SP.wait(d_pre_wo.tok())
            for kc4 in range(4):
                t_w = d_w.add(SP.e.dma_start(out=wout[:, 4 * kc4:4 * kc4 + 4, :], in_=WO[:, 4 * kc4:4 * kc4 + 4, :]))
            t_c3 = d_c.add(SP.e.dma_start(out=gmo[:], in_=g_moe.to_broadcast([128, D])))
            t_c3 = d_c.add(SP.e.dma_start(out=Wr[:, :, 0:4], in_=w_grp.rearrange("(c p) n -> p c n", p=128)))
            t_c3 = d_c.add(SP.e.dma_start(out=Wr[:, :, 4:36], in_=w_exp.rearrange("(c p) n -> p c n", p=128)))
            t_c3 = d_c.add(SP.e.dma_start(out=bias_bc[:, 0:4], in_=b_grp.to_broadcast([128, 4])))
            t_c3 = d_c.add(SP.e.dma_start(out=bias_bc[:, 4:36], in_=b_exp.to_broadcast([128, 32])))

            DVE.wait(t_c3)
            t_ = DVE.sig(DVE.e.tensor_copy(out=Whl[:, 0, :, :], in_=Wr))
            DVE.wait(t_)
            Wtmp = mf[:, 0:16 * 36].rearrange("p (c n) -> p c n", c=16)
            t_ = DVE.sig(DVE.e.tensor_tensor(out=Wtmp, in0=Wr, in1=Whl[:, 0, :, :], op=ALU.subtract))
            DVE.wait(t_)
            t_whl = DVE.sig(DVE.e.tensor_copy(out=Whl[:, 1, :, :], in_=Wtmp))
            mf_free0 = t_whl
            psH_free = [None, None]
            psF_free = [None, None]
            psZ_free = None
            mhl_free = None
            xt_free = [None, None]
            h1_free = [None, None]
            mb_free = [None, None]
            mf_free = t_whl
            mfT_free = None
            t_hmm = {}

            def op_mm(j):
                b = j % 2
                bg_issue(4)
                SP.wait(xt_free[b])
                t_x = dx[b].add(SP.e.dma_start(out=xt[b][:], in_=x[128 * j:128 * j + 128, :]))
                mm = []
                for half in range(2):
                    PE.wait(psH_free[half], t_w)
                    for nn in range(2):
                        for kc in range(16):
                            ins = PE.e.matmul(psH[:, half, nn, :], lhsT=mixedT[:, kc, 128 * j:128 * j + 128],
                                              rhs=wout[:, kc, 1024 * half + 512 * nn:1024 * half + 512 * nn + 512],
                                              start=(kc == 0), stop=(kc == 15))
                    mm.append(PE.sig(ins))
                t_hmm[j] = dict(t_x=t_x, mm=mm)

            def op_add(j):
                b = j % 2
                a = t_hmm[j]
                toks = []
                for half in range(2):
                    DVE.wait(a["mm"][half], a["t_x"], h1_free[b])
                    t_h = DVE.sig(DVE.e.tensor_tensor(out=h1[b][:, 1024 * half:1024 * half + 1024],
                                                      in0=psH[:, half, :, :].rearrange("p n f -> p (n f)"),
                                                      in1=xt[b][:, 1024 * half:1024 * half + 1024], op=ALU.add))
                    psH_free[half] = t_h
                    toks.append(t_h)
                xt_free[b] = toks[1]
                a["toks"] = toks

            def rest_a(j):
                nonlocal mf_free, mfT_free, psZ_free
                b = j % 2
                toks = t_hmm[j]["toks"]
                SP.wait(toks)
                t_hs = dh[b].add(SP.e.dma_start(out=H1[128 * j:128 * j + 128, :], in_=h1[b][:]))
                ACT.wait(mb_free)
                t_rs = rms_stats(16 + j, h1[b][:], junk[:], toks)
                DVE.wait(toks, t_c3, mf_free)
                t_mf = DVE.sig(DVE.e.tensor_tensor(out=mf[:], in0=h1[b][:], in1=gmo[:], op=ALU.mult))
                ACT.wait(t_mf, t_rs, mb_free)
                t_m = ACT.sig(ACT.e.activation(out=mb[b][:], in_=mf[:], func=AF.Copy, scale=rs[:, 16 + j:17 + j]))
                SP.wait(t_m)
                mb_free[b] = dm[b].add(SP.e.dma_start(out=Mscr[128 * j:128 * j + 128, :], in_=mb[b][:]))
                DVE.wait(t_mf, mhl_free)
                t_hi = DVE.sig(DVE.e.tensor_copy(out=mhl[:, 0, :], in_=mf[:]))
                DVE.wait(t_hi)
                t_lo = DVE.sig(DVE.e.tensor_tensor(out=mhl[:, 1, :], in0=mf[:], in1=mhl[:, 0, :], op=ALU.subtract))
                t_hmm[j].update(t_hs=t_hs, t_rs=t_rs, t_mf=t_mf, t_m=t_m, t_lo=t_lo)

            def rest_b(j):
                nonlocal mf_free, mfT_free, psZ_free, mhl_free
                b = j % 2
                a = t_hmm[j]
                t_hs, t_rs, t_mf, t_m, t_lo = a["t_hs"], a["t_rs"], a["t_mf"], a["t_m"], a["t_lo"]
                evs = []
                for q4 in range(4):
                    fb = q4 % 2
                    hl, ch = q4 // 2, q4 % 2
                    PE.wait(t_lo, psF_free[fb], t_idb)
                    for i8 in range(8):
                        c = 8 * ch + i8
                        ins = PE.e.transpose(out=psF[:, fb, 128 * i8:128 * i8 + 128], in_=mhl[:, hl, 128 * c:128 * c + 128],
                                             identity=ident_b[:])
                    t_tr = PE.sig(ins)
                    E = ACT if fb == 0 else DVE
                    E.wait(t_tr, mfT_free)
                    if fb == 0:
                        t_e = ACT.sig(ACT.e.activation(out=mhlT[:, hl, 8 * ch:8 * ch + 8, :],
                                                       in_=psF[:, fb, :].rearrange("p (c t) -> p c t", c=8),
                                                       func=AF.Copy))
                    else:
                        t_e = DVE.sig(DVE.e.tensor_copy(out=mhlT[:, hl, 8 * ch:8 * ch + 8, :],
                                                        in_=psF[:, fb, :].rearrange("p (c t) -> p c t", c=8)))
                    psF_free[fb] = t_e
                    evs.append(t_e)
                mhl_free = t_tr
                mf_free = [t_lo, t_m]
                h1_free[b] = [t_hs, t_mf, t_rs]
                PE.wait(evs, psZ_free, t_whl)
                n_mm = 0
                for (xa, wa_) in ((0, 0), (1, 0), (0, 1)):
                    for c in range(16):
                        ins = PE.e.matmul(psZ[:, 0:36], lhsT=mhlT[:, xa, c, :], rhs=Whl[:, wa_, c, :],
                                          start=(n_mm == 0), stop=(n_mm == 47))
                        n_mm += 1
                t_z = PE.sig(ins)
                mfT_free = t_z
                DVE.wait(t_z, t_rs)
                psZ_free = DVE.sig(DVE.e.scalar_tensor_tensor(out=lg_all[:, j, :], in0=psZ[:, 0:36],
                                                              scalar=rs[:, 16 + j:17 + j], in1=bias_bc[:],
                                                              op0=ALU.mult, op1=ALU.add))

            op_mm(0)
            op_add(0)
            for j in range(NT):
                if j + 1 < NT:
                    op_mm(j + 1)
                rest_a(j)
                if j + 1 < NT:
                    op_add(j + 1)
                rest_b(j)
            barrier()
        st_wout.close()
        st_mixed.close()

        if stage == 3:
            with ExitStack() as sd_:
                tmp = sb("dbgtmp", [128, S], F32, sd_)
                dd = mk_dsem("dbg")
                for c in range(16):
                    SP.wait(dd.tok())
                    dd.add(SP.e.dma_start(out=tmp[:], in_=H1[128 * c:128 * c + 128, :]))
                    SP.wait(dd.tok())
                    dd.add(SP.e.dma_start(out=dbg[128 * c:128 * c + 128, :], in_=tmp[:]))
                SP.wait(dd.tok())
                barrier()
            return nc

        bg_issue(1000)
        st_wpg = ExitStack()
        wpg = sb("wpg", [128, 16, D], BF16, st_wpg)
        d_wpg = mk_dsem("wpg")
        with ExitStack() as s5:
            NMT = 4
            mt = [sb("mt%d" % i, [128, D], BF16, s5) for i in range(NMT)]
            wgu = [sb("wgu%d" % i, [128, 16, 512], BF16, s5) for i in range(2)]
            wdn = [sb("wdn%d" % i, [128, 2, D], BF16, s5) for i in range(2)]
            xs = [[sb("xs%d_%d" % (i, s_), [128, D], BF16, s5) for s_ in range(2)] for i in range(2)]
            XT = [sb("XT%d" % i, [128, 16, 256], BF16, s5) for i in range(2)]
            sg = sb("sg", [128, 2, 256], F32, s5)
            hidT = sb("hidT", [128, 2, 256], BF16, s5)
            yb = [sb("yb%d" % i, [128, D], BF16, s5) for i in range(2)]
            dmt = [mk_dsem("mt%d" % i) for i in range(NMT)]
            dsc = [mk_dsem("sc%d" % i) for i in range(NMT)]
            dwe = [mk_dsem("we%d" % i) for i in range(2)]
            dxs = [mk_dsem("xs%d" % i) for i in range(2)]
            dys = [mk_dsem("ys%d" % i) for i in range(2)]
            mt_tok = {}
            for j in range(NMT):
                mt_tok[j] = dmt[j].add(SP.e.dma_start(out=mt[j][:], in_=Mscr[128 * j:128 * j + 128, :]))

            with ExitStack() as s4:
                def t3(name, shape, dt=F32):
                    return sb(name, shape, dt, s4)
                gmax = t3("gmax", [128, NT]); goh = t3("goh", [128, NT, 4]); tmp4 = t3("tmp4", [128, NT, 4])
                sume = t3("sume", [128, NT]); grpw = t3("grpw", [128, NT]); pen4 = t3("pen4", [128, NT, 4])
                Lm = t3("Lm", [128, NT, 32]); Lm2 = t3("Lm2", [128, NT, 32])
                m1 = t3("m1", [128, NT]); m2 = t3("m2", [128, NT])
                oh = [t3("oh%d" % k, [128, NT, 32]) for k in range(2)]
                Mk = t3("Mk", [128, NT, 32], BF16)
                pos = t3("pos", [128, NT, 32]); tmp32 = t3("tmp32", [128, NT, 32])
                iota3 = t3("iota3", [128, NT, 32])
                pk = t3("pk", [128, NT]); ek = t3("ek", [128, NT]); dk = t3("dk", [128, NT]); ok = t3("ok", [128, NT])
                d21 = t3("d21", [128, NT]); e21 = t3("e21", [128, NT]); wa = t3("wa", [128, NT]); wb_ = t3("wb_", [128, NT])
                psR = pst("psR", [128, 512], F32, s4)
                t_io = POOL.sig(POOL.e.iota(iota3[:], pattern=[[0, NT], [1, 32]], base=0, channel_multiplier=0,
                                            allow_small_or_imprecise_dtypes=True))
                last = [None]

                def dv(inst_fn, extra=None):
                    DVE.wait(last[0], extra)
                    last[0] = DVE.sig(inst_fn())
                    return last[0]

                Lg = lg_all[:, :, 0:4]
                Le4 = lg_all[:, :, 4:36].rearrange("p j (g e) -> p j g e", g=4)
                V = DVE.e
                dv(lambda: V.tensor_reduce(out=gmax[:], in_=Lg, axis=AX.X, op=ALU.max))
                dv(lambda: V.tensor_tensor(out=goh[:], in0=Lg, in1=gmax[:].to_broadcast([128, NT, 4]), op=ALU.is_equal))
                t = dv(lambda: V.tensor_tensor(out=tmp4[:], in0=Lg, in1=gmax[:].to_broadcast([128, NT, 4]), op=ALU.subtract))
                ACT.wait(t)
                t = ACT.sig(ACT.e.activation(out=tmp4[:], in_=tmp4[:], func=AF.Exp))
                dv(lambda: V.tensor_reduce(out=sume[:], in_=tmp4[:], axis=AX.X, op=ALU.add), t)
                dv(lambda: V.reciprocal(out=grpw[:], in_=sume[:]))
                dv(lambda: V.tensor_scalar(out=pen4[:], in0=goh[:], scalar1=BIGV, scalar2=-BIGV, op0=ALU.mult, op1=ALU.add))
                dv(lambda: V.tensor_tensor(out=Lm[:].rearrange("p j (g e) -> p j g e", g=4), in0=Le4,
                                           in1=pen4[:].to_broadcast([128, NT, 4, 8]), op=ALU.add))
                dv(lambda: V.tensor_reduce(out=m1[:], in_=Lm[:], axis=AX.X, op=ALU.max))
                dv(lambda: V.tensor_tensor(out=oh[0][:], in0=Lm[:], in1=m1[:].to_broadcast([128, NT, 32]), op=ALU.is_equal))
                dv(lambda: V.scalar_tensor_tensor(out=Lm2[:], in0=oh[0][:], scalar=-BIGV, in1=Lm[:], op0=ALU.mult, op1=ALU.add))
                dv(lambda: V.tensor_reduce(out=m2[:], in_=Lm2[:], axis=AX.X, op=ALU.max))
                dv(lambda: V.tensor_tensor(out=oh[1][:], in0=Lm2[:], in1=m2[:].to_broadcast([128, NT, 32]), op=ALU.is_equal))
                t_mk = dv(lambda: V.tensor_tensor(out=Mk[:], in0=oh[0][:], in1=oh[1][:], op=ALU.add))
                t = dv(lambda: V.tensor_tensor(out=d21[:], in0=m2[:], in1=m1[:], op=ALU.subtract))
                ACT.wait(t)
                t = ACT.sig(ACT.e.activation(out=e21[:], in_=d21[:], func=AF.Exp))
                dv(lambda: V.tensor_scalar(out=wa[:], in0=e21[:], scalar1=1.0, scalar2=None, op0=ALU.add), t)
                dv(lambda: V.reciprocal(out=wa[:], in_=wa[:]))
                dv(lambda: V.tensor_tensor(out=wb_[:], in0=wa[:], in1=e21[:], op=ALU.mult))
                dv(lambda: V.tensor_tensor(out=wa[:], in0=wa[:], in1=grpw[:], op=ALU.mult))
                dv(lambda: V.tensor_tensor(out=wb_[:], in0=wb_[:], in1=grpw[:], op=ALU.mult))
                PE.wait(t_mk, t_pc)
                for j in range(NT):
                    PE.e.matmul(psR[:, 32 * j:32 * j + 32], lhsT=Utri[:], rhs=Mk[:, j, :], start=(j == 0), stop=False)
                for j in range(NT - 1):
                    for j2 in range(j + 1, NT):
                        ins = PE.e.matmul(psR[:, 32 * j2:32 * j2 + 32], lhsT=ones_b[:], rhs=Mk[:, j, :], start=False,
                                          stop=(j == NT - 2))
                t_pos = PE.sig(ins)
                dv(lambda: V.tensor_copy(out=pos[:].rearrange("p j e -> p (j e)"), in_=psR[:]), t_pos)
                for k in range(2):
                    wk = wa if k == 0 else wb_
                    dv(lambda: V.tensor_tensor(out=tmp32[:], in0=oh[k][:], in1=pos[:], op=ALU.mult))
                    dv(lambda: V.tensor_reduce(out=pk[:], in_=tmp32[:], axis=AX.X, op=ALU.add))
                    dv(lambda: V.tensor_tensor(out=tmp32[:], in0=oh[k][:], in1=iota3[:], op=ALU.mult), t_io)
                    dv(lambda: V.tensor_reduce(out=ek[:], in_=tmp32[:], axis=AX.X, op=ALU.add))
                    dv(lambda: V.scalar_tensor_tensor(out=dk[:], in0=ek[:], scalar=float(CAP), in1=pk[:], op0=ALU.mult, op1=ALU.add))
                    dv(lambda: V.tensor_scalar(out=ok[:], in0=pk[:], scalar1=float(CAP), scalar2=1.0e6, op0=ALU.is_ge, op1=ALU.mult))
                    dv(lambda: V.tensor_tensor(out=dk[:], in0=dk[:], in1=ok[:], op=ALU.add))
                    dv(lambda: V.tensor_copy(out=dst_i[:, k, :], in_=dk[:]))
                    dv(lambda: V.tensor_scalar(out=ok[:], in0=pk[:], scalar1=float(CAP), scalar2=None, op0=ALU.is_lt))
                    dv(lambda: V.tensor_tensor(out=wts[:, k, :], in0=wk[:], in1=ok[:], op=ALU.mult))
                t_route = last[0]


            s5p = ExitStack()
            psT = pst("psT5", [128, 2, 1024], BF16, s5p)
            psG = pst("psG", [128, 2, 2, 256], F32, s5p)
            psY = pst("psY", [128, 2, 2, 512], F32, s5p)

            ACT.wait(d_pre_pg.tok())
            for kc4 in range(4):
                t_wpg = d_wpg.add(ACT.e.dma_start(out=wpg[:, 4 * kc4:4 * kc4 + 4, :], in_=WPG[:, 4 * kc4:4 * kc4 + 4, :]))
            sc_tok = [None] * NMT
            for j in range(NT):
                b = j % NMT
                if j >= NMT:
                    SP.wait(sc_tok[b])
                    mt_tok[j] = dmt[b].add(SP.e.dma_start(out=mt[b][:], in_=Mscr[128 * j:128 * j + 128, :]))
                POOL.wait(mt_tok[j], t_route)
                for k in range(2):
                    sc_tok[b] = dsc[b].add(POOL.e.indirect_dma_start(
                        out=Xg, out_offset=bass.IndirectOffsetOnAxis(ap=dst_i[:, k, j:j + 1], axis=0),
                        in_=mt[b][:], in_offset=None, bounds_check=bc_reg, oob_is_err=False))
            t_scatter = list(sc_tok)

            def load_w(e):
                b = e % 2
                ACT.wait(w_free[b], d_pre_ex[e // 4].tok())
                dwe[b].add(ACT.e.dma_start(out=wgu[b][:].rearrange("p c f -> p (c f)"), in_=WGU[e]))
                return dwe[b].add(ACT.e.dma_start(out=wdn[b][:].rearrange("p c d -> p (c d)"), in_=WDN[e]))

            def load_x(e):
                b = e % 2
                SP.wait(t_scatter, xs_free[b])
                for s_ in range(2):
                    t = dxs[b].add(SP.e.dma_start(out=xs[b][s_][:], in_=Xg[e * CAP + 128 * s_:e * CAP + 128 * s_ + 128, :]))
                return t

            w_free = [None, None]
            xs_free = [None, None]
            psT_free = [None, None]
            psG_free = [None, None]
            psY_free = [None, None]
            yb_free = [None, None]
            XT_free = [None, None]
            hid_free = None
            sg_free = None
            xt_evs = {}
            t_w = {0: load_w(0)}
            t_x = {0: load_x(0)}
            if NE > 1:
                t_x[1] = load_x(1)

            def tr_round(e, r):
                b = e % 2
                s_, ch = r // 2, r % 2
                tb = r % 2
                PE.wait(t_x[e], psT_free[tb], t_idb)
                for i8 in range(8):
                    c = 8 * ch + i8
                    ins = PE.e.transpose(out=psT[:, tb, 128 * i8:128 * i8 + 128],
                                         in_=xs[b][s_][:, 128 * c:128 * c + 128], identity=ident_b[:])
                t_tr = PE.sig(ins)
                E = ACT if tb == 0 else DVE
                E.wait(t_tr, XT_free[b])
                if tb == 0:
                    t_e = ACT.sig(ACT.e.activation(out=XT[b][:, 8 * ch:8 * ch + 8, 128 * s_:128 * s_ + 128],
                                                   in_=psT[:, tb, :].rearrange("p (c t) -> p c t", c=8), func=AF.Copy))
                else:
                    t_e = DVE.sig(DVE.e.tensor_copy(out=XT[b][:, 8 * ch:8 * ch + 8, 128 * s_:128 * s_ + 128],
                                                    in_=psT[:, tb, :].rearrange("p (c t) -> p c t", c=8)))
                psT_free[tb] = t_e
                xt_evs.setdefault(e, []).append(t_e)
                if r == 3:
                    xs_free[b] = t_tr

            for r in range(4):
                tr_round(0, r)
            for e in range(NE):
                b = e % 2
                if e + 1 < NE:
                    t_w[e + 1] = load_w(e + 1)
                if e + 2 < NE:
                    t_x[e + 2] = load_x(e + 2)
                hts = []
                for fc in range(2):
                    PE.wait(xt_evs[e], t_w[e], psG_free[fc])
                    for gu in range(2):
                        for kc in range(16):
                            ins = PE.e.matmul(psG[:, fc, gu, :], lhsT=wgu[b][:, kc, 256 * gu + 128 * fc:256 * gu + 128 * fc + 128],
                                              rhs=XT[b][:, kc, :], start=(kc == 0), stop=(kc == 15))
                        if gu == 1:
                            t_g = PE.sig(ins)
                        if e + 1 < NE:
                            tr_round(e + 1, 2 * fc + gu)
                    ACT.wait(t_g, sg_free)
                    t_s = ACT.sig(ACT.e.activation(out=sg[:, fc, :], in_=psG[:, fc, 0, :], func=AF.Silu))
                    DVE.wait(t_s, hid_free)
                    t_h = DVE.sig(DVE.e.tensor_tensor(out=hidT[:, fc, :], in0=sg[:, fc, :], in1=psG[:, fc, 1, :], op=ALU.mult))
                    psG_free[fc] = t_h
                    hts.append(t_h)
                XT_free[b] = t_g
                sg_free = hts[1]
                ev_s = []
                for u in range(4):
                    s_, half = u // 2, u % 2
                    yi = u % 2
                    PE.wait(hts, psY_free[yi])
                    for nn in range(2):
                        for fc in range(2):
                            ins = PE.e.matmul(psY[:, yi, nn, :], lhsT=hidT[:, fc, 128 * s_:128 * s_ + 128],
                                              rhs=wdn[b][:, fc, 1024 * half + 512 * nn:1024 * half + 512 * nn + 512],
                                              start=(fc == 0), stop=(fc == 1))
                    t_y = PE.sig(ins)
                    if yi == 0:
                        ACT.wait(t_y, yb_free[s_])
                        t_c = ACT.sig(ACT.e.activation(out=yb[s_][:, 1024 * half:1024 * half + 1024],
                                                       in_=psY[:, yi, :, :].rearrange("p n f -> p (n f)"), func=AF.Copy))
                    else:
                        DVE.wait(t_y, yb_free[s_])
                        t_c = DVE.sig(DVE.e.tensor_copy(out=yb[s_][:, 1024 * half:1024 * half + 1024],
                                                        in_=psY[:, yi, :, :].rearrange("p n f -> p (n f)")))
                    psY_free[yi] = t_c
                    ev_s.append(t_c)
                    if half == 1:
                        SP.wait(ev_s)
                        ev_s = []
                        yb_free[s_] = dys[s_].add(SP.e.dma_start(out=Y[e * CAP + 128 * s_:e * CAP + 128 * s_ + 128, :], in_=yb[s_][:]))
                hid_free = t_y
                w_free[b] = t_y
            barrier()
            s5p.close()

        with ExitStack() as s6:
            h2 = [sb("h2_%d" % i, [128, D], F32, s6) for i in range(3)]
            y1 = [sb("y1_%d" % i, [128, D], BF16, s6) for i in range(2)]
            y2 = [sb("y2_%d" % i, [128, D], BF16, s6) for i in range(2)]
            nb = [sb("nb%d" % i, [128, D], BF16, s6) for i in range(2)]
            nT = [sb("nT%d" % i, [128, 16, 128], BF16, s6) for i in range(2)]
            ptl = [sb("ptl%d" % i, [128, 256], F32, s6) for i in range(2)]
            pbf = [sb("pbf%d" % i, [128, 256], BF16, s6) for i in range(2)]
            pT = [sb("pT%d" % i, [128, 2, 128], BF16, s6) for i in range(2)]
            wple = sb("wple", [128, 2, D], BF16, s6)
            gpl = sb("gpl", [128, D], F32, s6)
            gfn = sb("gfn", [128, D], F32, s6)
            sgm = [sb("sgm%d" % i, [128, 512], F32, s6) for i in range(2)]
            ot = [sb("ot%d" % i, [128, D], F32, s6) for i in range(2)]
            junk = sb("junk6", [128, D], BF16, s6)
            psT = pst("psT6", [128, 2, 1024], BF16, s6)
            psGt = pst("psGt", [128, 2, 512], F32, s6)
            psPe = pst("psPe", [128, 2, 512], F32, s6)
            d_c6 = mk_dsem("c6")
            dh2 = [mk_dsem("h2l%d" % i) for i in range(3)]
            dy = [mk_dsem("yg%d" % i) for i in range(2)]
            dp = [mk_dsem("pl%d" % i) for i in range(2)]
            dout = [mk_dsem("out%d" % i) for i in range(2)]
            t_c6 = d_c6.add(POOL.e.dma_start(out=wple[:], in_=w_ple.rearrange("(c p) d -> p c d", p=128)))
            t_c6 = d_c6.add(SP.e.dma_start(out=gpl[:], in_=g_ple.to_broadcast([128, D])))
            t_c6 = d_c6.add(SP.e.dma_start(out=gfn[:], in_=g_final.to_broadcast([128, D])))
            h2_free = [None, None, None]
            y_free = [None, None]
            nb_free = [None, None]
            nT_free = [None, None]
            p_free = [None, None]
            pbf_free = [None, None]
            pT_free = [None, None]
            psT_free = [None, None]
            psQ_free = [None, None]
            sgm_free = [None, None]
            ot_free = [None, None]
            stA = {}

            def A_pre(j):
                b = j % 2
                SP.wait(h2_free[j % 3])
                t_h = dh2[j % 3].add(SP.e.dma_start(out=h2[j % 3][:], in_=H1[128 * j:128 * j + 128, :]))
                SP.wait(p_free[b])
                t_p = dp[b].add(SP.e.dma_start(out=ptl[b][:], in_=p_in[128 * j:128 * j + 128, :]))
                POOL.wait(y_free[b])
                dy[b].add(POOL.e.indirect_dma_start(out=y1[b][:], out_offset=None, in_=Y,
                                                    in_offset=bass.IndirectOffsetOnAxis(ap=dst_i[:, 0, j:j + 1], axis=0),
                                                    bounds_check=bc_reg, oob_is_err=False))
                t_y = dy[b].add(POOL.e.indirect_dma_start(out=y2[b][:], out_offset=None, in_=Y,
                                                          in_offset=bass.IndirectOffsetOnAxis(ap=dst_i[:, 1, j:j + 1], axis=0),
                                                          bounds_check=bc_reg, oob_is_err=False))
                DVE.wait(t_h, t_y)
                t1 = DVE.sig(DVE.e.scalar_tensor_tensor(out=h2[j % 3][:], in0=y1[b][:], scalar=wts[:, 0, j:j + 1], in1=h2[j % 3][:],
                                                        op0=ALU.mult, op1=ALU.add))
                DVE.wait(t1)
                t2 = DVE.sig(DVE.e.scalar_tensor_tensor(out=h2[j % 3][:], in0=y2[b][:], scalar=wts[:, 1, j:j + 1], in1=h2[j % 3][:],
                                                        op0=ALU.mult, op1=ALU.add))
                y_free[b] = t2
                t_rs = rms_stats(32 + j, h2[j % 3][:], junk[:], t2)
                DVE.wait(t_rs, t_c6, nb_free[b])
                t_n = DVE.sig(DVE.e.scalar_tensor_tensor(out=nb[b][:], in0=h2[j % 3][:], scalar=rs[:, 32 + j:33 + j], in1=gpl[:],
                                                         op0=ALU.mult, op1=ALU.mult))
                ACT.wait(t_p, pbf_free[b])
                t_pb = ACT.sig(ACT.e.activation(out=pbf[b][:], in_=ptl[b][:], func=AF.Copy))
                p_free[b] = t_pb
                stA[j] = dict(t_n=t_n, t_pb=t_pb)

            def A_tr(j):
                b = j % 2
                a = stA[j]
                evs = []
                for ch in range(2):
                    PE.wait(a["t_n"], psT_free[ch])
                    for i8 in range(8):
                        c = 8 * ch + i8
                        ins = PE.e.transpose(out=psT[:, ch, 128 * i8:128 * i8 + 128], in_=nb[b][:, 128 * c:128 * c + 128],
                                             identity=ident_b[:])
                    if ch == 0:
                        t_tr = PE.sig(ins)
                        ACT.wait(t_tr, nT_free[b])
                        t_e = ACT.sig(ACT.e.activation(out=nT[b][:, 0:8, :],
                                                       in_=psT[:, 0, :].rearrange("p (c t) -> p c t", c=8), func=AF.Copy))
                        psT_free[0] = t_e
                        evs.append(t_e)
                    else:
                        t_tr = PE.sig(ins)
                        ACT.wait(t_tr, nT_free[b])
                        t_e = ACT.sig(ACT.e.activation(out=nT[b][:, 8:16, :],
                                                       in_=psT[:, 1, :].rearrange("p (c t) -> p c t", c=8), func=AF.Copy))
                        psT_free[1] = t_e
                        evs.append(t_e)
                nb_free[b] = t_tr
                PE.wait(a["t_pb"], psT_free[0])
                for c in range(2):
                    ins = PE.e.transpose(out=psT[:, 0, 128 * c:128 * c + 128], in_=pbf[b][:, 128 * c:128 * c + 128],
                                         identity=ident_b[:])
                t_tr = PE.sig(ins)
                pbf_free[b] = t_tr
                ACT.wait(t_tr, pT_free[b])
                t_e = ACT.sig(ACT.e.activation(out=pT[b][:], in_=psT[:, 0, 0:256].rearrange("p (c t) -> p c t", c=2),
                                               func=AF.Copy))
                psT_free[0] = t_e
                evs.append(t_e)
                a["evs"] = evs

            def B_mm_q(j, q):
                b = j % 2
                a = stA[j]
                qb = q % 2
                PE.wait(a["evs"], t_wpg, t_c6, psQ_free[qb])
                for kc in range(16):
                    PE.e.matmul(psGt[:, qb, :], lhsT=nT[b][:, kc, :], rhs=wpg[:, kc, 512 * q:512 * q + 512],
                                start=(kc == 0), stop=(kc == 15))
                for kc in range(2):
                    ins = PE.e.matmul(psPe[:, qb, :], lhsT=pT[b][:, kc, :], rhs=wple[:, kc, 512 * q:512 * q + 512],
                                      start=(kc == 0), stop=(kc == 1))
                t_mm = PE.sig(ins)
                ACT.wait(t_mm, sgm_free[qb])
                t_s = ACT.sig(ACT.e.activation(out=sgm[qb][:], in_=psGt[:, qb, :], func=AF.Sigmoid))
                DVE.wait(t_s)
                t_a = DVE.sig(DVE.e.tensor_tensor(out=sgm[qb][:], in0=sgm[qb][:], in1=psPe[:, qb, :], op=ALU.mult))
                psQ_free[qb] = t_a
                DVE.wait(t_a)
                tq = DVE.sig(DVE.e.tensor_tensor(out=h2[j % 3][:, 512 * q:512 * q + 512],
                                                 in0=h2[j % 3][:, 512 * q:512 * q + 512], in1=sgm[qb][:], op=ALU.add))
                sgm_free[qb] = tq
                a["tq"] = tq
                if q == 3:
                    nT_free[b] = t_mm
                    pT_free[b] = t_mm

            def B_fin(j):
                b = j % 2
                a = stA[j]
                t_rs = rms_stats(48 + j, h2[j % 3][:], junk[:], a["tq"])
                DVE.wait(t_rs, ot_free[b])
                t_o = DVE.sig(DVE.e.scalar_tensor_tensor(out=ot[b][:], in0=h2[j % 3][:], scalar=rs[:, 48 + j:49 + j], in1=gfn[:],
                                                         op0=ALU.mult, op1=ALU.mult))
                h2_free[j % 3] = t_o
                ACT.wait(t_o)
                ot_free[b] = dout[b].add(ACT.e.dma_start(out=out[128 * j:128 * j + 128, :], in_=ot[b][:]))

            A_pre(0)
            A_tr(0)
            for j in range(NT):
                B_mm_q(j, 0)
                B_mm_q(j, 1)
                if j + 1 < NT:
                    A_pre(j + 1)
                B_mm_q(j, 2)
                B_mm_q(j, 3)
                if j + 1 < NT:
                    A_tr(j + 1)
                B_fin(j)
            barrier()
        st_wpg.close()
    return nc


_IN_NAMES = ["x", "p", "w_in", "w_out", "sinks", "g_mix", "g_moe", "g_ple", "g_final", "w_grp", "b_grp",
             "w_exp", "b_exp", "w_gate", "w_up", "w_down", "w_ple", "w_ple_gate"]


def make_in_maps(inputs, n_cores=8):
    f = lambda a: np.ascontiguousarray(np.asarray(a, dtype=np.float32))
    shared = {
        "w_in": f(inputs["w_in"][0]), "w_out": f(inputs["w_out"][0]),
        "sinks": f(inputs["sinks"]).reshape(1, 16),
        "g_mix": f(inputs["g_mix"]).reshape(1, D), "g_moe": f(inputs["g_moe"]).reshape(1, D),
        "g_ple": f(inputs["g_ple"]).reshape(1, D), "g_final": f(inputs["g_final"]).reshape(1, D),
        "w_grp": f(inputs["w_grp"][0]), "b_grp": f(inputs["b_grp"]).reshape(1, 4),
        "w_exp": f(inputs["w_exp"][0]).reshape(D, 32), "b_exp": f(inputs["b_exp"]).reshape(1, 32),
        "w_gate": f(inputs["w_gate"][0]), "w_up": f(inputs["w_up"][0]), "w_down": f(inputs["w_down"][0]),
        "w_ple": f(inputs["w_ple"][0]), "w_ple_gate": f(inputs["w_ple_gate"][0]),
    }
    xs = f(inputs["x"])
    ps = f(inputs["p"])
    maps = []
    for c in range(n_cores):
        m = dict(shared)
        m["x"] = xs[c]
        m["p"] = ps[0, c]
        maps.append(m)
    return maps


def kernel(**inputs):
    nc = build()
    in_maps = make_in_maps(inputs)
    res = run_bass_kernel_spmd(nc, in_maps, core_ids=list(range(8)))
    return np.stack([r["out"] for r in res.results], axis=0).astype(np.float32)
```

```python
import numpy as np
from contextlib import ExitStack
import concourse.bass as bass
import concourse.mybir as mybir
from concourse.bass_utils import run_bass_kernel_spmd

F32 = mybir.dt.float32
BF16 = mybir.dt.bfloat16
I32 = mybir.dt.int32
AF = mybir.ActivationFunctionType
ALU = mybir.AluOpType
AX = mybir.AxisListType

S = 2048
D = 2048
NT = 16
NE = 32
CAP = 256
NEGV = -30000.0
BIGV = 1.0e4
EPS = 1e-6


class Eng:
    def __init__(self, nc, es, eng, name):
        self.e = eng
        self.sem = es.enter_context(nc.semaphore("sem_" + name))
        self.cnt = 0
        self.seen = {}

    def wait(self, *toks):
        for t in toks:
            if t is None:
                continue
            if isinstance(t, list):
                self.wait(*t)
                continue
            sem, val = t
            k = id(sem)
            if self.seen.get(k, 0) >= val:
                continue
            self.e.wait_ge(sem, val)
            self.seen[k] = val

    def sig(self, inst):
        self.cnt += 1
        inst.then_inc(self.sem, 1)
        return (self.sem, self.cnt)


class DSem:
    def __init__(self, nc, es, name, reg):
        self.sem = es.enter_context(nc.semaphore("dma_" + name))
        self.cnt = 0
        reg.append(self)

    def add(self, inst):
        self.cnt += 16
        inst.then_inc(self.sem, 16)
        return (self.sem, self.cnt)

    def tok(self):
        return (self.sem, self.cnt) if self.cnt else None


def build(stage=99):
    nc = bass.Bass("TRN2", target_bir_lowering=False)

    def din(n, s, d=F32):
        return nc.dram_tensor(n, s, d, kind="ExternalInput").ap()

    x = din("x", [S, D])
    p_in = din("p", [S, 256])
    w_in = din("w_in", [D, 4352])
    w_out = din("w_out", [D, D])
    sinks = din("sinks", [1, 16])
    g_mix = din("g_mix", [1, D])
    g_moe = din("g_moe", [1, D])
    g_ple = din("g_ple", [1, D])
    g_final = din("g_final", [1, D])
    w_grp = din("w_grp", [D, 4])
    b_grp = din("b_grp", [1, 4])
    w_exp = din("w_exp", [D, 32])
    b_exp = din("b_exp", [1, 32])
    w_gate = din("w_gate", [NE, D, 256])
    w_up = din("w_up", [NE, D, 256])
    w_down = din("w_down", [NE, 256, D])
    w_ple = din("w_ple", [256, D])
    w_pg = din("w_ple_gate", [D, D])
    out = nc.dram_tensor("out", [S, D], F32, kind="ExternalOutput").ap()
    H1 = nc.dram_tensor("H1", [S, D], F32, kind="Internal").ap()
    Mscr = nc.dram_tensor("Mscr", [S, D], BF16, kind="Internal").ap()
    Xg = nc.dram_tensor("Xg", [NE * CAP, D], BF16, kind="Internal").ap()
    Y = nc.dram_tensor("Y", [NE * CAP, D], BF16, kind="Internal").ap()
    WGU = nc.dram_tensor("WGU", [NE, 128, 16 * 512], BF16, kind="Internal").ap()
    WDN = nc.dram_tensor("WDN", [NE, 128, 2 * D], BF16, kind="Internal").ap()
    WO = nc.dram_tensor("WO", [128, 16, D], BF16, kind="Internal").ap()
    WPG = nc.dram_tensor("WPG", [128, 16, D], BF16, kind="Internal").ap()

    es = ExitStack()
    with es:
        PE = Eng(nc, es, nc.tensor, "pe")
        ACT = Eng(nc, es, nc.scalar, "act")
        DVE = Eng(nc, es, nc.vector, "dve")
        POOL = Eng(nc, es, nc.gpsimd, "pool")
        SP = Eng(nc, es, nc.sync, "sp")
        ENGS = [PE, ACT, DVE, POOL, SP]
        dsems = []
        bar_sem = es.enter_context(nc.semaphore("bar"))
        bar_cnt = [0]

        def mk_dsem(name):
            return DSem(nc, es, name, dsems)

        def barrier():
            for ds in dsems:
                SP.wait(ds.tok())
            bar_cnt[0] += len(ENGS)
            for E in ENGS:
                E.e.drain().then_inc(bar_sem, 1)
            for E in ENGS:
                E.e.wait_ge(bar_sem, bar_cnt[0])

        d_pre_wo = DSem(nc, es, "pre_wo", [])
        d_pre_ex = [DSem(nc, es, "pre_ex%d" % g_, []) for g_ in range(8)]
        d_pre_pg = DSem(nc, es, "pre_pg", [])
        bg_list = []
        for kc in range(16):
            bg_list.append((d_pre_wo, WO[:, kc, :], w_out[128 * kc:128 * kc + 128, :]))
        for kc in range(16):
            bg_list.append((d_pre_pg, WPG[:, kc, :], w_pg[128 * kc:128 * kc + 128, :]))
        for e in range(NE):
            wv = WGU[e].rearrange("p (c f) -> p c f", c=16)
            bg_list.append((d_pre_ex[e // 4], wv[:, :, 0:256], w_gate[e].rearrange("(c p) f -> p c f", p=128)))
            bg_list.append((d_pre_ex[e // 4], wv[:, :, 256:512], w_up[e].rearrange("(c p) f -> p c f", p=128)))
            bg_list.append((d_pre_ex[e // 4], WDN[e].rearrange("p (c d) -> p c d", c=2),
                            w_down[e].rearrange("(c p) d -> p c d", p=128)))
        bg_pos = [0]

        def bg_issue(n):
            for _ in range(n):
                if bg_pos[0] >= len(bg_list):
                    return
                ds, o, i = bg_list[bg_pos[0]]
                bg_pos[0] += 1
                ds.add(POOL.e.dma_start(out=o, in_=i))

        def sb(name, shape, dt, stack=es):
            return stack.enter_context(nc.sbuf_tensor(name, shape, dt))

        def pst(name, shape, dt, stack):
            return stack.enter_context(nc.psum_tensor(name, shape, dt))

        ident_f = sb("ident_f", [128, 128], F32)
        ident_b = sb("ident_b", [128, 128], BF16)
        ones_b = sb("ones_b", [128, 128], BF16)
        Utri = sb("Utri", [128, 128], BF16)
        DT = sb("DT", [128, 256], F32)
        NEG_A = sb("NEG_A", [128, 256], F32)
        NEG_B = sb("NEG_B", [128, 256], F32)
        es_t = sb("es_t", [128, 16], F32)
        sk_t = sb("sk_t", [128, 16], F32)
        ss = sb("ss", [128, 64], F32)
        sd = sb("sd", [128, 64], F32)
        rs = sb("rs", [128, 64], F32)
        d_c = mk_dsem("const")
        lg_all = sb("lg_all", [128, NT, 36], F32)
        dst_i = sb("dst_i", [128, 2, NT], I32)
        wts = sb("wts", [128, 2, NT], F32)

        g = POOL.e
        g.memset(ident_f[:], 1.0)
        g.affine_select(out=ident_f[:], in_=ident_f[:], pattern=[[-1, 128]], compare_op=ALU.is_equal,
                        fill=0.0, base=0, channel_multiplier=1)
        g.memset(ones_b[:], 1.0)
        g.memset(Utri[:], 1.0)
        g.affine_select(out=Utri[:], in_=Utri[:], pattern=[[1, 128]], compare_op=ALU.is_gt,
                        fill=0.0, base=0, channel_multiplier=-1)
        g.iota(DT[:, 0:128], pattern=[[1, 128]], base=128, channel_multiplier=-1,
               allow_small_or_imprecise_dtypes=True)
        g.iota(DT[:, 128:256], pattern=[[1, 128]], base=0, channel_multiplier=-1,
               allow_small_or_imprecise_dtypes=True)
        g.memset(NEG_A[:], 0.0)
        g.memset(NEG_B[:], 0.0)
        g.affine_select(out=NEG_A[:, 0:128], in_=NEG_A[:, 0:128], pattern=[[-1, 128]], compare_op=ALU.is_ge,
                        fill=NEGV, base=0, channel_multiplier=1)
        g.affine_select(out=NEG_A[:, 128:256], in_=NEG_A[:, 128:256], pattern=[[1, 128]], compare_op=ALU.is_ge,
                        fill=NEGV, base=0, channel_multiplier=-1)
        g.affine_select(out=NEG_B[:, 0:128], in_=NEG_B[:, 0:128], pattern=[[-1, 128]], compare_op=ALU.is_ge,
                        fill=NEGV, base=-1, channel_multiplier=1)
        t_pc = POOL.sig(g.affine_select(out=NEG_B[:, 128:256], in_=NEG_B[:, 128:256], pattern=[[1, 128]],
                                        compare_op=ALU.is_ge, fill=NEGV, base=0, channel_multiplier=-1))
        d_sk = mk_dsem("sk")
        bc_reg = POOL.e.to_reg(NE * CAP - 1)
        t_sk = d_sk.add(SP.e.dma_start(out=sk_t[:], in_=sinks.to_broadcast([128, 16])))
        DVE.wait(t_pc)
        t_idb = DVE.sig(DVE.e.tensor_copy(out=ident_b[:], in_=ident_f[:]))
        ACT.wait(t_sk)
        t_es = ACT.sig(ACT.e.activation(out=es_t[:], in_=sk_t[:], func=AF.Exp))
        t_const = [t_pc, t_idb, t_es]

        def rms_stats(col, src, junk, src_tok):
            ACT.wait(src_tok)
            t1 = ACT.sig(ACT.e.activation(out=junk, in_=src, func=AF.Square, accum_out=ss[:, col:col + 1]))
            ACT.wait(t1)
            t2 = ACT.sig(ACT.e.activation(out=sd[:, col:col + 1], in_=ss[:, col:col + 1], func=AF.Sqrt,
                                          scale=1.0 / D, bias=EPS))
            DVE.wait(t2)
            t3 = DVE.sig(DVE.e.reciprocal(out=rs[:, col:col + 1], in_=sd[:, col:col + 1]))
            return t3

        dbg = None
        if stage < 99:
            dbg = nc.dram_tensor("dbg", [S, D], F32, kind="ExternalOutput").ap()

        st_mixed = ExitStack()
        mixedT = sb("mixedT", [128, 16, S], BF16, st_mixed)
        st_aT = ExitStack()
        aT = sb("aT", [128, 16, S], BF16, st_aT)

        with ExitStack() as s1:
            xt = [sb("xt%d" % i, [128, D], F32, s1) for i in range(2)]
            at = [sb("at%d" % i, [128, D], BF16, s1) for i in range(2)]
            gm = sb("gm", [128, D], F32, s1)
            junk = sb("junk", [128, D], BF16, s1)
            psT = pst("psT1", [128, 2, 1024], BF16, s1)
            dx = [mk_dsem("x%d" % i) for i in range(2)]
            d_g = mk_dsem("gmix")
            t_g = d_g.add(SP.e.dma_start(out=gm[:], in_=g_mix.to_broadcast([128, D])))
            xt_free = [None, None]
            at_free = [None, None]
            psT_free = [None, None]
            p1 = {}

            def pre1(j):
                b = j % 2
                SP.wait(xt_free[b])
                t_x = dx[b].add(SP.e.dma_start(out=xt[b][:], in_=x[128 * j:128 * j + 128, :]))
                t_rs = rms_stats(j, xt[b][:], junk[:], t_x)
                DVE.wait(t_rs, t_x, t_g, at_free[b])
                t_a = DVE.sig(DVE.e.scalar_tensor_tensor(out=at[b][:], in0=xt[b][:], scalar=rs[:, j:j + 1],
                                                         in1=gm[:], op0=ALU.mult, op1=ALU.mult))
                xt_free[b] = t_a
                p1[j] = t_a

            def post1(j):
                b = j % 2
                PE.wait(p1[j], t_idb)
                t_tr = [None, None]
                for c in range(16):
                    if c % 8 == 0:
                        PE.wait(psT_free[c // 8])
                    ins = PE.e.transpose(out=psT[:, c // 8, (c % 8) * 128:(c % 8) * 128 + 128],
                                         in_=at[b][:, c * 128:c * 128 + 128], identity=ident_b[:])
                    if c % 8 == 7:
                        t_tr[c // 8] = PE.sig(ins)
                at_free[b] = t_tr[1]
                ACT.wait(t_tr[0])
                psT_free[0] = ACT.sig(ACT.e.activation(
                    out=aT[:, 0:8, 128 * j:128 * j + 128],
                    in_=psT[:, 0, :].rearrange("p (c t) -> p c t", c=8), func=AF.Copy))
                DVE.wait(t_tr[1])
                psT_free[1] = DVE.sig(DVE.e.tensor_copy(
                    out=aT[:, 8:16, 128 * j:128 * j + 128],
                    in_=psT[:, 1, :].rearrange("p (c t) -> p c t", c=8)))

            pre1(0)
            for j in range(NT):
                if j + 1 < NT:
                    pre1(j + 1)
                post1(j)
            t_aT = [psT_free[0], psT_free[1]]
            barrier()

        if stage == 1:
            with ExitStack() as sd_:
                tmp = sb("dbgtmp", [128, S], F32, sd_)
                dd = mk_dsem("dbg")
                for c in range(16):
                    DVE.wait(dd.tok())
                    t = DVE.sig(DVE.e.tensor_copy(out=tmp[:], in_=aT[:, c, :]))
                    SP.wait(t)
                    dd.add(SP.e.dma_start(out=dbg[128 * c:128 * c + 128, :], in_=tmp[:]))
                SP.wait(dd.tok())
                barrier()
            st_aT.close()
            st_mixed.close()
            return nc

        d_wo2 = mk_dsem("wout_pf")
        with ExitStack() as s2:
            slab = [sb("slab%d" % i, [128, 16, 128], BF16, s2) for i in range(4)]
            dW = [mk_dsem("slab%d" % i) for i in range(4)]
            QT = sb("QT", [128, S], BF16, s2)
            KT = sb("KT", [128, S], BF16, s2)
            VT = sb("VT", [128, S], BF16, s2)
            Vs = [sb("Vs%d" % i, [128, 16, 128], BF16, s2) for i in range(2)]
            ODacc = sb("ODacc", [128, 2, S], F32, s2)
            PT = [sb("PT%d" % i, [128, 512], BF16, s2) for i in range(2)]
            MB2 = [[sb("MB_%d_%d" % (i, r), [128, 512], BF16, s2) for r in range(3)] for i in range(2)]
            btmp = [sb("btmp%d" % i, [128, 2, 128], F32, s2) for i in range(2)]
            psP = pst("psP", [128, 2, 512], F32, s2)
            psT = pst("psT2", [128, 1, 1024], BF16, s2)
            psS = pst("psS", [128, 2, 512], F32, s2)
            psO = pst("psO", [128, 2, 512], F32, s2)

            st = dict(slab_n=0, slab_free=[None] * 4, psP_n=0, psP_free=[None, None],
                      psT_free=None, vs_n=0, vs_free=[None, None], mb_n=0, mb_free=[None, None])
            buf_tok = {}

            def mat_job(pieces, dst, scale, dst_free):
                k = st["slab_n"] % 4
                st["slab_n"] += 1
                POOL.wait(st["slab_free"][k])
                off = 0
                tok = None
                for (c0, n) in pieces:
                    tok = dW[k].add(POOL.e.dma_start(
                        out=slab[k][:, :, off:off + n],
                        in_=w_in[:, c0:c0 + n].rearrange("(c p) n -> p c n", p=128)))
                    off += n
                assert off == 128
                bg_issue(3)
                last = None
                for t4 in range(4):
                    bk = st["psP_n"] % 2
                    st["psP_n"] += 1
                    PE.wait(tok, st["psP_free"][bk], t_aT)
                    for kc in range(16):
                        ins = PE.e.matmul(psP[:, bk, :], lhsT=slab[k][:, kc, :], rhs=aT[:, kc, 512 * t4:512 * t4 + 512],
                                          start=(kc == 0), stop=(kc == 15))
                    t_mm = PE.sig(ins)
                    if bk == 0:
                        ACT.wait(t_mm, dst_free)
                        last = ACT.sig(ACT.e.activation(out=dst[:, 512 * t4:512 * t4 + 512], in_=psP[:, bk, :],
                                                        func=AF.Copy, scale=float(scale)))
                    else:
                        DVE.wait(t_mm, dst_free)
                        last = DVE.sig(DVE.e.tensor_scalar(out=dst[:, 512 * t4:512 * t4 + 512], in0=psP[:, bk, :],
                                                           scalar1=float(scale), scalar2=None, op0=ALU.mult))
                    st["psP_free"][bk] = last
                    if t4 == 2:
                        prev = last
                st["slab_free"][k] = t_mm
                return [prev, last]

            def v_layout(src, src_tok, s):
                vi = st["vs_n"] % 2
                st["vs_n"] += 1
                V = Vs[vi]
                toks = []
                for half in range(2):
                    PE.wait(src_tok, st["psT_free"], t_idb)
                    for i8 in range(8):
                        slot = half * 8 + i8
                        b_, rho = slot // s, slot % s
                        start = s * 128 * b_ + rho
                        ins = PE.e.transpose(out=psT[:, 0, i8 * 128:i8 * 128 + 128],
                                             in_=src[:, start:start + 127 * s + 1:s], identity=ident_b[:])
                    t_tr = PE.sig(ins)
                    buf_tok["vt_free"] = t_tr
                    E = ACT if half == 0 else DVE
                    E.wait(t_tr, st["vs_free"][vi])
                    if half == 0:
                        t_e = ACT.sig(ACT.e.activation(out=V[:, 0:8, :],
                                                       in_=psT[:, 0, :].rearrange("p (c t) -> p c t", c=8),
                                                       func=AF.Copy))
                    else:
                        t_e = DVE.sig(DVE.e.tensor_copy(out=V[:, 8:16, :],
                                                        in_=psT[:, 0, :].rearrange("p (c t) -> p c t", c=8)))
                    st["psT_free"] = t_e
                    toks.append(t_e)
                return vi, toks

            def make_mask(coefs, negt):
                par = st["mb_n"] % 2
                st["mb_n"] += 1
                DVE.wait(st["mb_free"][par], t_const)
                t = None
                for r, c in enumerate(coefs):
                    for u in range(2):
                        t = DVE.sig(DVE.e.scalar_tensor_tensor(out=MB2[par][r][:, 256 * u:256 * u + 256], in0=DT[:],
                                                               scalar=-float(c), in1=negt[:], op0=ALU.mult,
                                                               op1=ALU.add))
                return par, t

            batches = []

            def unit_cols(s, b_, rho):
                start = s * 128 * b_ + rho
                return slice(start, start + 127 * s + 1, s)

            for h in range(8):
                slope = 2.0 ** (-(h + 1))
                hstate = {}

                def pre_head(h=h, hstate=hstate, slope=slope):
                    qk_free = buf_tok.get("qk_free")
                    vt_free = buf_tok.get("vt_free")
                    hstate["q"] = mat_job([(h * 128, 128)], QT, 128.0 ** -0.5, qk_free)
                    hstate["k"] = mat_job([(1024 + h * 128, 128)], KT, 1.0, qk_free)
                    hstate["v"] = mat_job([(2048 + h * 128, 128)], VT, 1.0, vt_free)
                    hstate["mb"] = make_mask([slope * 1, slope * 4, slope * 16], NEG_A)

                for r, s in enumerate((1, 4, 16)):
                    units = [(b_, rho) for b_ in range(16 // s) for rho in range(s)]
                    bstate = {}

                    def pre_branch(s=s, hstate=hstate, bstate=bstate):
                        bstate["v"] = v_layout(VT, hstate["v"], s)

                    for ib in range(8):
                        us = units[2 * ib:2 * ib + 2]
                        pres = []
                        if r == 0 and ib == 0:
                            pres.append(pre_head)
                        if ib == 0:
                            pres.append(pre_branch)
                        batches.append(dict(kind="A", h=h, r=r, s=s, units=us, pres=pres, hstate=hstate,
                                            bstate=bstate, first=(r == 0), last_of_head=(r == 2 and ib == 7),
                                            last_of_branch=(ib == 7), rows=slice(0, 128)))

            kvstates = [{}, {}]
            for c in range(8):
                gkv = c // 4
                hstate = {}
                kvstate = kvstates[gkv]

                def pre_kv(gkv=gkv, kvstate=kvstate):
                    qk_free = buf_tok.get("qk_free")
                    vt_free = buf_tok.get("vt_free")
                    k0 = 4096 + gkv * 64
                    v0 = 4224 + gkv * 64
                    kvstate["k"] = mat_job([(k0, 64), (k0, 64)], KT, 1.0, qk_free)
                    kvstate["vT"] = mat_job([(v0, 64), (v0, 64)], VT, 1.0, vt_free)
                    kvstate["v"] = v_layout(VT, kvstate["vT"], 1)

                def pre_chunk(c=c, hstate=hstate, kvstate=kvstate):
                    qk_free = buf_tok.get("qk_free")
                    hstate["q"] = mat_job([(3072 + c * 128, 128)], QT, 64.0 ** -0.5, qk_free)
                    if c == 7:
                        SP.wait(st["slab_free"][(st["slab_n"] - 1) % 4], d_pre_wo.tok())
                        for kc4 in range(4):
                            buf_tok["t_w"] = d_wo2.add(SP.e.dma_start(out=aT[:, 4 * kc4:4 * kc4 + 4, :],
                                                                       in_=WO[:, 4 * kc4:4 * kc4 + 4, :]))
                    hstate["k"] = kvstate["k"]
                    sl = [2.0 ** (-8.0 * (2 * c + hh + 1) / 16.0) for hh in range(2)]
                    hstate["mb"] = make_mask(sl, NEG_B)

                for hh in range(2):
                    for ib in range(8):
                        us = [(2 * ib, 0), (2 * ib + 1, 0)]
                        pres = []
                        if hh == 0 and ib == 0:
                            if c % 4 == 0:
                                pres.append(pre_kv)
                            pres.append(pre_chunk)
                        batches.append(dict(kind="B", h=2 * c + hh, c=c, hh=hh, r=hh, s=1, units=us, pres=pres,
                                            hstate=hstate, bstate=kvstate, first=True, last_of_head=False,
                                            last_of_branch=(c % 4 == 3 and hh == 1 and ib == 7),
                                            last_q=(hh == 1 and ib == 7),
                                            rows=slice(64 * hh, 64 * hh + 64)))

            if stage == 2:
                batches = [bb for bb in batches if bb["kind"] == "A" and bb["h"] < 1]

            NB = len(batches)
            S_free = [None, None]
            PT_free = [None, None]
            O_free = [None, None]
            tS = [None] * NB
            tP = [None] * NB

            def emit_qk(n):
                bb = batches[n]
                for f in bb["pres"]:
                    f()
                pb = n % 2
                hs = bb["hstate"]
                rows = bb["rows"]
                par, t_mb = hs["mb"]
                PE.wait(S_free[pb], hs["q"], hs["k"], t_mb, t_idb)
                first = True
                for u, (b_, rho) in enumerate(bb["units"]):
                    qc = unit_cols(bb["s"], b_, rho)
                    if b_ > 0:
                        kc = unit_cols(bb["s"], b_ - 1, rho)
                        PE.e.matmul(psS[:, pb, 256 * u:256 * u + 128], lhsT=KT[rows, kc], rhs=QT[rows, qc],
                                    start=first, stop=False)
                        first = False
                    PE.e.matmul(psS[:, pb, 256 * u + 128:256 * u + 256], lhsT=KT[rows, qc], rhs=QT[rows, qc],
                                start=first, stop=False)
                    first = False
                ins = PE.e.matmul(psS[:, pb, :], lhsT=ident_b[:], rhs=MB2[par][bb["r"]][:], start=False, stop=True)
                tS[n] = PE.sig(ins)
                if bb["last_of_head"] or bb.get("last_q"):
                    buf_tok["qk_free"] = tS[n]
                if bb.get("last_q") or bb["last_of_head"]:
                    st["mb_free"][par] = tS[n]

            def emit_exp(n):
                pb = n % 2
                ACT.wait(tS[n], PT_free[pb])
                tP[n] = ACT.sig(ACT.e.activation(out=PT[pb][:], in_=psS[:, pb, :], func=AF.Exp))
                S_free[pb] = tP[n]

            def emit_pv(n):
                bb = batches[n]
                pb = n % 2
                vi, vtoks = bb["bstate"]["v"]
                V = Vs[vi]
                s = bb["s"]
                PE.wait(tP[n], O_free[pb], vtoks)
                first = True
                ins = None
                for u, (b_, rho) in enumerate(bb["units"]):
                    slot = b_ * s + rho
                    parts = []
                    if b_ > 0:
                        parts.append(((b_ - 1) * s + rho, PT[pb][:, 256 * u:256 * u + 128]))
                    parts.append((slot, PT[pb][:, 256 * u + 128:256 * u + 256]))
                    for i, (sl, pt) in enumerate(parts):
                        PE.e.matmul(psO[:, pb, 256 * u:256 * u + 128], lhsT=V[:, sl, :], rhs=pt,
                                    start=first, stop=False)
                        first = False
                    for i, (sl, pt) in enumerate(parts):
                        ins = PE.e.matmul(psO[:, pb, 256 * u + 128:256 * u + 256], lhsT=ones_b[:], rhs=pt,
                                          start=False, stop=(u == 1 and i == len(parts) - 1))
                t_o = PE.sig(ins)
                PT_free[pb] = t_o
                if bb["last_of_branch"]:
                    st["vs_free"][vi] = t_o
                return t_o

            def emit_evac(n, t_o):
                bb = batches[n]
                pb = n % 2
                s = bb["s"]
                (b0, r0), (b1, r1) = bb["units"]
                if bb["kind"] == "A":
                    if s == 1:
                        oap = ODacc[:, :, 128 * b0:128 * b0 + 256].rearrange("p o (u q) -> p o u q", u=2)
                    else:
                        lo = s * 128 * b0
                        oap = ODacc[:, :, lo:lo + 128 * s].rearrange("p o (q r) -> p o r q", r=s)[:, :, r0:r0 + 2, :]
                    iap = psO[:, pb, :].rearrange("p (u o q) -> p o u q", u=2, o=2)
                    DVE.wait(t_o, buf_tok.get("od_free"), buf_tok.get("od_last"))
                    if bb["first"]:
                        t_e = DVE.sig(DVE.e.tensor_copy(out=oap, in_=iap))
                    else:
                        t_e = DVE.sig(DVE.e.tensor_tensor(out=oap, in0=iap, in1=oap, op=ALU.add))
                    buf_tok["od_last"] = t_e
                    O_free[pb] = t_e
                    if bb["last_of_head"]:
                        h = bb["h"]
                        ACT.wait(t_e)
                        t0 = ACT.sig(ACT.e.activation(out=ODacc[:, 1, :], in_=ODacc[:, 1, :], func=AF.Ln))
                        ACT.wait(t0)
                        t1 = ACT.sig(ACT.e.activation(out=ODacc[:, 1, :], in_=ODacc[:, 1, :], func=AF.Exp, scale=-1.0))
                        DVE.wait(t1)
                        t2 = DVE.sig(DVE.e.tensor_tensor(out=mixedT[:, h, :], in0=ODacc[:, 0, :], in1=ODacc[:, 1, :],
                                                         op=ALU.mult))
                        buf_tok["od_free"] = t2
                        buf_tok["mixed_last"] = t2
                else:
                    rows = bb["rows"]
                    h = bb["h"]
                    c = bb["c"]
                    iv = psO[rows, pb, :].rearrange("p (u o q) -> p o u q", u=2, o=2)
                    tb = btmp[pb]
                    ACT.wait(t_o, t_es, buf_tok.get("btmp_free%d" % pb))
                    ta = ACT.sig(ACT.e.activation(out=tb[rows, :, :], in_=iv[:, 1, :, :], func=AF.Ln,
                                                  bias=es_t[rows, h:h + 1], scale=1.0))
                    ACT.wait(ta)
                    tb_ = ACT.sig(ACT.e.activation(out=tb[rows, :, :], in_=tb[rows, :, :], func=AF.Exp, scale=-1.0))
                    DVE.wait(tb_)
                    t_e = DVE.sig(DVE.e.tensor_tensor(
                        out=mixedT[rows, 8 + c, 128 * b0:128 * b0 + 256].rearrange("p (u q) -> p u q", u=2),
                        in0=iv[:, 0, :, :], in1=tb[rows, :, :], op=ALU.mult))
                    O_free[pb] = t_e
                    buf_tok["btmp_free%d" % pb] = t_e
                    buf_tok["mixed_last"] = t_e

            emit_qk(0)
            emit_exp(0)
            for n in range(NB):
                if n + 1 < NB:
                    emit_qk(n + 1)
                    emit_exp(n + 1)
                t_o = emit_pv(n)
                emit_evac(n, t_o)
            barrier()

        if stage in (2, 22):
            with ExitStack() as sd_:
                tmp = sb("dbgtmp", [128, S], F32, sd_)
                dd = mk_dsem("dbg")
                for c in range(16):
                    DVE.wait(dd.tok())
                    t = DVE.sig(DVE.e.tensor_copy(out=tmp[:], in_=mixedT[:, c, :]))
                    SP.wait(t)
                    dd.add(SP.e.dma_start(out=dbg[128 * c:128 * c + 128, :], in_=tmp[:]))
                SP.wait(dd.tok())
                barrier()
            st_aT.close()
            st_mixed.close()
            return nc

        wout = aT
        with ExitStack() as s3:
            xt = [sb("xt3_%d" % i, [128, D], F32, s3) for i in range(2)]
            h1 = [sb("h1_%d" % i, [128, D], F32, s3) for i in range(2)]
            mf = sb("mf", [128, D], F32, s3)
            mhl = sb("mhl", [128, 2, D], BF16, s3)
            mhlT = sb("mhlT", [128, 2, 16, 128], BF16, s3)
            Whl = sb("Whl", [128, 2, 16, 36], BF16, s3)
            mb1 = sb("mb1", [128, D], BF16, s3)
            mb = [mb1, mb1]
            gmo = sb("gmo", [128, D], F32, s3)
            junk = mb1
            Wr = mf[:, 1024:1024 + 16 * 36].rearrange("p (c n) -> p c n", c=16)
            bias_bc = sb("bias_bc", [128, 36], F32, s3)
            psH = pst("psH", [128, 2, 2, 512], F32, s3)
            psF = pst("psF", [128, 2, 1024], BF16, s3)
            psZ = pst("psZ", [128, 512], F32, s3)
            d_w = mk_dsem("wout")
            dx = [mk_dsem("x3_%d" % i) for i in range(2)]
            dh = [mk_dsem("h1s%d" % i) for i in range(2)]
            dm1 = mk_dsem("ms")
            dm = [dm1, dm1]
            t_w = buf_tok["t_w"]
            t_c3 = d_c.add(SP.e.dma_start(out=gmo[:], in_=g_moe.to_broadcast([128, D])))
            t_c3 = d_c.add(SP.e.dma_start(out=Wr[:, :, 0:4], in_=w_grp.rearrange("(c p) n -> p c n", p=128)))
            t_c3 = d_c.add(SP.e.dma_start(out=Wr[:, :, 4:36], in_=w_exp.rearrange("(c p) n -> p c n", p=128)))
            t_c3 = d_c.add(SP.e.dma_start(out=bias_bc[:, 0:4], in_=b_grp.to_broadcast([128, 4])))
            t_c3 = d_c.add(SP.e.dma_start(out=bias_bc[:, 4:36], in_=b_exp.to_broadcast([128, 32])))

            DVE.wait(t_c3)
            t_ = DVE.sig(DVE.e.tensor_copy(out=Whl[:, 0, :, :], in_=Wr))
            DVE.wait(t_)
            Wtmp = mf[:, 0:16 * 36].rearrange("p (c n) -> p c n", c=16)
            t_ = DVE.sig(DVE.e.tensor_tensor(out=Wtmp, in0=Wr, in1=Whl[:, 0, :, :], op=ALU.subtract))
            DVE.wait(t_)
            t_whl = DVE.sig(DVE.e.tensor_copy(out=Whl[:, 1, :, :], in_=Wtmp))
            mf_free0 = t_whl
            psH_free = [None, None]
            psF_free = [None, None]
            psZ_free = None
            mhl_free = None
            xt_free = [None, None]
            h1_free = [None, None]
            mb_free = [None, None]
            mf_free = t_whl
            mfT_free = None
            t_hmm = {}

            def op_mm(j):
                b = j % 2
                bg_issue(4)
                SP.wait(xt_free[b])
                t_x = dx[b].add(SP.e.dma_start(out=xt[b][:], in_=x[128 * j:128 * j + 128, :]))
                mm = []
                for half in range(2):
                    PE.wait(psH_free[half], t_w)
                    for nn in range(2):
                        for kc in range(16):
                            ins = PE.e.matmul(psH[:, half, nn, :], lhsT=mixedT[:, kc, 128 * j:128 * j + 128],
                                              rhs=wout[:, kc, 1024 * half + 512 * nn:1024 * half + 512 * nn + 512],
                                              start=(kc == 0), stop=(kc == 15))
                    mm.append(PE.sig(ins))
                t_hmm[j] = dict(t_x=t_x, mm=mm)

            def op_add(j):
                b = j % 2
                a = t_hmm[j]
                toks = []
                for half in range(2):
                    DVE.wait(a["mm"][half], a["t_x"], h1_free[b])
                    t_h = DVE.sig(DVE.e.tensor_tensor(out=h1[b][:, 1024 * half:1024 * half + 1024],
                                                      in0=psH[:, half, :, :].rearrange("p n f -> p (n f)"),
                                                      in1=xt[b][:, 1024 * half:1024 * half + 1024], op=ALU.add))
                    psH_free[half] = t_h
                    toks.append(t_h)
                xt_free[b] = toks[1]
                a["toks"] = toks

            def rest_a(j):
                nonlocal mf_free, mfT_free, psZ_free
                b = j % 2
                toks = t_hmm[j]["toks"]
                SP.wait(toks)
                t_hs = dh[b].add(SP.e.dma_start(out=H1[128 * j:128 * j + 128, :], in_=h1[b][:]))
                ACT.wait(mb_free)
                t_rs = rms_stats(16 + j, h1[b][:], junk[:], toks)
                DVE.wait(toks, t_c3, mf_free)
                t_mf = DVE.sig(DVE.e.tensor_tensor(out=mf[:], in0=h1[b][:], in1=gmo[:], op=ALU.mult))
                ACT.wait(t_mf, t_rs, mb_free)
                t_m = ACT.sig(ACT.e.activation(out=mb[b][:], in_=mf[:], func=AF.Copy, scale=rs[:, 16 + j:17 + j]))
                SP.wait(t_m)
                mb_free[b] = dm[b].add(SP.e.dma_start(out=Mscr[128 * j:128 * j + 128, :], in_=mb[b][:]))
                DVE.wait(t_mf, mhl_free)
                t_hi = DVE.sig(DVE.e.tensor_copy(out=mhl[:, 0, :], in_=mf[:]))
                DVE.wait(t_hi)
                t_lo = DVE.sig(DVE.e.tensor_tensor(out=mhl[:, 1, :], in0=mf[:], in1=mhl[:, 0, :], op=ALU.subtract))
                t_hmm[j].update(t_hs=t_hs, t_rs=t_rs, t_mf=t_mf, t_m=t_m, t_lo=t_lo)

            def rest_b(j):
                nonlocal mf_free, mfT_free, psZ_free, mhl_free
                b = j % 2
                a = t_hmm[j]
                t_hs, t_rs, t_mf, t_m, t_lo = a["t_hs"], a["t_rs"], a["t_mf"], a["t_m"], a["t_lo"]
                evs = []
                for q4 in range(4):
                    fb = q4 % 2
                    hl, ch = q4 // 2, q4 % 2
                    PE.wait(t_lo, psF_free[fb], t_idb)
                    for i8 in range(8):
                        c = 8 * ch + i8
                        ins = PE.e.transpose(out=psF[:, fb, 128 * i8:128 * i8 + 128], in_=mhl[:, hl, 128 * c:128 * c + 128],
                                             identity=ident_b[:])
                    t_tr = PE.sig(ins)
                    E = ACT if fb == 0 else DVE
                    E.wait(t_tr, mfT_free)
                    if fb == 0:
                        t_e = ACT.sig(ACT.e.activation(out=mhlT[:, hl, 8 * ch:8 * ch + 8, :],
                                                       in_=psF[:, fb, :].rearrange("p (c t) -> p c t", c=8),
                                                       func=AF.Copy))
                    else:
                        t_e = DVE.sig(DVE.e.tensor_copy(out=mhlT[:, hl, 8 * ch:8 * ch + 8, :],
                                                        in_=psF[:, fb, :].rearrange("p (c t) -> p c t", c=8)))
                    psF_free[fb] = t_e
                    evs.append(t_e)
                mhl_free = t_tr
                mf_free = [t_lo, t_m]
                h1_free[b] = [t_hs, t_mf, t_rs]
                PE.wait(evs, psZ_free, t_whl)
                n_mm = 0
                for (xa, wa_) in ((0, 0), (1, 0), (0, 1)):
                    for c in range(16):
                        ins = PE.e.matmul(psZ[:, 0:36], lhsT=mhlT[:, xa, c, :], rhs=Whl[:, wa_, c, :],
                                          start=(n_mm == 0), stop=(n_mm == 47))
                        n_mm += 1
                t_z = PE.sig(ins)
                mfT_free = t_z
                DVE.wait(t_z, t_rs)
                psZ_free = DVE.sig(DVE.e.scalar_tensor_tensor(out=lg_all[:, j, :], in0=psZ[:, 0:36],
                                                              scalar=rs[:, 16 + j:17 + j], in1=bias_bc[:],
                                                              op0=ALU.mult, op1=ALU.add))

            op_mm(0)
            op_add(0)
            for j in range(NT):
                if j + 1 < NT:
                    op_mm(j + 1)
                rest_a(j)
                if j + 1 < NT:
                    op_add(j + 1)
                rest_b(j)
            barrier()
        st_aT.close()
        st_mixed.close()

        if stage == 3:
            with ExitStack() as sd_:
                tmp = sb("dbgtmp", [128, S], F32, sd_)
                dd = mk_dsem("dbg")
                for c in range(16):
                    SP.wait(dd.tok())
                    dd.add(SP.e.dma_start(out=tmp[:], in_=H1[128 * c:128 * c + 128, :]))
                    SP.wait(dd.tok())
                    dd.add(SP.e.dma_start(out=dbg[128 * c:128 * c + 128, :], in_=tmp[:]))
                SP.wait(dd.tok())
                barrier()
            return nc

        bg_issue(1000)
        st_wpg = ExitStack()
        wpg = sb("wpg", [128, 16, D], BF16, st_wpg)
        d_wpg = mk_dsem("wpg")
        with ExitStack() as s5:
            NMT = 4
            mt = [sb("mt%d" % i, [128, D], BF16, s5) for i in range(NMT)]
            wgu = [sb("wgu%d" % i, [128, 16, 512], BF16, s5) for i in range(2)]
            wdn = [sb("wdn%d" % i, [128, 2, D], BF16, s5) for i in range(2)]
            xs = [[sb("xs%d_%d" % (i, s_), [128, D], BF16, s5) for s_ in range(2)] for i in range(2)]
            XT = [sb("XT%d" % i, [128, 16, 256], BF16, s5) for i in range(2)]
            sg = sb("sg", [128, 2, 256], F32, s5)
            hidT = sb("hidT", [128, 2, 256], BF16, s5)
            yb = [sb("yb%d" % i, [128, D], BF16, s5) for i in range(2)]
            dmt = [mk_dsem("mt%d" % i) for i in range(NMT)]
            dsc = [mk_dsem("sc%d" % i) for i in range(NMT)]
            dwe = [mk_dsem("we%d" % i) for i in range(2)]
            dxs = [mk_dsem("xs%d" % i) for i in range(2)]
            dys = [mk_dsem("ys%d" % i) for i in range(2)]
            mt_tok = {}
            for j in range(NMT):
                mt_tok[j] = dmt[j].add(SP.e.dma_start(out=mt[j][:], in_=Mscr[128 * j:128 * j + 128, :]))

            with ExitStack() as s4:
                def t3(name, shape, dt=F32):
                    return sb(name, shape, dt, s4)
                gmax = t3("gmax", [128, NT]); goh = t3("goh", [128, NT, 4]); tmp4 = t3("tmp4", [128, NT, 4])
                sume = t3("sume", [128, NT]); grpw = t3("grpw", [128, NT]); pen4 = t3("pen4", [128, NT, 4])
                Lm = t3("Lm", [128, NT, 32]); Lm2 = t3("Lm2", [128, NT, 32])
                m1 = t3("m1", [128, NT]); m2 = t3("m2", [128, NT])
                oh = [t3("oh%d" % k, [128, NT, 32]) for k in range(2)]
                Mk = t3("Mk", [128, NT, 32], BF16)
                pos = t3("pos", [128, NT, 32]); tmp32 = t3("tmp32", [128, NT, 32])
                iota3 = t3("iota3", [128, NT, 32])
                pk = t3("pk", [128, NT]); ek = t3("ek", [128, NT]); dk = t3("dk", [128, NT]); ok = t3("ok", [128, NT])
                d21 = t3("d21", [128, NT]); e21 = t3("e21", [128, NT]); wa = t3("wa", [128, NT]); wb_ = t3("wb_", [128, NT])
                psR = pst("psR", [128, 512], F32, s4)
                t_io = POOL.sig(POOL.e.iota(iota3[:], pattern=[[0, NT], [1, 32]], base=0, channel_multiplier=0,
                                            allow_small_or_imprecise_dtypes=True))
                last = [None]

                def dv(inst_fn, extra=None):
                    DVE.wait(last[0], extra)
                    last[0] = DVE.sig(inst_fn())
                    return last[0]

                Lg = lg_all[:, :, 0:4]
                Le4 = lg_all[:, :, 4:36].rearrange("p j (g e) -> p j g e", g=4)
                V = DVE.e
                dv(lambda: V.tensor_reduce(out=gmax[:], in_=Lg, axis=AX.X, op=ALU.max))
                dv(lambda: V.tensor_tensor(out=goh[:], in0=Lg, in1=gmax[:].to_broadcast([128, NT, 4]), op=ALU.is_equal))
                t = dv(lambda: V.tensor_tensor(out=tmp4[:], in0=Lg, in1=gmax[:].to_broadcast([128, NT, 4]), op=ALU.subtract))
                ACT.wait(t)
                t = ACT.sig(ACT.e.activation(out=tmp4[:], in_=tmp4[:], func=AF.Exp))
                dv(lambda: V.tensor_reduce(out=sume[:], in_=tmp4[:], axis=AX.X, op=ALU.add), t)
                dv(lambda: V.reciprocal(out=grpw[:], in_=sume[:]))
                dv(lambda: V.tensor_scalar(out=pen4[:], in0=goh[:], scalar1=BIGV, scalar2=-BIGV, op0=ALU.mult, op1=ALU.add))
                dv(lambda: V.tensor_tensor(out=Lm[:].rearrange("p j (g e) -> p j g e", g=4), in0=Le4,
                                           in1=pen4[:].to_broadcast([128, NT, 4, 8]), op=ALU.add))
                dv(lambda: V.tensor_reduce(out=m1[:], in_=Lm[:], axis=AX.X, op=ALU.max))
                dv(lambda: V.tensor_tensor(out=oh[0][:], in0=Lm[:], in1=m1[:].to_broadcast([128, NT, 32]), op=ALU.is_equal))
                dv(lambda: V.scalar_tensor_tensor(out=Lm2[:], in0=oh[0][:], scalar=-BIGV, in1=Lm[:], op0=ALU.mult, op1=ALU.add))
                dv(lambda: V.tensor_reduce(out=m2[:], in_=Lm2[:], axis=AX.X, op=ALU.max))
                dv(lambda: V.tensor_tensor(out=oh[1][:], in0=Lm2[:], in1=m2[:].to_broadcast([128, NT, 32]), op=ALU.is_equal))
                t_mk = dv(lambda: V.tensor_tensor(out=Mk[:], in0=oh[0][:], in1=oh[1][:], op=ALU.add))
                t = dv(lambda: V.tensor_tensor(out=d21[:], in0=m2[:], in1=m1[:], op=ALU.subtract))
                ACT.wait(t)
                t = ACT.sig(ACT.e.activation(out=e21[:], in_=d21[:], func=AF.Exp))
                dv(lambda: V.tensor_scalar(out=wa[:], in0=e21[:], scalar1=1.0, scalar2=None, op0=ALU.add), t)
                dv(lambda: V.reciprocal(out=wa[:], in_=wa[:]))
                dv(lambda: V.tensor_tensor(out=wb_[:], in0=wa[:], in1=e21[:], op=ALU.mult))
                dv(lambda: V.tensor_tensor(out=wa[:], in0=wa[:], in1=grpw[:], op=ALU.mult))
                dv(lambda: V.tensor_tensor(out=wb_[:], in0=wb_[:], in1=grpw[:], op=ALU.mult))
                PE.wait(t_mk, t_pc)
                for j in range(NT):
                    PE.e.matmul(psR[:, 32 * j:32 * j + 32], lhsT=Utri[:], rhs=Mk[:, j, :], start=(j == 0), stop=False)
                for j in range(NT - 1):
                    for j2 in range(j + 1, NT):
                        ins = PE.e.matmul(psR[:, 32 * j2:32 * j2 + 32], lhsT=ones_b[:], rhs=Mk[:, j, :], start=False,
                                          stop=(j == NT - 2))
                t_pos = PE.sig(ins)
                dv(lambda: V.tensor_copy(out=pos[:].rearrange("p j e -> p (j e)"), in_=psR[:]), t_pos)
                for k in range(2):
                    wk = wa if k == 0 else wb_
                    dv(lambda: V.tensor_tensor(out=tmp32[:], in0=oh[k][:], in1=pos[:], op=ALU.mult))
                    dv(lambda: V.tensor_reduce(out=pk[:], in_=tmp32[:], axis=AX.X, op=ALU.add))
                    dv(lambda: V.tensor_tensor(out=tmp32[:], in0=oh[k][:], in1=iota3[:], op=ALU.mult), t_io)
                    dv(lambda: V.tensor_reduce(out=ek[:], in_=tmp32[:], axis=AX.X, op=ALU.add))
                    dv(lambda: V.scalar_tensor_tensor(out=dk[:], in0=ek[:], scalar=float(CAP), in1=pk[:], op0=ALU.mult, op1=ALU.add))
                    dv(lambda: V.tensor_scalar(out=ok[:], in0=pk[:], scalar1=float(CAP), scalar2=1.0e6, op0=ALU.is_ge, op1=ALU.mult))
                    dv(lambda: V.tensor_tensor(out=dk[:], in0=dk[:], in1=ok[:], op=ALU.add))
                    dv(lambda: V.tensor_copy(out=dst_i[:, k, :], in_=dk[:]))
                    dv(lambda: V.tensor_scalar(out=ok[:], in0=pk[:], scalar1=float(CAP), scalar2=None, op0=ALU.is_lt))
                    dv(lambda: V.tensor_tensor(out=wts[:, k, :], in0=wk[:], in1=ok[:], op=ALU.mult))
                t_route = last[0]


            s5p = ExitStack()
            psT = pst("psT5", [128, 2, 1024], BF16, s5p)
            psG = pst("psG", [128, 2, 2, 256], F32, s5p)
            psY = pst("psY", [128, 2, 2, 512], F32, s5p)

            ACT.wait(d_pre_pg.tok())
            for kc4 in range(4):
                t_wpg = d_wpg.add(ACT.e.dma_start(out=wpg[:, 4 * kc4:4 * kc4 + 4, :], in_=WPG[:, 4 * kc4:4 * kc4 + 4, :]))
            sc_tok = [None] * NMT
            for j in range(NT):
                b = j % NMT
                if j >= NMT:
                    SP.wait(sc_tok[b])
                    mt_tok[j] = dmt[b].add(SP.e.dma_start(out=mt[b][:], in_=Mscr[128 * j:128 * j + 128, :]))
                POOL.wait(mt_tok[j], t_route)
                for k in range(2):
                    sc_tok[b] = dsc[b].add(POOL.e.indirect_dma_start(
                        out=Xg, out_offset=bass.IndirectOffsetOnAxis(ap=dst_i[:, k, j:j + 1], axis=0),
                        in_=mt[b][:], in_offset=None, bounds_check=bc_reg, oob_is_err=False))
            t_scatter = list(sc_tok)

            def load_w(e):
                b = e % 2
                ACT.wait(w_free[b], d_pre_ex[e // 4].tok())
                dwe[b].add(ACT.e.dma_start(out=wgu[b][:].rearrange("p c f -> p (c f)"), in_=WGU[e]))
                return dwe[b].add(ACT.e.dma_start(out=wdn[b][:].rearrange("p c d -> p (c d)"), in_=WDN[e]))

            def load_x(e):
                b = e % 2
                SP.wait(t_scatter, xs_free[b])
                for s_ in range(2):
                    t = dxs[b].add(SP.e.dma_start(out=xs[b][s_][:], in_=Xg[e * CAP + 128 * s_:e * CAP + 128 * s_ + 128, :]))
                return t

            w_free = [None, None]
            xs_free = [None, None]
            psT_free = [None, None]
            psG_free = [None, None]
            psY_free = [None, None]
            yb_free = [None, None]
            XT_free = [None, None]
            hid_free = None
            sg_free = None
            xt_evs = {}
            t_w = {0: load_w(0)}
            t_x = {0: load_x(0)}
            if NE > 1:
                t_x[1] = load_x(1)

            def tr_round(e, r):
                b = e % 2
                s_, ch = r // 2, r % 2
                tb = r % 2
                PE.wait(t_x[e], psT_free[tb], t_idb)
                for i8 in range(8):
                    c = 8 * ch + i8
                    ins = PE.e.transpose(out=psT[:, tb, 128 * i8:128 * i8 + 128],
                                         in_=xs[b][s_][:, 128 * c:128 * c + 128], identity=ident_b[:])
                t_tr = PE.sig(ins)
                E = ACT if tb == 0 else DVE
                E.wait(t_tr, XT_free[b])
                if tb == 0:
                    t_e = ACT.sig(ACT.e.activation(out=XT[b][:, 8 * ch:8 * ch + 8, 128 * s_:128 * s_ + 128],
                                                   in_=psT[:, tb, :].rearrange("p (c t) -> p c t", c=8), func=AF.Copy))
                else:
                    t_e = DVE.sig(DVE.e.tensor_copy(out=XT[b][:, 8 * ch:8 * ch + 8, 128 * s_:128 * s_ + 128],
                                                    in_=psT[:, tb, :].rearrange("p (c t) -> p c t", c=8)))
                psT_free[tb] = t_e
                xt_evs.setdefault(e, []).append(t_e)
                if r == 3:
                    xs_free[b] = t_tr

            for r in range(4):
                tr_round(0, r)
            for e in range(NE):
                b = e % 2
                if e + 1 < NE:
                    t_w[e + 1] = load_w(e + 1)
                if e + 2 < NE:
                    t_x[e + 2] = load_x(e + 2)
                hts = []
                for fc in range(2):
                    PE.wait(xt_evs[e], t_w[e], psG_free[fc])
                    for gu in range(2):
                        for kc in range(16):
                            ins = PE.e.matmul(psG[:, fc, gu, :], lhsT=wgu[b][:, kc, 256 * gu + 128 * fc:256 * gu + 128 * fc + 128],
                                              rhs=XT[b][:, kc, :], start=(kc == 0), stop=(kc == 15))
                        if gu == 1:
                            t_g = PE.sig(ins)
                        if e + 1 < NE:
                            tr_round(e + 1, 2 * fc + gu)
                    ACT.wait(t_g, sg_free)
                    t_s = ACT.sig(ACT.e.activation(out=sg[:, fc, :], in_=psG[:, fc, 0, :], func=AF.Silu))
                    DVE.wait(t_s, hid_free)
                    t_h = DVE.sig(DVE.e.tensor_tensor(out=hidT[:, fc, :], in0=sg[:, fc, :], in1=psG[:, fc, 1, :], op=ALU.mult))
                    psG_free[fc] = t_h
                    hts.append(t_h)
                XT_free[b] = t_g
                sg_free = hts[1]
                ev_s = []
                for u in range(4):
                    s_, half = u // 2, u % 2
                    yi = u % 2
                    PE.wait(hts, psY_free[yi])
                    for nn in range(2):
                        for fc in range(2):
                            ins = PE.e.matmul(psY[:, yi, nn, :], lhsT=hidT[:, fc, 128 * s_:128 * s_ + 128],
                                              rhs=wdn[b][:, fc, 1024 * half + 512 * nn:1024 * half + 512 * nn + 512],
                                              start=(fc == 0), stop=(fc == 1))
                    t_y = PE.sig(ins)
                    if yi == 0:
                        ACT.wait(t_y, yb_free[s_])
                        t_c = ACT.sig(ACT.e.activation(out=yb[s_][:, 1024 * half:1024 * half + 1024],
                                                       in_=psY[:, yi, :, :].rearrange("p n f -> p (n f)"), func=AF.Copy))
                    else:
                        DVE.wait(t_y, yb_free[s_])
                        t_c = DVE.sig(DVE.e.tensor_copy(out=yb[s_][:, 1024 * half:1024 * half + 1024],
                                                        in_=psY[:, yi, :, :].rearrange("p n f -> p (n f)")))
                    psY_free[yi] = t_c
                    ev_s.append(t_c)
                    if half == 1:
                        SP.wait(ev_s)
                        ev_s = []
                        yb_free[s_] = dys[s_].add(SP.e.dma_start(out=Y[e * CAP + 128 * s_:e * CAP + 128 * s_ + 128, :], in_=yb[s_][:]))
                hid_free = t_y
                w_free[b] = t_y
            barrier()
            s5p.close()

        with ExitStack() as s6:
            h2 = [sb("h2_%d" % i, [128, D], F32, s6) for i in range(3)]
            y1 = [sb("y1_%d" % i, [128, D], BF16, s6) for i in range(2)]
            y2 = [sb("y2_%d" % i, [128, D], BF16, s6) for i in range(2)]
            nb = [sb("nb%d" % i, [128, D], BF16, s6) for i in range(2)]
            nT = [sb("nT%d" % i, [128, 16, 128], BF16, s6) for i in range(2)]
            ptl = [sb("ptl%d" % i, [128, 256], F32, s6) for i in range(2)]
            pbf = [sb("pbf%d" % i, [128, 256], BF16, s6) for i in range(2)]
            pT = [sb("pT%d" % i, [128, 2, 128], BF16, s6) for i in range(2)]
            wple = sb("wple", [128, 2, D], BF16, s6)
            gpl = sb("gpl", [128, D], F32, s6)
            gfn = sb("gfn", [128, D], F32, s6)
            sgm = [sb("sgm%d" % i, [128, 512], F32, s6) for i in range(2)]
            ot = [sb("ot%d" % i, [128, D], F32, s6) for i in range(2)]
            junk = sb("junk6", [128, D], BF16, s6)
            psT = pst("psT6", [128, 2, 1024], BF16, s6)
            psGt = pst("psGt", [128, 2, 512], F32, s6)
            psPe = pst("psPe", [128, 2, 512], F32, s6)
            d_c6 = mk_dsem("c6")
            dh2 = [mk_dsem("h2l%d" % i) for i in range(3)]
            dy = [mk_dsem("yg%d" % i) for i in range(2)]
            dp = [mk_dsem("pl%d" % i) for i in range(2)]
            dout = [mk_dsem("out%d" % i) for i in range(2)]
            t_c6 = d_c6.add(POOL.e.dma_start(out=wple[:], in_=w_ple.rearrange("(c p) d -> p c d", p=128)))
            t_c6 = d_c6.add(SP.e.dma_start(out=gpl[:], in_=g_ple.to_broadcast([128, D])))
            t_c6 = d_c6.add(SP.e.dma_start(out=gfn[:], in_=g_final.to_broadcast([128, D])))
            h2_free = [None, None, None]
            y_free = [None, None]
            nb_free = [None, None]
            nT_free = [None, None]
            p_free = [None, None]
            pbf_free = [None, None]
            pT_free = [None, None]
            psT_free = [None, None]
            psQ_free = [None, None]
            sgm_free = [None, None]
            ot_free = [None, None]
            stA = {}

            def A_pre(j):
                b = j % 2
                SP.wait(h2_free[j % 3])
                t_h = dh2[j % 3].add(SP.e.dma_start(out=h2[j % 3][:], in_=H1[128 * j:128 * j + 128, :]))
                SP.wait(p_free[b])
                t_p = dp[b].add(SP.e.dma_start(out=ptl[b][:], in_=p_in[128 * j:128 * j + 128, :]))
                POOL.wait(y_free[b])
                dy[b].add(POOL.e.indirect_dma_start(out=y1[b][:], out_offset=None, in_=Y,
                                                    in_offset=bass.IndirectOffsetOnAxis(ap=dst_i[:, 0, j:j + 1], axis=0),
                                                    bounds_check=bc_reg, oob_is_err=False))
                t_y = dy[b].add(POOL.e.indirect_dma_start(out=y2[b][:], out_offset=None, in_=Y,
                                                          in_offset=bass.IndirectOffsetOnAxis(ap=dst_i[:, 1, j:j + 1], axis=0),
                                                          bounds_check=bc_reg, oob_is_err=False))
                DVE.wait(t_h, t_y)
                t1 = DVE.sig(DVE.e.scalar_tensor_tensor(out=h2[j % 3][:], in0=y1[b][:], scalar=wts[:, 0, j:j + 1], in1=h2[j % 3][:],
                                                        op0=ALU.mult, op1=ALU.add))
                DVE.wait(t1)
                t2 = DVE.sig(DVE.e.scalar_tensor_tensor(out=h2[j % 3][:], in0=y2[b][:], scalar=wts[:, 1, j:j + 1], in1=h2[j % 3][:],
                                                        op0=ALU.mult, op1=ALU.add))
                y_free[b] = t2
                t_rs = rms_stats(32 + j, h2[j % 3][:], junk[:], t2)
                DVE.wait(t_rs, t_c6, nb_free[b])
                t_n = DVE.sig(DVE.e.scalar_tensor_tensor(out=nb[b][:], in0=h2[j % 3][:], scalar=rs[:, 32 + j:33 + j], in1=gpl[:],
                                                         op0=ALU.mult, op1=ALU.mult))
                ACT.wait(t_p, pbf_free[b])
                t_pb = ACT.sig(ACT.e.activation(out=pbf[b][:], in_=ptl[b][:], func=AF.Copy))
                p_free[b] = t_pb
                stA[j] = dict(t_n=t_n, t_pb=t_pb)

            def A_tr(j):
                b = j % 2
                a = stA[j]
                evs = []
                for ch in range(2):
                    PE.wait(a["t_n"], psT_free[ch])
                    for i8 in range(8):
                        c = 8 * ch + i8
                        ins = PE.e.transpose(out=psT[:, ch, 128 * i8:128 * i8 + 128], in_=nb[b][:, 128 * c:128 * c + 128],
                                             identity=ident_b[:])
                    if ch == 0:
                        t_tr = PE.sig(ins)
                        ACT.wait(t_tr, nT_free[b])
                        t_e = ACT.sig(ACT.e.activation(out=nT[b][:, 0:8, :],
                                                       in_=psT[:, 0, :].rearrange("p (c t) -> p c t", c=8), func=AF.Copy))
                        psT_free[0] = t_e
                        evs.append(t_e)
                    else:
                        t_tr = PE.sig(ins)
                        ACT.wait(t_tr, nT_free[b])
                        t_e = ACT.sig(ACT.e.activation(out=nT[b][:, 8:16, :],
                                                       in_=psT[:, 1, :].rearrange("p (c t) -> p c t", c=8), func=AF.Copy))
                        psT_free[1] = t_e
                        evs.append(t_e)
                nb_free[b] = t_tr
                PE.wait(a["t_pb"], psT_free[0])
                for c in range(2):
                    ins = PE.e.transpose(out=psT[:, 0, 128 * c:128 * c + 128], in_=pbf[b][:, 128 * c:128 * c + 128],
                                         identity=ident_b[:])
                t_tr = PE.sig(ins)
                pbf_free[b] = t_tr
                ACT.wait(t_tr, pT_free[b])
                t_e = ACT.sig(ACT.e.activation(out=pT[b][:], in_=psT[:, 0, 0:256].rearrange("p (c t) -> p c t", c=2),
                                               func=AF.Copy))
                psT_free[0] = t_e
                evs.append(t_e)
                a["evs"] = evs

            def B_mm_q(j, q):
                b = j % 2
                a = stA[j]
                qb = q % 2
                PE.wait(a["evs"], t_wpg, t_c6, psQ_free[qb])
                for kc in range(16):
                    PE.e.matmul(psGt[:, qb, :], lhsT=nT[b][:, kc, :], rhs=wpg[:, kc, 512 * q:512 * q + 512],
                                start=(kc == 0), stop=(kc == 15))
                for kc in range(2):
                    ins = PE.e.matmul(psPe[:, qb, :], lhsT=pT[b][:, kc, :], rhs=wple[:, kc, 512 * q:512 * q + 512],
                                      start=(kc == 0), stop=(kc == 1))
                t_mm = PE.sig(ins)
                ACT.wait(t_mm, sgm_free[qb])
                t_s = ACT.sig(ACT.e.activation(out=sgm[qb][:], in_=psGt[:, qb, :], func=AF.Sigmoid))
                DVE.wait(t_s)
                t_a = DVE.sig(DVE.e.tensor_tensor(out=sgm[qb][:], in0=sgm[qb][:], in1=psPe[:, qb, :], op=ALU.mult))
                psQ_free[qb] = t_a
                DVE.wait(t_a)
                tq = DVE.sig(DVE.e.tensor_tensor(out=h2[j % 3][:, 512 * q:512 * q + 512],
                                                 in0=h2[j % 3][:, 512 * q:512 * q + 512], in1=sgm[qb][:], op=ALU.add))
                sgm_free[qb] = tq
                a["tq"] = tq
                if q == 3:
                    nT_free[b] = t_mm
                    pT_free[b] = t_mm

            def B_fin(j):
                b = j % 2
                a = stA[j]
                t_rs = rms_stats(48 + j, h2[j % 3][:], junk[:], a["tq"])
                DVE.wait(t_rs, ot_free[b])
                t_o = DVE.sig(DVE.e.scalar_tensor_tensor(out=ot[b][:], in0=h2[j % 3][:], scalar=rs[:, 48 + j:49 + j], in1=gfn[:],
                                                         op0=ALU.mult, op1=ALU.mult))
                h2_free[j % 3] = t_o
                ACT.wait(t_o)
                ot_free[b] = dout[b].add(ACT.e.dma_start(out=out[128 * j:128 * j + 128, :], in_=ot[b][:]))

            A_pre(0)
            A_tr(0)
            for j in range(NT):
                B_mm_q(j, 0)
                if j + 1 < NT:
                    A_pre(j + 1)
                B_mm_q(j, 1)
                B_mm_q(j, 2)
                B_mm_q(j, 3)
                if j + 1 < NT:
                    A_tr(j + 1)
                B_fin(j)
            barrier()
        st_wpg.close()
    return nc


_IN_NAMES = ["x", "p", "w_in", "w_out", "sinks", "g_mix", "g_moe", "g_ple", "g_final", "w_grp", "b_grp",
             "w_exp", "b_exp", "w_gate", "w_up", "w_down", "w_ple", "w_ple_gate"]


def make_in_maps(inputs, n_cores=8):
    f = lambda a: np.ascontiguousarray(np.asarray(a, dtype=np.float32))
    shared = {
        "w_in": f(inputs["w_in"][0]), "w_out": f(inputs["w_out"][0]),
        "sinks": f(inputs["sinks"]).reshape(1, 16),
        "g_mix": f(inputs["g_mix"]).reshape(1, D), "g_moe": f(inputs["g_moe"]).reshape(1, D),
        "g_ple": f(inputs["g_ple"]).reshape(1, D), "g_final": f(inputs["g_final"]).reshape(1, D),
        "w_grp": f(inputs["w_grp"][0]), "b_grp": f(inputs["b_grp"]).reshape(1, 4),
        "w_exp": f(inputs["w_exp"][0]).reshape(D, 32), "b_exp": f(inputs["b_exp"]).reshape(1, 32),
        "w_gate": f(inputs["w_gate"][0]), "w_up": f(inputs["w_up"][0]), "w_down": f(inputs["w_down"][0]),
        "w_ple": f(inputs["w_ple"][0]), "w_ple_gate": f(inputs["w_ple_gate"][0]),
    }
    xs = f(inputs["x"])
    ps = f(inputs["p"])
    maps = []
    for c in range(n_cores):
        m = dict(shared)
        m["x"] = xs[c]
        m["p"] = ps[0, c]
        maps.append(m)
    return maps


def kernel(**inputs):
    nc = build()
    in_maps = make_in_maps(inputs)
    res = run_bass_kernel_spmd(nc, in_maps, core_ids=list(range(8)))
    return np.stack([r["out"] for r in res.results], axis=0).astype(np.float32)
```

```python
import numpy as np
from contextlib import ExitStack
import concourse.bass as bass
import concourse.mybir as mybir
from concourse.bass_utils import run_bass_kernel_spmd

F32 = mybir.dt.float32
BF16 = mybir.dt.bfloat16
I32 = mybir.dt.int32
AF = mybir.ActivationFunctionType
ALU = mybir.AluOpType
AX = mybir.AxisListType

S = 2048
D = 2048
NT = 16
NE = 32
CAP = 256
NEGV = -30000.0
BIGV = 1.0e4
EPS = 1e-6


class Eng:
    def __init__(self, nc, es, eng, name):
        self.e = eng
        self.sem = es.enter_context(nc.semaphore("sem_" + name))
        self.cnt = 0
        self.seen = {}

    def wait(self, *toks):
        for t in toks:
            if t is None:
                continue
            if isinstance(t, list):
                self.wait(*t)
                continue
            sem, val = t
            k = id(sem)
            if self.seen.get(k, 0) >= val:
                continue
            self.e.wait_ge(sem, val)
            self.seen[k] = val

    def sig(self, inst):
        self.cnt += 1
        inst.then_inc(self.sem, 1)
        return (self.sem, self.cnt)


class DSem:
    def __init__(self, nc, es, name, reg):
        self.sem = es.enter_context(nc.semaphore("dma_" + name))
        self.cnt = 0
        reg.append(self)

    def add(self, inst):
        self.cnt += 16
        inst.then_inc(self.sem, 16)
        return (self.sem, self.cnt)

    def tok(self):
        return (self.sem, self.cnt) if self.cnt else None


def build(stage=99):
    nc = bass.Bass("TRN2", target_bir_lowering=False)

    def din(n, s, d=F32):
        return nc.dram_tensor(n, s, d, kind="ExternalInput").ap()

    x = din("x", [S, D])
    p_in = din("p", [S, 256])
    w_in = din("w_in", [D, 4352])
    w_out = din("w_out", [D, D])
    sinks = din("sinks", [1, 16])
    g_mix = din("g_mix", [1, D])
    g_moe = din("g_moe", [1, D])
    g_ple = din("g_ple", [1, D])
    g_final = din("g_final", [1, D])
    w_grp = din("w_grp", [D, 4])
    b_grp = din("b_grp", [1, 4])
    w_exp = din("w_exp", [D, 32])
    b_exp = din("b_exp", [1, 32])
    w_gate = din("w_gate", [NE, D, 256])
    w_up = din("w_up", [NE, D, 256])
    w_down = din("w_down", [NE, 256, D])
    w_ple = din("w_ple", [256, D])
    w_pg = din("w_ple_gate", [D, D])
    out = nc.dram_tensor("out", [S, D], F32, kind="ExternalOutput").ap()
    H1 = nc.dram_tensor("H1", [S, D], F32, kind="Internal").ap()
    Mscr = nc.dram_tensor("Mscr", [S, D], BF16, kind="Internal").ap()
    Xg = nc.dram_tensor("Xg", [NE * CAP, D], BF16, kind="Internal").ap()
    Y = nc.dram_tensor("Y", [NE * CAP, D], BF16, kind="Internal").ap()
    WGU = nc.dram_tensor("WGU", [NE, 128, 16 * 512], BF16, kind="Internal").ap()
    WDN = nc.dram_tensor("WDN", [NE, 128, 2 * D], BF16, kind="Internal").ap()
    WO = nc.dram_tensor("WO", [128, 16, D], BF16, kind="Internal").ap()
    WPG = nc.dram_tensor("WPG", [128, 16, D], BF16, kind="Internal").ap()

    es = ExitStack()
    with es:
        PE = Eng(nc, es, nc.tensor, "pe")
        ACT = Eng(nc, es, nc.scalar, "act")
        DVE = Eng(nc, es, nc.vector, "dve")
        POOL = Eng(nc, es, nc.gpsimd, "pool")
        SP = Eng(nc, es, nc.sync, "sp")
        ENGS = [PE, ACT, DVE, POOL, SP]
        dsems = []
        bar_sem = es.enter_context(nc.semaphore("bar"))
        bar_cnt = [0]

        def mk_dsem(name):
            return DSem(nc, es, name, dsems)

        def barrier():
            for ds in dsems:
                SP.wait(ds.tok())
            bar_cnt[0] += len(ENGS)
            for E in ENGS:
                E.e.drain().then_inc(bar_sem, 1)
            for E in ENGS:
                E.e.wait_ge(bar_sem, bar_cnt[0])

        d_pre_wo = DSem(nc, es, "pre_wo", [])
        d_pre_ex = [DSem(nc, es, "pre_ex%d" % g_, []) for g_ in range(8)]
        d_pre_pg = DSem(nc, es, "pre_pg", [])
        bg_list = []
        for kc in range(16):
            bg_list.append((d_pre_wo, WO[:, kc, :], w_out[128 * kc:128 * kc + 128, :]))
        for kc in range(16):
            bg_list.append((d_pre_pg, WPG[:, kc, :], w_pg[128 * kc:128 * kc + 128, :]))
        for e in range(NE):
            wv = WGU[e].rearrange("p (c f) -> p c f", c=16)
            bg_list.append((d_pre_ex[e // 4], wv[:, :, 0:256], w_gate[e].rearrange("(c p) f -> p c f", p=128)))
            bg_list.append((d_pre_ex[e // 4], wv[:, :, 256:512], w_up[e].rearrange("(c p) f -> p c f", p=128)))
            bg_list.append((d_pre_ex[e // 4], WDN[e].rearrange("p (c d) -> p c d", c=2),
                            w_down[e].rearrange("(c p) d -> p c d", p=128)))
        bg_pos = [0]

        def bg_issue(n):
            for _ in range(n):
                if bg_pos[0] >= len(bg_list):
                    return
                ds, o, i = bg_list[bg_pos[0]]
                bg_pos[0] += 1
                ds.add(POOL.e.dma_start(out=o, in_=i))

        def sb(name, shape, dt, stack=es):
            return stack.enter_context(nc.sbuf_tensor(name, shape, dt))

        def pst(name, shape, dt, stack):
            return stack.enter_context(nc.psum_tensor(name, shape, dt))

        ident_f = sb("ident_f", [128, 128], F32)
        ident_b = sb("ident_b", [128, 128], BF16)
        ones_b = sb("ones_b", [128, 128], BF16)
        Utri = sb("Utri", [128, 128], BF16)
        DT = sb("DT", [128, 256], F32)
        NEG_A = sb("NEG_A", [128, 256], F32)
        NEG_B = sb("NEG_B", [128, 256], F32)
        es_t = sb("es_t", [128, 16], F32)
        sk_t = sb("sk_t", [128, 16], F32)
        ss = sb("ss", [128, 64], F32)
        sd = sb("sd", [128, 64], F32)
        rs = sb("rs", [128, 64], F32)
        d_c = mk_dsem("const")
        lg_all = sb("lg_all", [128, NT, 36], F32)
        dst_i = sb("dst_i", [128, 2, NT], I32)
        wts = sb("wts", [128, 2, NT], F32)

        g = POOL.e
        g.memset(ident_f[:], 1.0)
        g.affine_select(out=ident_f[:], in_=ident_f[:], pattern=[[-1, 128]], compare_op=ALU.is_equal,
                        fill=0.0, base=0, channel_multiplier=1)
        g.memset(ones_b[:], 1.0)
        g.memset(Utri[:], 1.0)
        g.affine_select(out=Utri[:], in_=Utri[:], pattern=[[1, 128]], compare_op=ALU.is_gt,
                        fill=0.0, base=0, channel_multiplier=-1)
        g.iota(DT[:, 0:128], pattern=[[1, 128]], base=128, channel_multiplier=-1,
               allow_small_or_imprecise_dtypes=True)
        g.iota(DT[:, 128:256], pattern=[[1, 128]], base=0, channel_multiplier=-1,
               allow_small_or_imprecise_dtypes=True)
        g.memset(NEG_A[:], 0.0)
        g.memset(NEG_B[:], 0.0)
        g.affine_select(out=NEG_A[:, 0:128], in_=NEG_A[:, 0:128], pattern=[[-1, 128]], compare_op=ALU.is_ge,
                        fill=NEGV, base=0, channel_multiplier=1)
        g.affine_select(out=NEG_A[:, 128:256], in_=NEG_A[:, 128:256], pattern=[[1, 128]], compare_op=ALU.is_ge,
                        fill=NEGV, base=0, channel_multiplier=-1)
        g.affine_select(out=NEG_B[:, 0:128], in_=NEG_B[:, 0:128], pattern=[[-1, 128]], compare_op=ALU.is_ge,
                        fill=NEGV, base=-1, channel_multiplier=1)
        t_pc = POOL.sig(g.affine_select(out=NEG_B[:, 128:256], in_=NEG_B[:, 128:256], pattern=[[1, 128]],
                                        compare_op=ALU.is_ge, fill=NEGV, base=0, channel_multiplier=-1))
        d_sk = mk_dsem("sk")
        bc_reg = POOL.e.to_reg(NE * CAP - 1)
        t_sk = d_sk.add(SP.e.dma_start(out=sk_t[:], in_=sinks.to_broadcast([128, 16])))
        DVE.wait(t_pc)
        t_idb = DVE.sig(DVE.e.tensor_copy(out=ident_b[:], in_=ident_f[:]))
        ACT.wait(t_sk)
        t_es = ACT.sig(ACT.e.activation(out=es_t[:], in_=sk_t[:], func=AF.Exp))
        t_const = [t_pc, t_idb, t_es]

        def rms_stats(col, src, junk, src_tok):
            ACT.wait(src_tok)
            t1 = ACT.sig(ACT.e.activation(out=junk, in_=src, func=AF.Square, accum_out=ss[:, col:col + 1]))
            ACT.wait(t1)
            t2 = ACT.sig(ACT.e.activation(out=sd[:, col:col + 1], in_=ss[:, col:col + 1], func=AF.Sqrt,
                                          scale=1.0 / D, bias=EPS))
            DVE.wait(t2)
            t3 = DVE.sig(DVE.e.reciprocal(out=rs[:, col:col + 1], in_=sd[:, col:col + 1]))
            return t3

        dbg = None
        if stage < 99:
            dbg = nc.dram_tensor("dbg", [S, D], F32, kind="ExternalOutput").ap()

        st_mixed = ExitStack()
        mixedT = sb("mixedT", [128, 16, S], BF16, st_mixed)
        st_aT = ExitStack()
        aT = sb("aT", [128, 16, S], BF16, st_aT)

        with ExitStack() as s1:
            xt = [sb("xt%d" % i, [128, D], F32, s1) for i in range(2)]
            at = [sb("at%d" % i, [128, D], BF16, s1) for i in range(2)]
            gm = sb("gm", [128, D], F32, s1)
            junk = sb("junk", [128, D], BF16, s1)
            psT = pst("psT1", [128, 2, 1024], BF16, s1)
            dx = [mk_dsem("x%d" % i) for i in range(2)]
            d_g = mk_dsem("gmix")
            t_g = d_g.add(SP.e.dma_start(out=gm[:], in_=g_mix.to_broadcast([128, D])))
            xt_free = [None, None]
            at_free = [None, None]
            psT_free = [None, None]
            p1 = {}

            def pre1(j):
                b = j % 2
                SP.wait(xt_free[b])
                t_x = dx[b].add(SP.e.dma_start(out=xt[b][:], in_=x[128 * j:128 * j + 128, :]))
                t_rs = rms_stats(j, xt[b][:], junk[:], t_x)
                DVE.wait(t_rs, t_x, t_g, at_free[b])
                t_a = DVE.sig(DVE.e.scalar_tensor_tensor(out=at[b][:], in0=xt[b][:], scalar=rs[:, j:j + 1],
                                                         in1=gm[:], op0=ALU.mult, op1=ALU.mult))
                xt_free[b] = t_a
                p1[j] = t_a

            def post1(j):
                b = j % 2
                PE.wait(p1[j], t_idb)
                t_tr = [None, None]
                for c in range(16):
                    if c % 8 == 0:
                        PE.wait(psT_free[c // 8])
                    ins = PE.e.transpose(out=psT[:, c // 8, (c % 8) * 128:(c % 8) * 128 + 128],
                                         in_=at[b][:, c * 128:c * 128 + 128], identity=ident_b[:])
                    if c % 8 == 7:
                        t_tr[c // 8] = PE.sig(ins)
                at_free[b] = t_tr[1]
                ACT.wait(t_tr[0])
                psT_free[0] = ACT.sig(ACT.e.activation(
                    out=aT[:, 0:8, 128 * j:128 * j + 128],
                    in_=psT[:, 0, :].rearrange("p (c t) -> p c t", c=8), func=AF.Copy))
                DVE.wait(t_tr[1])
                psT_free[1] = DVE.sig(DVE.e.tensor_copy(
                    out=aT[:, 8:16, 128 * j:128 * j + 128],
                    in_=psT[:, 1, :].rearrange("p (c t) -> p c t", c=8)))

            pre1(0)
            for j in range(NT):
                if j + 1 < NT:
                    pre1(j + 1)
                post1(j)
            t_aT = [psT_free[0], psT_free[1]]
            barrier()

        if stage == 1:
            with ExitStack() as sd_:
                tmp = sb("dbgtmp", [128, S], F32, sd_)
                dd = mk_dsem("dbg")
                for c in range(16):
                    DVE.wait(dd.tok())
                    t = DVE.sig(DVE.e.tensor_copy(out=tmp[:], in_=aT[:, c, :]))
                    SP.wait(t)
                    dd.add(SP.e.dma_start(out=dbg[128 * c:128 * c + 128, :], in_=tmp[:]))
                SP.wait(dd.tok())
                barrier()
            st_aT.close()
            st_mixed.close()
            return nc

        d_wo2 = mk_dsem("wout_pf")
        with ExitStack() as s2:
            slab = [sb("slab%d" % i, [128, 16, 128], BF16, s2) for i in range(4)]
            dW = [mk_dsem("slab%d" % i) for i in range(4)]
            QT = sb("QT", [128, S], BF16, s2)
            KT = sb("KT", [128, S], BF16, s2)
            VT = sb("VT", [128, S], BF16, s2)
            Vs = [sb("Vs%d" % i, [128, 16, 128], BF16, s2) for i in range(2)]
            ODacc = sb("ODacc", [128, 2, S], F32, s2)
            PT = [sb("PT%d" % i, [128, 512], BF16, s2) for i in range(2)]
            MB2 = [[sb("MB_%d_%d" % (i, r), [128, 512], BF16, s2) for r in range(3)] for i in range(2)]
            btmp = [sb("btmp%d" % i, [128, 2, 128], F32, s2) for i in range(2)]
            psP = pst("psP", [128, 2, 512], F32, s2)
            psT = pst("psT2", [128, 1, 1024], BF16, s2)
            psS = pst("psS", [128, 2, 512], F32, s2)
            psO = pst("psO", [128, 2, 512], F32, s2)

            st = dict(slab_n=0, slab_free=[None] * 4, psP_n=0, psP_free=[None, None],
                      psT_free=None, vs_n=0, vs_free=[None, None], mb_n=0, mb_free=[None, None])
            buf_tok = {}

            def mat_job(pieces, dst, scale, dst_free):
                k = st["slab_n"] % 4
                st["slab_n"] += 1
                POOL.wait(st["slab_free"][k])
                off = 0
                tok = None
                for (c0, n) in pieces:
                    tok = dW[k].add(POOL.e.dma_start(
                        out=slab[k][:, :, off:off + n],
                        in_=w_in[:, c0:c0 + n].rearrange("(c p) n -> p c n", p=128)))
                    off += n
                assert off == 128
                bg_issue(3)
                last = None
                for t4 in range(4):
                    bk = st["psP_n"] % 2
                    st["psP_n"] += 1
                    PE.wait(tok, st["psP_free"][bk], t_aT)
                    for kc in range(16):
                        ins = PE.e.matmul(psP[:, bk, :], lhsT=slab[k][:, kc, :], rhs=aT[:, kc, 512 * t4:512 * t4 + 512],
                                          start=(kc == 0), stop=(kc == 15))
                    t_mm = PE.sig(ins)
                    if bk == 0:
                        ACT.wait(t_mm, dst_free)
                        last = ACT.sig(ACT.e.activation(out=dst[:, 512 * t4:512 * t4 + 512], in_=psP[:, bk, :],
                                                        func=AF.Copy, scale=float(scale)))
                    else:
                        DVE.wait(t_mm, dst_free)
                        last = DVE.sig(DVE.e.tensor_scalar(out=dst[:, 512 * t4:512 * t4 + 512], in0=psP[:, bk, :],
                                                           scalar1=float(scale), scalar2=None, op0=ALU.mult))
                    st["psP_free"][bk] = last
                    if t4 == 2:
                        prev = last
                st["slab_free"][k] = t_mm
                return [prev, last]

            def v_layout(src, src_tok, s):
                vi = st["vs_n"] % 2
                st["vs_n"] += 1
                V = Vs[vi]
                toks = []
                for half in range(2):
                    PE.wait(src_tok, st["psT_free"], t_idb)
                    for i8 in range(8):
                        slot = half * 8 + i8
                        b_, rho = slot // s, slot % s
                        start = s * 128 * b_ + rho
                        ins = PE.e.transpose(out=psT[:, 0, i8 * 128:i8 * 128 + 128],
                                             in_=src[:, start:start + 127 * s + 1:s], identity=ident_b[:])
                    t_tr = PE.sig(ins)
                    buf_tok["vt_free"] = t_tr
                    E = ACT if half == 0 else DVE
                    E.wait(t_tr, st["vs_free"][vi])
                    if half == 0:
                        t_e = ACT.sig(ACT.e.activation(out=V[:, 0:8, :],
                                                       in_=psT[:, 0, :].rearrange("p (c t) -> p c t", c=8),
                                                       func=AF.Copy))
                    else:
                        t_e = DVE.sig(DVE.e.tensor_copy(out=V[:, 8:16, :],
                                                        in_=psT[:, 0, :].rearrange("p (c t) -> p c t", c=8)))
                    st["psT_free"] = t_e
                    toks.append(t_e)
                return vi, toks

            def make_mask(coefs, negt):
                par = st["mb_n"] % 2
                st["mb_n"] += 1
                DVE.wait(st["mb_free"][par], t_const)
                t = None
                for r, c in enumerate(coefs):
                    for u in range(2):
                        t = DVE.sig(DVE.e.scalar_tensor_tensor(out=MB2[par][r][:, 256 * u:256 * u + 256], in0=DT[:],
                                                               scalar=-float(c), in1=negt[:], op0=ALU.mult,
                                                               op1=ALU.add))
                return par, t

            batches = []

            def unit_cols(s, b_, rho):
                start = s * 128 * b_ + rho
                return slice(start, start + 127 * s + 1, s)

            for h in range(8):
                slope = 2.0 ** (-(h + 1))
                hstate = {}

                def pre_head(h=h, hstate=hstate, slope=slope):
                    qk_free = buf_tok.get("qk_free")
                    vt_free = buf_tok.get("vt_free")
                    hstate["q"] = mat_job([(h * 128, 128)], QT, 128.0 ** -0.5, qk_free)
                    hstate["k"] = mat_job([(1024 + h * 128, 128)], KT, 1.0, qk_free)
                    hstate["v"] = mat_job([(2048 + h * 128, 128)], VT, 1.0, vt_free)
                    hstate["mb"] = make_mask([slope * 1, slope * 4, slope * 16], NEG_A)

                for r, s in enumerate((1, 4, 16)):
                    units = [(b_, rho) for b_ in range(16 // s) for rho in range(s)]
                    bstate = {}

                    def pre_branch(s=s, hstate=hstate, bstate=bstate):
                        bstate["v"] = v_layout(VT, hstate["v"], s)

                    for ib in range(8):
                        us = units[2 * ib:2 * ib + 2]
                        pres = []
                        if r == 0 and ib == 0:
                            pres.append(pre_head)
                        if ib == 0:
                            pres.append(pre_branch)
                        batches.append(dict(kind="A", h=h, r=r, s=s, units=us, pres=pres, hstate=hstate,
                                            bstate=bstate, first=(r == 0), last_of_head=(r == 2 and ib == 7),
                                            last_of_branch=(ib == 7), rows=slice(0, 128)))

            kvstates = [{}, {}]
            for c in range(8):
                gkv = c // 4
                hstate = {}
                kvstate = kvstates[gkv]

                def pre_kv(gkv=gkv, kvstate=kvstate):
                    qk_free = buf_tok.get("qk_free")
                    vt_free = buf_tok.get("vt_free")
                    k0 = 4096 + gkv * 64
                    v0 = 4224 + gkv * 64
                    kvstate["k"] = mat_job([(k0, 64), (k0, 64)], KT, 1.0, qk_free)
                    kvstate["vT"] = mat_job([(v0, 64), (v0, 64)], VT, 1.0, vt_free)
                    kvstate["v"] = v_layout(VT, kvstate["vT"], 1)

                def pre_chunk(c=c, hstate=hstate, kvstate=kvstate):
                    qk_free = buf_tok.get("qk_free")
                    hstate["q"] = mat_job([(3072 + c * 128, 128)], QT, 64.0 ** -0.5, qk_free)
                    if c == 7:
                        SP.wait(st["slab_free"][(st["slab_n"] - 1) % 4], d_pre_wo.tok())
                        for kc4 in range(4):
                            buf_tok["t_w"] = d_wo2.add(SP.e.dma_start(out=aT[:, 4 * kc4:4 * kc4 + 4, :],
                                                                       in_=WO[:, 4 * kc4:4 * kc4 + 4, :]))
                    hstate["k"] = kvstate["k"]
                    sl = [2.0 ** (-8.0 * (2 * c + hh + 1) / 16.0) for hh in range(2)]
                    hstate["mb"] = make_mask(sl, NEG_B)

                for hh in range(2):
                    for ib in range(8):
                        us = [(2 * ib, 0), (2 * ib + 1, 0)]
                        pres = []
                        if hh == 0 and ib == 0:
                            if c % 4 == 0:
                                pres.append(pre_kv)
                            pres.append(pre_chunk)
                        batches.append(dict(kind="B", h=2 * c + hh, c=c, hh=hh, r=hh, s=1, units=us, pres=pres,
                                            hstate=hstate, bstate=kvstate, first=True, last_of_head=False,
                                            last_of_branch=(c % 4 == 3 and hh == 1 and ib == 7),
                                            last_q=(hh == 1 and ib == 7),
                                            rows=slice(64 * hh, 64 * hh + 64)))

            if stage == 2:
                batches = [bb for bb in batches if bb["kind"] == "A" and bb["h"] < 1]

            NB = len(batches)
            S_free = [None, None]
            PT_free = [None, None]
            O_free = [None, None]
            tS = [None] * NB
            tP = [None] * NB

            def emit_qk(n):
                bb = batches[n]
                for f in bb["pres"]:
                    f()
                pb = n % 2
                hs = bb["hstate"]
                rows = bb["rows"]
                par, t_mb = hs["mb"]
                PE.wait(S_free[pb], hs["q"], hs["k"], t_mb, t_idb)
                first = True
                for u, (b_, rho) in enumerate(bb["units"]):
                    qc = unit_cols(bb["s"], b_, rho)
                    if b_ > 0:
                        kc = unit_cols(bb["s"], b_ - 1, rho)
                        PE.e.matmul(psS[:, pb, 256 * u:256 * u + 128], lhsT=KT[rows, kc], rhs=QT[rows, qc],
                                    start=first, stop=False)
                        first = False
                    PE.e.matmul(psS[:, pb, 256 * u + 128:256 * u + 256], lhsT=KT[rows, qc], rhs=QT[rows, qc],
                                start=first, stop=False)
                    first = False
                ins = PE.e.matmul(psS[:, pb, :], lhsT=ident_b[:], rhs=MB2[par][bb["r"]][:], start=False, stop=True)
                tS[n] = PE.sig(ins)
                if bb["last_of_head"] or bb.get("last_q"):
                    buf_tok["qk_free"] = tS[n]
                if bb.get("last_q") or bb["last_of_head"]:
                    st["mb_free"][par] = tS[n]

            def emit_exp(n):
                pb = n % 2
                ACT.wait(tS[n], PT_free[pb])
                tP[n] = ACT.sig(ACT.e.activation(out=PT[pb][:], in_=psS[:, pb, :], func=AF.Exp))
                S_free[pb] = tP[n]

            def emit_pv(n):
                bb = batches[n]
                pb = n % 2
                vi, vtoks = bb["bstate"]["v"]
                V = Vs[vi]
                s = bb["s"]
                PE.wait(tP[n], O_free[pb], vtoks)
                first = True
                ins = None
                for u, (b_, rho) in enumerate(bb["units"]):
                    slot = b_ * s + rho
                    parts = []
                    if b_ > 0:
                        parts.append(((b_ - 1) * s + rho, PT[pb][:, 256 * u:256 * u + 128]))
                    parts.append((slot, PT[pb][:, 256 * u + 128:256 * u + 256]))
                    for i, (sl, pt) in enumerate(parts):
                        PE.e.matmul(psO[:, pb, 256 * u:256 * u + 128], lhsT=V[:, sl, :], rhs=pt,
                                    start=first, stop=False)
                        first = False
                    for i, (sl, pt) in enumerate(parts):
                        ins = PE.e.matmul(psO[:, pb, 256 * u + 128:256 * u + 256], lhsT=ones_b[:], rhs=pt,
                                          start=False, stop=(u == 1 and i == len(parts) - 1))
                t_o = PE.sig(ins)
                PT_free[pb] = t_o
                if bb["last_of_branch"]:
                    st["vs_free"][vi] = t_o
                return t_o

            def emit_evac(n, t_o):
                bb = batches[n]
                pb = n % 2
                s = bb["s"]
                (b0, r0), (b1, r1) = bb["units"]
                if bb["kind"] == "A":
                    if s == 1:
                        oap = ODacc[:, :, 128 * b0:128 * b0 + 256].rearrange("p o (u q) -> p o u q", u=2)
                    else:
                        lo = s * 128 * b0
                        oap = ODacc[:, :, lo:lo + 128 * s].rearrange("p o (q r) -> p o r q", r=s)[:, :, r0:r0 + 2, :]
                    iap = psO[:, pb, :].rearrange("p (u o q) -> p o u q", u=2, o=2)
                    DVE.wait(t_o, buf_tok.get("od_free"), buf_tok.get("od_last"))
                    if bb["first"]:
                        t_e = DVE.sig(DVE.e.tensor_copy(out=oap, in_=iap))
                    else:
                        t_e = DVE.sig(DVE.e.tensor_tensor(out=oap, in0=iap, in1=oap, op=ALU.add))
                    buf_tok["od_last"] = t_e
                    O_free[pb] = t_e
                    if bb["last_of_head"]:
                        h = bb["h"]
                        ACT.wait(t_e)
                        t0 = ACT.sig(ACT.e.activation(out=ODacc[:, 1, :], in_=ODacc[:, 1, :], func=AF.Ln))
                        ACT.wait(t0)
                        t1 = ACT.sig(ACT.e.activation(out=ODacc[:, 1, :], in_=ODacc[:, 1, :], func=AF.Exp, scale=-1.0))
                        DVE.wait(t1)
                        t2 = DVE.sig(DVE.e.tensor_tensor(out=mixedT[:, h, :], in0=ODacc[:, 0, :], in1=ODacc[:, 1, :],
                                                         op=ALU.mult))
                        buf_tok["od_free"] = t2
                        buf_tok["mixed_last"] = t2
                else:
                    rows = bb["rows"]
                    h = bb["h"]
                    c = bb["c"]
                    iv = psO[rows, pb, :].rearrange("p (u o q) -> p o u q", u=2, o=2)
                    tb = btmp[pb]
                    ACT.wait(t_o, t_es, buf_tok.get("btmp_free%d" % pb))
                    ta = ACT.sig(ACT.e.activation(out=tb[rows, :, :], in_=iv[:, 1, :, :], func=AF.Ln,
                                                  bias=es_t[rows, h:h + 1], scale=1.0))
                    ACT.wait(ta)
                    tb_ = ACT.sig(ACT.e.activation(out=tb[rows, :, :], in_=tb[rows, :, :], func=AF.Exp, scale=-1.0))
                    DVE.wait(tb_)
                    t_e = DVE.sig(DVE.e.tensor_tensor(
                        out=mixedT[rows, 8 + c, 128 * b0:128 * b0 + 256].rearrange("p (u q) -> p u q", u=2),
                        in0=iv[:, 0, :, :], in1=tb[rows, :, :], op=ALU.mult))
                    O_free[pb] = t_e
                    buf_tok["btmp_free%d" % pb] = t_e
                    buf_tok["mixed_last"] = t_e

            emit_qk(0)
            emit_exp(0)
            for n in range(NB):
                if n + 1 < NB:
                    emit_qk(n + 1)
                    emit_exp(n + 1)
                t_o = emit_pv(n)
                emit_evac(n, t_o)
            barrier()

        if stage in (2, 22):
            with ExitStack() as sd_:
                tmp = sb("dbgtmp", [128, S], F32, sd_)
                dd = mk_dsem("dbg")
                for c in range(16):
                    DVE.wait(dd.tok())
                    t = DVE.sig(DVE.e.tensor_copy(out=tmp[:], in_=mixedT[:, c, :]))
                    SP.wait(t)
                    dd.add(SP.e.dma_start(out=dbg[128 * c:128 * c + 128, :], in_=tmp[:]))
                SP.wait(dd.tok())
                barrier()
            st_aT.close()
            st_mixed.close()
            return nc

        wout = aT
        with ExitStack() as s3:
            xt = [sb("xt3_%d" % i, [128, D], F32, s3) for i in range(2)]
            h1 = [sb("h1_%d" % i, [128, D], F32, s3) for i in range(2)]
            mf = sb("mf", [128, D], F32, s3)
            mhl = sb("mhl", [128, 2, D], BF16, s3)
            mhlT = sb("mhlT", [128, 2, 16, 128], BF16, s3)
            Whl = sb("Whl", [128, 2, 16, 36], BF16, s3)
            mb1 = sb("mb1", [128, D], BF16, s3)
            mb = [mb1, mb1]
            gmo = sb("gmo", [128, D], F32, s3)
            junk = mb1
            Wr = mf[:, 1024:1024 + 16 * 36].rearrange("p (c n) -> p c n", c=16)
            bias_bc = sb("bias_bc", [128, 36], F32, s3)
            psH = pst("psH", [128, 2, 2, 512], F32, s3)
            psF = pst("psF", [128, 2, 1024], BF16, s3)
            psZ = pst("psZ", [128, 512], F32, s3)
            d_w = mk_dsem("wout")
            dx = [mk_dsem("x3_%d" % i) for i in range(2)]
            dh = [mk_dsem("h1s%d" % i) for i in range(2)]
            dm1 = mk_dsem("ms")
            dm = [dm1, dm1]
            t_w = buf_tok["t_w"]
            t_c3 = d_c.add(SP.e.dma_start(out=gmo[:], in_=g_moe.to_broadcast([128, D])))
            t_c3 = d_c.add(SP.e.dma_start(out=Wr[:, :, 0:4], in_=w_grp.rearrange("(c p) n -> p c n", p=128)))
            t_c3 = d_c.add(SP.e.dma_start(out=Wr[:, :, 4:36], in_=w_exp.rearrange("(c p) n -> p c n", p=128)))
            t_c3 = d_c.add(SP.e.dma_start(out=bias_bc[:, 0:4], in_=b_grp.to_broadcast([128, 4])))
            t_c3 = d_c.add(SP.e.dma_start(out=bias_bc[:, 4:36], in_=b_exp.to_broadcast([128, 32])))

            DVE.wait(t_c3)
            t_ = DVE.sig(DVE.e.tensor_copy(out=Whl[:, 0, :, :], in_=Wr))
            DVE.wait(t_)
            Wtmp = mf[:, 0:16 * 36].rearrange("p (c n) -> p c n", c=16)
            t_ = DVE.sig(DVE.e.tensor_tensor(out=Wtmp, in0=Wr, in1=Whl[:, 0, :, :], op=ALU.subtract))
            DVE.wait(t_)
            t_whl = DVE.sig(DVE.e.tensor_copy(out=Whl[:, 1, :, :], in_=Wtmp))
            mf_free0 = t_whl
            psH_free = [None, None]
            psF_free = [None, None]
            psZ_free = None
            mhl_free = None
            xt_free = [None, None]
            h1_free = [None, None]
            mb_free = [None, None]
            mf_free = t_whl
            mfT_free = None
            t_hmm = {}

            def op_mm(j):
                b = j % 2
                bg_issue(4)
                SP.wait(xt_free[b])
                t_x = dx[b].add(SP.e.dma_start(out=xt[b][:], in_=x[128 * j:128 * j + 128, :]))
                mm = []
                for half in range(2):
                    PE.wait(psH_free[half], t_w)
                    for nn in range(2):
                        for kc in range(16):
                            ins = PE.e.matmul(psH[:, half, nn, :], lhsT=mixedT[:, kc, 128 * j:128 * j + 128],
                                              rhs=wout[:, kc, 1024 * half + 512 * nn:1024 * half + 512 * nn + 512],
                                              start=(kc == 0), stop=(kc == 15))
                    mm.append(PE.sig(ins))
                t_hmm[j] = dict(t_x=t_x, mm=mm)

            def op_add(j):
                b = j % 2
                a = t_hmm[j]
                toks = []
                for half in range(2):
                    DVE.wait(a["mm"][half], a["t_x"], h1_free[b])
                    t_h = DVE.sig(DVE.e.tensor_tensor(out=h1[b][:, 1024 * half:1024 * half + 1024],
                                                      in0=psH[:, half, :, :].rearrange("p n f -> p (n f)"),
                                                      in1=xt[b][:, 1024 * half:1024 * half + 1024], op=ALU.add))
                    psH_free[half] = t_h
                    toks.append(t_h)
                xt_free[b] = toks[1]
                a["toks"] = toks

            def rest_a(j):
                nonlocal mf_free, mfT_free, psZ_free
                b = j % 2
                toks = t_hmm[j]["toks"]
                SP.wait(toks)
                t_hs = dh[b].add(SP.e.dma_start(out=H1[128 * j:128 * j + 128, :], in_=h1[b][:]))
                ACT.wait(mb_free)
                t_rs = rms_stats(16 + j, h1[b][:], junk[:], toks)
                DVE.wait(toks, t_c3, mf_free)
                t_mf = DVE.sig(DVE.e.tensor_tensor(out=mf[:], in0=h1[b][:], in1=gmo[:], op=ALU.mult))
                ACT.wait(t_mf, t_rs, mb_free)
                t_m = ACT.sig(ACT.e.activation(out=mb[b][:], in_=mf[:], func=AF.Copy, scale=rs[:, 16 + j:17 + j]))
                SP.wait(t_m)
                mb_free[b] = dm[b].add(SP.e.dma_start(out=Mscr[128 * j:128 * j + 128, :], in_=mb[b][:]))
                DVE.wait(t_mf, mhl_free)
                t_hi = DVE.sig(DVE.e.tensor_copy(out=mhl[:, 0, :], in_=mf[:]))
                DVE.wait(t_hi)
                t_lo = DVE.sig(DVE.e.tensor_tensor(out=mhl[:, 1, :], in0=mf[:], in1=mhl[:, 0, :], op=ALU.subtract))
                t_hmm[j].update(t_hs=t_hs, t_rs=t_rs, t_mf=t_mf, t_m=t_m, t_lo=t_lo)

            def rest_b(j):
                nonlocal mf_free, mfT_free, psZ_free, mhl_free
                b = j % 2
                a = t_hmm[j]
                t_hs, t_rs, t_mf, t_m, t_lo = a["t_hs"], a["t_rs"], a["t_mf"], a["t_m"], a["t_lo"]
                evs = []
                for q4 in range(4):
                    fb = q4 % 2
                    hl, ch = q4 // 2, q4 % 2
                    PE.wait(t_lo, psF_free[fb], t_idb)
                    for i8 in range(8):
                        c = 8 * ch + i8
                        ins = PE.e.transpose(out=psF[:, fb, 128 * i8:128 * i8 + 128], in_=mhl[:, hl, 128 * c:128 * c + 128],
                                             identity=ident_b[:])
                    t_tr = PE.sig(ins)
                    E = ACT if fb == 0 else DVE
                    E.wait(t_tr, mfT_free)
                    if fb == 0:
                        t_e = ACT.sig(ACT.e.activation(out=mhlT[:, hl, 8 * ch:8 * ch + 8, :],
                                                       in_=psF[:, fb, :].rearrange("p (c t) -> p c t", c=8),
                                                       func=AF.Copy))
                    else:
                        t_e = DVE.sig(DVE.e.tensor_copy(out=mhlT[:, hl, 8 * ch:8 * ch + 8, :],
                                                        in_=psF[:, fb, :].rearrange("p (c t) -> p c t", c=8)))
                    psF_free[fb] = t_e
                    evs.append(t_e)
                mhl_free = t_tr
                mf_free = [t_lo, t_m]
                h1_free[b] = [t_hs, t_mf, t_rs]
                PE.wait(evs, psZ_free, t_whl)
                n_mm = 0
                for (xa, wa_) in ((0, 0), (1, 0), (0, 1)):
                    for c in range(16):
                        ins = PE.e.matmul(psZ[:, 0:36], lhsT=mhlT[:, xa, c, :], rhs=Whl[:, wa_, c, :],
                                          start=(n_mm == 0), stop=(n_mm == 47))
                        n_mm += 1
                t_z = PE.sig(ins)
                mfT_free = t_z
                DVE.wait(t_z, t_rs)
                psZ_free = DVE.sig(DVE.e.scalar_tensor_tensor(out=lg_all[:, j, :], in0=psZ[:, 0:36],
                                                              scalar=rs[:, 16 + j:17 + j], in1=bias_bc[:],
                                                              op0=ALU.mult, op1=ALU.add))

            op_mm(0)
            op_add(0)
            for j in range(NT):
                if j + 1 < NT:
                    op_mm(j + 1)
                rest_a(j)
                if j + 1 < NT:
                    op_add(j + 1)
                rest_b(j)
            barrier()
        st_aT.close()
        st_mixed.close()

        if stage == 3:
            with ExitStack() as sd_:
                tmp = sb("dbgtmp", [128, S], F32, sd_)
                dd = mk_dsem("dbg")
                for c in range(16):
                    SP.wait(dd.tok())
                    dd.add(SP.e.dma_start(out=tmp[:], in_=H1[128 * c:128 * c + 128, :]))
                    SP.wait(dd.tok())
                    dd.add(SP.e.dma_start(out=dbg[128 * c:128 * c + 128, :], in_=tmp[:]))
                SP.wait(dd.tok())
                barrier()
            return nc

        bg_issue(1000)
        st_wpg = ExitStack()
        wpg = sb("wpg", [128, 16, D], BF16, st_wpg)
        d_wpg = mk_dsem("wpg")
        with ExitStack() as s5:
            NMT = 4
            mt = [sb("mt%d" % i, [128, D], BF16, s5) for i in range(NMT)]
            wgu = [sb("wgu%d" % i, [128, 16, 512], BF16, s5) for i in range(2)]
            wdn = [sb("wdn%d" % i, [128, 2, D], BF16, s5) for i in range(2)]
            xs = [[sb("xs%d_%d" % (i, s_), [128, D], BF16, s5) for s_ in range(2)] for i in range(2)]
            XT = [sb("XT%d" % i, [128, 16, 256], BF16, s5) for i in range(2)]
            sg = sb("sg", [128, 2, 256], F32, s5)
            hidT = sb("hidT", [128, 2, 256], BF16, s5)
            yb = [sb("yb%d" % i, [128, D], BF16, s5) for i in range(2)]
            dmt = [mk_dsem("mt%d" % i) for i in range(NMT)]
            dsc = [mk_dsem("sc%d" % i) for i in range(NMT)]
            dwe = [mk_dsem("we%d" % i) for i in range(2)]
            dxs = [mk_dsem("xs%d" % i) for i in range(2)]
            dys = [mk_dsem("ys%d" % i) for i in range(2)]
            mt_tok = {}
            for j in range(NMT):
                mt_tok[j] = dmt[j].add(SP.e.dma_start(out=mt[j][:], in_=Mscr[128 * j:128 * j + 128, :]))

            with ExitStack() as s4:
                def t3(name, shape, dt=F32):
                    return sb(name, shape, dt, s4)
                gmax = t3("gmax", [128, NT]); goh = t3("goh", [128, NT, 4]); tmp4 = t3("tmp4", [128, NT, 4])
                sume = t3("sume", [128, NT]); grpw = t3("grpw", [128, NT]); pen4 = t3("pen4", [128, NT, 4])
                Lm = t3("Lm", [128, NT, 32]); Lm2 = t3("Lm2", [128, NT, 32])
                m1 = t3("m1", [128, NT]); m2 = t3("m2", [128, NT])
                oh = [t3("oh%d" % k, [128, NT, 32]) for k in range(2)]
                Mk = t3("Mk", [128, NT, 32], BF16)
                pos = t3("pos", [128, NT, 32]); tmp32 = t3("tmp32", [128, NT, 32])
                iota3 = t3("iota3", [128, NT, 32])
                pk = t3("pk", [128, NT]); ek = t3("ek", [128, NT]); dk = t3("dk", [128, NT]); ok = t3("ok", [128, NT])
                d21 = t3("d21", [128, NT]); e21 = t3("e21", [128, NT]); wa = t3("wa", [128, NT]); wb_ = t3("wb_", [128, NT])
                psR = pst("psR", [128, 512], F32, s4)
                t_io = POOL.sig(POOL.e.iota(iota3[:], pattern=[[0, NT], [1, 32]], base=0, channel_multiplier=0,
                                            allow_small_or_imprecise_dtypes=True))
                last = [None]

                def dv(inst_fn, extra=None):
                    DVE.wait(last[0], extra)
                    last[0] = DVE.sig(inst_fn())
                    return last[0]

                Lg = lg_all[:, :, 0:4]
                Le4 = lg_all[:, :, 4:36].rearrange("p j (g e) -> p j g e", g=4)
                V = DVE.e
                dv(lambda: V.tensor_reduce(out=gmax[:], in_=Lg, axis=AX.X, op=ALU.max))
                dv(lambda: V.tensor_tensor(out=goh[:], in0=Lg, in1=gmax[:].to_broadcast([128, NT, 4]), op=ALU.is_equal))
                t = dv(lambda: V.tensor_tensor(out=tmp4[:], in0=Lg, in1=gmax[:].to_broadcast([128, NT, 4]), op=ALU.subtract))
                ACT.wait(t)
                t = ACT.sig(ACT.e.activation(out=tmp4[:], in_=tmp4[:], func=AF.Exp))
                dv(lambda: V.tensor_reduce(out=sume[:], in_=tmp4[:], axis=AX.X, op=ALU.add), t)
                dv(lambda: V.reciprocal(out=grpw[:], in_=sume[:]))
                dv(lambda: V.tensor_scalar(out=pen4[:], in0=goh[:], scalar1=BIGV, scalar2=-BIGV, op0=ALU.mult, op1=ALU.add))
                dv(lambda: V.tensor_tensor(out=Lm[:].rearrange("p j (g e) -> p j g e", g=4), in0=Le4,
                                           in1=pen4[:].to_broadcast([128, NT, 4, 8]), op=ALU.add))
                dv(lambda: V.tensor_reduce(out=m1[:], in_=Lm[:], axis=AX.X, op=ALU.max))
                dv(lambda: V.tensor_tensor(out=oh[0][:], in0=Lm[:], in1=m1[:].to_broadcast([128, NT, 32]), op=ALU.is_equal))
                dv(lambda: V.scalar_tensor_tensor(out=Lm2[:], in0=oh[0][:], scalar=-BIGV, in1=Lm[:], op0=ALU.mult, op1=ALU.add))
                dv(lambda: V.tensor_reduce(out=m2[:], in_=Lm2[:], axis=AX.X, op=ALU.max))
                dv(lambda: V.tensor_tensor(out=oh[1][:], in0=Lm2[:], in1=m2[:].to_broadcast([128, NT, 32]), op=ALU.is_equal))
                t_mk = dv(lambda: V.tensor_tensor(out=Mk[:], in0=oh[0][:], in1=oh[1][:], op=ALU.add))
                t = dv(lambda: V.tensor_tensor(out=d21[:], in0=m2[:], in1=m1[:], op=ALU.subtract))
                ACT.wait(t)
                t = ACT.sig(ACT.e.activation(out=e21[:], in_=d21[:], func=AF.Exp))
                dv(lambda: V.tensor_scalar(out=wa[:], in0=e21[:], scalar1=1.0, scalar2=None, op0=ALU.add), t)
                dv(lambda: V.reciprocal(out=wa[:], in_=wa[:]))
                dv(lambda: V.tensor_tensor(out=wb_[:], in0=wa[:], in1=e21[:], op=ALU.mult))
                dv(lambda: V.tensor_tensor(out=wa[:], in0=wa[:], in1=grpw[:], op=ALU.mult))
                dv(lambda: V.tensor_tensor(out=wb_[:], in0=wb_[:], in1=grpw[:], op=ALU.mult))
                PE.wait(t_mk, t_pc)
                for j in range(NT):
                    PE.e.matmul(psR[:, 32 * j:32 * j + 32], lhsT=Utri[:], rhs=Mk[:, j, :], start=(j == 0), stop=False)
                for j in range(NT - 1):
                    for j2 in range(j + 1, NT):
                        ins = PE.e.matmul(psR[:, 32 * j2:32 * j2 + 32], lhsT=ones_b[:], rhs=Mk[:, j, :], start=False,
                                          stop=(j == NT - 2))
                t_pos = PE.sig(ins)
                dv(lambda: V.tensor_copy(out=pos[:].rearrange("p j e -> p (j e)"), in_=psR[:]), t_pos)
                for k in range(2):
                    wk = wa if k == 0 else wb_
                    dv(lambda: V.tensor_tensor(out=tmp32[:], in0=oh[k][:], in1=pos[:], op=ALU.mult))
                    dv(lambda: V.tensor_reduce(out=pk[:], in_=tmp32[:], axis=AX.X, op=ALU.add))
                    dv(lambda: V.tensor_tensor(out=tmp32[:], in0=oh[k][:], in1=iota3[:], op=ALU.mult), t_io)
                    dv(lambda: V.tensor_reduce(out=ek[:], in_=tmp32[:], axis=AX.X, op=ALU.add))
                    dv(lambda: V.scalar_tensor_tensor(out=dk[:], in0=ek[:], scalar=float(CAP), in1=pk[:], op0=ALU.mult, op1=ALU.add))
                    dv(lambda: V.tensor_scalar(out=ok[:], in0=pk[:], scalar1=float(CAP), scalar2=1.0e6, op0=ALU.is_ge, op1=ALU.mult))
                    dv(lambda: V.tensor_tensor(out=dk[:], in0=dk[:], in1=ok[:], op=ALU.add))
                    dv(lambda: V.tensor_copy(out=dst_i[:, k, :], in_=dk[:]))
                    dv(lambda: V.tensor_scalar(out=ok[:], in0=pk[:], scalar1=float(CAP), scalar2=None, op0=ALU.is_lt))
                    dv(lambda: V.tensor_tensor(out=wts[:, k, :], in0=wk[:], in1=ok[:], op=ALU.mult))
                t_route = last[0]


            s5p = ExitStack()
            psT = pst("psT5", [128, 2, 1024], BF16, s5p)
            psG = pst("psG", [128, 2, 2, 256], F32, s5p)
            psY = pst("psY", [128, 2, 2, 512], F32, s5p)

            ACT.wait(d_pre_pg.tok())
            for kc4 in range(4):
                t_wpg = d_wpg.add(ACT.e.dma_start(out=wpg[:, 4 * kc4:4 * kc4 + 4, :], in_=WPG[:, 4 * kc4:4 * kc4 + 4, :]))
            NMT2 = 2 * NMT
            mt = mt + [sb("mtx%d" % i, [128, D], BF16, s5) for i in range(NMT)]
            dmt = dmt + [mk_dsem("mtx%d" % i) for i in range(NMT)]
            dsc = dsc + [mk_dsem("scx%d" % i) for i in range(NMT)]
            sc_tok = [None] * NMT2
            for j in range(NT):
                b = j % NMT2
                if j >= NMT:
                    SP.wait(sc_tok[b], t_route)
                    mt_tok[j] = dmt[b].add(SP.e.dma_start(out=mt[b][:], in_=Mscr[128 * j:128 * j + 128, :]))
                POOL.wait(mt_tok[j], t_route)
                for k in range(2):
                    sc_tok[b] = dsc[b].add(POOL.e.indirect_dma_start(
                        out=Xg, out_offset=bass.IndirectOffsetOnAxis(ap=dst_i[:, k, j:j + 1], axis=0),
                        in_=mt[b][:], in_offset=None, bounds_check=bc_reg, oob_is_err=False))
            t_scatter = list(sc_tok)

            def load_w(e):
                b = e % 2
                ACT.wait(w_free[b], d_pre_ex[e // 4].tok())
                dwe[b].add(ACT.e.dma_start(out=wgu[b][:].rearrange("p c f -> p (c f)"), in_=WGU[e]))
                return dwe[b].add(ACT.e.dma_start(out=wdn[b][:].rearrange("p c d -> p (c d)"), in_=WDN[e]))

            def load_x(e):
                b = e % 2
                SP.wait(t_scatter, xs_free[b])
                for s_ in range(2):
                    t = dxs[b].add(SP.e.dma_start(out=xs[b][s_][:], in_=Xg[e * CAP + 128 * s_:e * CAP + 128 * s_ + 128, :]))
                return t

            w_free = [None, None]
            xs_free = [None, None]
            psT_free = [None, None]
            psG_free = [None, None]
            psY_free = [None, None]
            yb_free = [None, None]
            XT_free = [None, None]
            hid_free = None
            sg_free = None
            xt_evs = {}
            t_w = {0: load_w(0)}
            t_x = {0: load_x(0)}
            if NE > 1:
                t_x[1] = load_x(1)

            def tr_round(e, r):
                b = e % 2
                s_, ch = r // 2, r % 2
                tb = r % 2
                PE.wait(t_x[e], psT_free[tb], t_idb)
                for i8 in range(8):
                    c = 8 * ch + i8
                    ins = PE.e.transpose(out=psT[:, tb, 128 * i8:128 * i8 + 128],
                                         in_=xs[b][s_][:, 128 * c:128 * c + 128], identity=ident_b[:])
                t_tr = PE.sig(ins)
                E = ACT if tb == 0 else DVE
                E.wait(t_tr, XT_free[b])
                if tb == 0:
                    t_e = ACT.sig(ACT.e.activation(out=XT[b][:, 8 * ch:8 * ch + 8, 128 * s_:128 * s_ + 128],
                                                   in_=psT[:, tb, :].rearrange("p (c t) -> p c t", c=8), func=AF.Copy))
                else:
                    t_e = DVE.sig(DVE.e.tensor_copy(out=XT[b][:, 8 * ch:8 * ch + 8, 128 * s_:128 * s_ + 128],
                                                    in_=psT[:, tb, :].rearrange("p (c t) -> p c t", c=8)))
                psT_free[tb] = t_e
                xt_evs.setdefault(e, []).append(t_e)
                if r == 3:
                    xs_free[b] = t_tr

            for r in range(4):
                tr_round(0, r)
            for e in range(NE):
                b = e % 2
                if e + 1 < NE:
                    t_w[e + 1] = load_w(e + 1)
                if e + 2 < NE:
                    t_x[e + 2] = load_x(e + 2)
                hts = []
                for fc in range(2):
                    PE.wait(xt_evs[e], t_w[e], psG_free[fc])
                    for gu in range(2):
                        for kc in range(16):
                            ins = PE.e.matmul(psG[:, fc, gu, :], lhsT=wgu[b][:, kc, 256 * gu + 128 * fc:256 * gu + 128 * fc + 128],
                                              rhs=XT[b][:, kc, :], start=(kc == 0), stop=(kc == 15))
                        if gu == 1:
                            t_g = PE.sig(ins)
                        if e + 1 < NE:
                            tr_round(e + 1, 2 * fc + gu)
                    ACT.wait(t_g, sg_free)
                    t_s = ACT.sig(ACT.e.activation(out=sg[:, fc, :], in_=psG[:, fc, 0, :], func=AF.Silu))
                    DVE.wait(t_s, hid_free)
                    t_h = DVE.sig(DVE.e.tensor_tensor(out=hidT[:, fc, :], in0=sg[:, fc, :], in1=psG[:, fc, 1, :], op=ALU.mult))
                    psG_free[fc] = t_h
                    hts.append(t_h)
                XT_free[b] = t_g
                sg_free = hts[1]
                ev_s = []
                for u in range(4):
                    s_, half = u // 2, u % 2
                    yi = u % 2
                    PE.wait(hts, psY_free[yi])
                    for nn in range(2):
                        for fc in range(2):
                            ins = PE.e.matmul(psY[:, yi, nn, :], lhsT=hidT[:, fc, 128 * s_:128 * s_ + 128],
                                              rhs=wdn[b][:, fc, 1024 * half + 512 * nn:1024 * half + 512 * nn + 512],
                                              start=(fc == 0), stop=(fc == 1))
                    t_y = PE.sig(ins)
                    if yi == 0:
                        ACT.wait(t_y, yb_free[s_])
                        t_c = ACT.sig(ACT.e.activation(out=yb[s_][:, 1024 * half:1024 * half + 1024],
                                                       in_=psY[:, yi, :, :].rearrange("p n f -> p (n f)"), func=AF.Copy))
                    else:
                        DVE.wait(t_y, yb_free[s_])
                        t_c = DVE.sig(DVE.e.tensor_copy(out=yb[s_][:, 1024 * half:1024 * half + 1024],
                                                        in_=psY[:, yi, :, :].rearrange("p n f -> p (n f)")))
                    psY_free[yi] = t_c
                    ev_s.append(t_c)
                    if half == 1:
                        SP.wait(ev_s)
                        ev_s = []
                        yb_free[s_] = dys[s_].add(SP.e.dma_start(out=Y[e * CAP + 128 * s_:e * CAP + 128 * s_ + 128, :], in_=yb[s_][:]))
                hid_free = t_y
                w_free[b] = t_y
            barrier()
            s5p.close()

        with ExitStack() as s6:
            h2 = [sb("h2_%d" % i, [128, D], F32, s6) for i in range(3)]
            y1 = [sb("y1_%d" % i, [128, D], BF16, s6) for i in range(2)]
            y2 = [sb("y2_%d" % i, [128, D], BF16, s6) for i in range(2)]
            nb = [sb("nb%d" % i, [128, D], BF16, s6) for i in range(2)]
            nT = [sb("nT%d" % i, [128, 16, 128], BF16, s6) for i in range(2)]
            ptl = [sb("ptl%d" % i, [128, 256], F32, s6) for i in range(2)]
            pbf = [sb("pbf%d" % i, [128, 256], BF16, s6) for i in range(2)]
            pT = [sb("pT%d" % i, [128, 2, 128], BF16, s6) for i in range(2)]
            wple = sb("wple", [128, 2, D], BF16, s6)
            gpl = sb("gpl", [128, D], F32, s6)
            gfn = sb("gfn", [128, D], F32, s6)
            sgm = [sb("sgm%d" % i, [128, 512], F32, s6) for i in range(2)]
            ot = [sb("ot%d" % i, [128, D], F32, s6) for i in range(2)]
            junk = sb("junk6", [128, D], BF16, s6)
            psT = pst("psT6", [128, 2, 1024], BF16, s6)
            psGt = pst("psGt", [128, 2, 512], F32, s6)
            psPe = pst("psPe", [128, 2, 512], F32, s6)
            d_c6 = mk_dsem("c6")
            dh2 = [mk_dsem("h2l%d" % i) for i in range(3)]
            dy = [mk_dsem("yg%d" % i) for i in range(2)]
            dp = [mk_dsem("pl%d" % i) for i in range(2)]
            dout = [mk_dsem("out%d" % i) for i in range(2)]
            t_c6 = d_c6.add(POOL.e.dma_start(out=wple[:], in_=w_ple.rearrange("(c p) d -> p c d", p=128)))
            t_c6 = d_c6.add(SP.e.dma_start(out=gpl[:], in_=g_ple.to_broadcast([128, D])))
            t_c6 = d_c6.add(SP.e.dma_start(out=gfn[:], in_=g_final.to_broadcast([128, D])))
            h2_free = [None, None, None]
            y_free = [None, None]
            nb_free = [None, None]
            nT_free = [None, None]
            p_free = [None, None]
            pbf_free = [None, None]
            pT_free = [None, None]
            psT_free = [None, None]
            psQ_free = [None, None]
            sgm_free = [None, None]
            ot_free = [None, None]
            stA = {}

            def A_pre(j):
                b = j % 2
                SP.wait(h2_free[j % 3])
                t_h = dh2[j % 3].add(SP.e.dma_start(out=h2[j % 3][:], in_=H1[128 * j:128 * j + 128, :]))
                SP.wait(p_free[b])
                t_p = dp[b].add(SP.e.dma_start(out=ptl[b][:], in_=p_in[128 * j:128 * j + 128, :]))
                POOL.wait(y_free[b])
                dy[b].add(POOL.e.indirect_dma_start(out=y1[b][:], out_offset=None, in_=Y,
                                                    in_offset=bass.IndirectOffsetOnAxis(ap=dst_i[:, 0, j:j + 1], axis=0),
                                                    bounds_check=bc_reg, oob_is_err=False))
                t_y = dy[b].add(POOL.e.indirect_dma_start(out=y2[b][:], out_offset=None, in_=Y,
                                                          in_offset=bass.IndirectOffsetOnAxis(ap=dst_i[:, 1, j:j + 1], axis=0),
                                                          bounds_check=bc_reg, oob_is_err=False))
                DVE.wait(t_h, t_y)
                t1 = DVE.sig(DVE.e.scalar_tensor_tensor(out=h2[j % 3][:], in0=y1[b][:], scalar=wts[:, 0, j:j + 1], in1=h2[j % 3][:],
                                                        op0=ALU.mult, op1=ALU.add))
                DVE.wait(t1)
                t2 = DVE.sig(DVE.e.scalar_tensor_tensor(out=h2[j % 3][:], in0=y2[b][:], scalar=wts[:, 1, j:j + 1], in1=h2[j % 3][:],
                                                        op0=ALU.mult, op1=ALU.add))
                y_free[b] = t2
                t_rs = rms_stats(32 + j, h2[j % 3][:], junk[:], t2)
                DVE.wait(t_rs, t_c6, nb_free[b])
                t_n = DVE.sig(DVE.e.scalar_tensor_tensor(out=nb[b][:], in0=h2[j % 3][:], scalar=rs[:, 32 + j:33 + j], in1=gpl[:],
                                                         op0=ALU.mult, op1=ALU.mult))
                ACT.wait(t_p, pbf_free[b])
                t_pb = ACT.sig(ACT.e.activation(out=pbf[b][:], in_=ptl[b][:], func=AF.Copy))
                p_free[b] = t_pb
                stA[j] = dict(t_n=t_n, t_pb=t_pb)

            def A_tr(j):
                b = j % 2
                a = stA[j]
                evs = []
                for ch in range(2):
                    PE.wait(a["t_n"], psT_free[ch])
                    for i8 in range(8):
                        c = 8 * ch + i8
                        ins = PE.e.transpose(out=psT[:, ch, 128 * i8:128 * i8 + 128], in_=nb[b][:, 128 * c:128 * c + 128],
                                             identity=ident_b[:])
                    if ch == 0:
                        t_tr = PE.sig(ins)
                        ACT.wait(t_tr, nT_free[b])
                        t_e = ACT.sig(ACT.e.activation(out=nT[b][:, 0:8, :],
                                                       in_=psT[:, 0, :].rearrange("p (c t) -> p c t", c=8), func=AF.Copy))
                        psT_free[0] = t_e
                        evs.append(t_e)
                    else:
                        t_tr = PE.sig(ins)
                        ACT.wait(t_tr, nT_free[b])
                        t_e = ACT.sig(ACT.e.activation(out=nT[b][:, 8:16, :],
                                                       in_=psT[:, 1, :].rearrange("p (c t) -> p c t", c=8), func=AF.Copy))
                        psT_free[1] = t_e
                        evs.append(t_e)
                nb_free[b] = t_tr
                PE.wait(a["t_pb"], psT_free[0])
                for c in range(2):
                    ins = PE.e.transpose(out=psT[:, 0, 128 * c:128 * c + 128], in_=pbf[b][:, 128 * c:128 * c + 128],
                                         identity=ident_b[:])
                t_tr = PE.sig(ins)
                pbf_free[b] = t_tr
                ACT.wait(t_tr, pT_free[b])
                t_e = ACT.sig(ACT.e.activation(out=pT[b][:], in_=psT[:, 0, 0:256].rearrange("p (c t) -> p c t", c=2),
                                               func=AF.Copy))
                psT_free[0] = t_e
                evs.append(t_e)
                a["evs"] = evs

            def B_mm_q(j, q):
                b = j % 2
                a = stA[j]
                qb = q % 2
                PE.wait(a["evs"], t_wpg, t_c6, psQ_free[qb])
                for kc in range(16):
                    PE.e.matmul(psGt[:, qb, :], lhsT=nT[b][:, kc, :], rhs=wpg[:, kc, 512 * q:512 * q + 512],
                                start=(kc == 0), stop=(kc == 15))
                for kc in range(2):
                    ins = PE.e.matmul(psPe[:, qb, :], lhsT=pT[b][:, kc, :], rhs=wple[:, kc, 512 * q:512 * q + 512],
                                      start=(kc == 0), stop=(kc == 1))
                t_mm = PE.sig(ins)
                ACT.wait(t_mm, sgm_free[qb])
                t_s = ACT.sig(ACT.e.activation(out=sgm[qb][:], in_=psGt[:, qb, :], func=AF.Sigmoid))
                DVE.wait(t_s)
                t_a = DVE.sig(DVE.e.tensor_tensor(out=sgm[qb][:], in0=sgm[qb][:], in1=psPe[:, qb, :], op=ALU.mult))
                psQ_free[qb] = t_a
                DVE.wait(t_a)
                tq = DVE.sig(DVE.e.tensor_tensor(out=h2[j % 3][:, 512 * q:512 * q + 512],
                                                 in0=h2[j % 3][:, 512 * q:512 * q + 512], in1=sgm[qb][:], op=ALU.add))
                sgm_free[qb] = tq
                a["tq"] = tq
                if q == 3:
                    nT_free[b] = t_mm
                    pT_free[b] = t_mm

            def B_fin(j):
                b = j % 2
                a = stA[j]
                t_rs = rms_stats(48 + j, h2[j % 3][:], junk[:], a["tq"])
                DVE.wait(t_rs, ot_free[b])
                t_o = DVE.sig(DVE.e.scalar_tensor_tensor(out=ot[b][:], in0=h2[j % 3][:], scalar=rs[:, 48 + j:49 + j], in1=gfn[:],
                                                         op0=ALU.mult, op1=ALU.mult))
                h2_free[j % 3] = t_o
                ACT.wait(t_o)
                ot_free[b] = dout[b].add(ACT.e.dma_start(out=out[128 * j:128 * j + 128, :], in_=ot[b][:]))

            A_pre(0)
            A_tr(0)
            for j in range(NT):
                B_mm_q(j, 0)
                B_mm_q(j, 1)
                if j + 1 < NT:
                    A_pre(j + 1)
                B_mm_q(j, 2)
                B_mm_q(j, 3)
                if j + 1 < NT:
                    A_tr(j + 1)
                B_fin(j)
            barrier()
        st_wpg.close()
    return nc


_IN_NAMES = ["x", "p", "w_in", "w_out", "sinks", "g_mix", "g_moe", "g_ple", "g_final", "w_grp", "b_grp",
             "w_exp", "b_exp", "w_gate", "w_up", "w_down", "w_ple", "w_ple_gate"]


def make_in_maps(inputs, n_cores=8):
    f = lambda a: np.ascontiguousarray(np.asarray(a, dtype=np.float32))
    shared = {
        "w_in": f(inputs["w_in"][0]), "w_out": f(inputs["w_out"][0]),
        "sinks": f(inputs["sinks"]).reshape(1, 16),
        "g_mix": f(inputs["g_mix"]).reshape(1, D), "g_moe": f(inputs["g_moe"]).reshape(1, D),
        "g_ple": f(inputs["g_ple"]).reshape(1, D), "g_final": f(inputs["g_final"]).reshape(1, D),
        "w_grp": f(inputs["w_grp"][0]), "b_grp": f(inputs["b_grp"]).reshape(1, 4),
        "w_exp": f(inputs["w_exp"][0]).reshape(D, 32), "b_exp": f(inputs["b_exp"]).reshape(1, 32),
        "w_gate": f(inputs["w_gate"][0]), "w_up": f(inputs["w_up"][0]), "w_down": f(inputs["w_down"][0]),
        "w_ple": f(inputs["w_ple"][0]), "w_ple_gate": f(inputs["w_ple_gate"][0]),
    }
    xs = f(inputs["x"])
    ps = f(inputs["p"])
    maps = []
    for c in range(n_cores):
        m = dict(shared)
        m["x"] = xs[c]
        m["p"] = ps[0, c]
        maps.append(m)
    return maps


def kernel(**inputs):
    nc = build()
    in_maps = make_in_maps(inputs)
    res = run_bass_kernel_spmd(nc, in_maps, core_ids=list(range(8)))
    return np.stack([r["out"] for r in res.results], axis=0).astype(np.float32)
```

```python
import numpy as np
from contextlib import ExitStack
import concourse.bass as bass
import concourse.mybir as mybir
from concourse.bass_utils import run_bass_kernel_spmd

F32 = mybir.dt.float32
BF16 = mybir.dt.bfloat16
I32 = mybir.dt.int32
AF = mybir.ActivationFunctionType
ALU = mybir.AluOpType
AX = mybir.AxisListType

S = 2048
D = 2048
NT = 16
NE = 32
CAP = 256
NEGV = -30000.0
BIGV = 1.0e4
EPS = 1e-6


class Eng:
    def __init__(self, nc, es, eng, name):
        self.e = eng
        self.sem = es.enter_context(nc.semaphore("sem_" + name))
        self.cnt = 0
        self.seen = {}

    def wait(self, *toks):
        for t in toks:
            if t is None:
                continue
            if isinstance(t, list):
                self.wait(*t)
                continue
            sem, val = t
            k = id(sem)
            if self.seen.get(k, 0) >= val:
                continue
            self.e.wait_ge(sem, val)
            self.seen[k] = val

    def sig(self, inst):
        self.cnt += 1
        inst.then_inc(self.sem, 1)
        return (self.sem, self.cnt)


class DSem:
    def __init__(self, nc, es, name, reg):
        self.sem = es.enter_context(nc.semaphore("dma_" + name))
        self.cnt = 0
        reg.append(self)

    def add(self, inst):
        self.cnt += 16
        inst.then_inc(self.sem, 16)
        return (self.sem, self.cnt)

    def tok(self):
        return (self.sem, self.cnt) if self.cnt else None


def build(stage=99):
    nc = bass.Bass("TRN2", target_bir_lowering=False)

    def din(n, s, d=F32):
        return nc.dram_tensor(n, s, d, kind="ExternalInput").ap()

    x = din("x", [S, D])
    p_in = din("p", [S, 256])
    w_in = din("w_in", [D, 4352])
    w_out = din("w_out", [D, D])
    sinks = din("sinks", [1, 16])
    g_mix = din("g_mix", [1, D])
    g_moe = din("g_moe", [1, D])
    g_ple = din("g_ple", [1, D])
    g_final = din("g_final", [1, D])
    w_grp = din("w_grp", [D, 4])
    b_grp = din("b_grp", [1, 4])
    w_exp = din("w_exp", [D, 32])
    b_exp = din("b_exp", [1, 32])
    w_gate = din("w_gate", [NE, D, 256])
    w_up = din("w_up", [NE, D, 256])
    w_down = din("w_down", [NE, 256, D])
    w_ple = din("w_ple", [256, D])
    w_pg = din("w_ple_gate", [D, D])
    out = nc.dram_tensor("out", [S, D], F32, kind="ExternalOutput").ap()
    H1 = nc.dram_tensor("H1", [S, D], F32, kind="Internal").ap()
    Mscr = nc.dram_tensor("Mscr", [S, D], BF16, kind="Internal").ap()
    Xg = nc.dram_tensor("Xg", [NE * CAP, D], BF16, kind="Internal").ap()
    Y = nc.dram_tensor("Y", [NE * CAP, D], BF16, kind="Internal").ap()
    WGU = nc.dram_tensor("WGU", [NE, 128, 16 * 512], BF16, kind="Internal").ap()
    WDN = nc.dram_tensor("WDN", [NE, 128, 2 * D], BF16, kind="Internal").ap()
    WO = nc.dram_tensor("WO", [128, 16, D], BF16, kind="Internal").ap()
    WPG = nc.dram_tensor("WPG", [128, 16, D], BF16, kind="Internal").ap()

    es = ExitStack()
    with es:
        PE = Eng(nc, es, nc.tensor, "pe")
        ACT = Eng(nc, es, nc.scalar, "act")
        DVE = Eng(nc, es, nc.vector, "dve")
        POOL = Eng(nc, es, nc.gpsimd, "pool")
        SP = Eng(nc, es, nc.sync, "sp")
        ENGS = [PE, ACT, DVE, POOL, SP]
        dsems = []
        bar_sem = es.enter_context(nc.semaphore("bar"))
        bar_cnt = [0]

        def mk_dsem(name):
            return DSem(nc, es, name, dsems)

        def barrier():
            for ds in dsems:
                SP.wait(ds.tok())
            bar_cnt[0] += len(ENGS)
            for E in ENGS:
                E.e.drain().then_inc(bar_sem, 1)
            for E in ENGS:
                E.e.wait_ge(bar_sem, bar_cnt[0])

        d_pre_wo = DSem(nc, es, "pre_wo", [])
        d_pre_ex = [DSem(nc, es, "pre_ex%d" % g_, []) for g_ in range(8)]
        d_pre_pg = DSem(nc, es, "pre_pg", [])
        bg_list = []
        for kc in range(16):
            bg_list.append((d_pre_wo, WO[:, kc, :], w_out[128 * kc:128 * kc + 128, :]))
        for kc in range(16):
            bg_list.append((d_pre_pg, WPG[:, kc, :], w_pg[128 * kc:128 * kc + 128, :]))
        for e in range(NE):
            wv = WGU[e].rearrange("p (c f) -> p c f", c=16)
            bg_list.append((d_pre_ex[e // 4], wv[:, :, 0:256], w_gate[e].rearrange("(c p) f -> p c f", p=128)))
            bg_list.append((d_pre_ex[e // 4], wv[:, :, 256:512], w_up[e].rearrange("(c p) f -> p c f", p=128)))
            bg_list.append((d_pre_ex[e // 4], WDN[e].rearrange("p (c d) -> p c d", c=2),
                            w_down[e].rearrange("(c p) d -> p c d", p=128)))
        bg_pos = [0]

        def bg_issue(n):
            for _ in range(n):
                if bg_pos[0] >= len(bg_list):
                    return
                ds, o, i = bg_list[bg_pos[0]]
                bg_pos[0] += 1
                ds.add(POOL.e.dma_start(out=o, in_=i))

        def sb(name, shape, dt, stack=es):
            return stack.enter_context(nc.sbuf_tensor(name, shape, dt))

        def pst(name, shape, dt, stack):
            return stack.enter_context(nc.psum_tensor(name, shape, dt))

        ident_f = sb("ident_f", [128, 128], F32)
        ident_b = sb("ident_b", [128, 128], BF16)
        ones_b = sb("ones_b", [128, 128], BF16)
        Utri = sb("Utri", [128, 128], BF16)
        DT = sb("DT", [128, 256], F32)
        NEG_A = sb("NEG_A", [128, 256], F32)
        NEG_B = sb("NEG_B", [128, 256], F32)
        es_t = sb("es_t", [128, 16], F32)
        sk_t = sb("sk_t", [128, 16], F32)
        ss = sb("ss", [128, 64], F32)
        sd = sb("sd", [128, 64], F32)
        rs = sb("rs", [128, 64], F32)
        d_c = mk_dsem("const")
        lg_all = sb("lg_all", [128, NT, 36], F32)
        dst_i = sb("dst_i", [128, 2, NT], I32)
        wts = sb("wts", [128, 2, NT], F32)

        g = POOL.e
        g.memset(ident_f[:], 1.0)
        g.affine_select(out=ident_f[:], in_=ident_f[:], pattern=[[-1, 128]], compare_op=ALU.is_equal,
                        fill=0.0, base=0, channel_multiplier=1)
        g.memset(ones_b[:], 1.0)
        g.memset(Utri[:], 1.0)
        g.affine_select(out=Utri[:], in_=Utri[:], pattern=[[1, 128]], compare_op=ALU.is_gt,
                        fill=0.0, base=0, channel_multiplier=-1)
        g.iota(DT[:, 0:128], pattern=[[1, 128]], base=128, channel_multiplier=-1,
               allow_small_or_imprecise_dtypes=True)
        g.iota(DT[:, 128:256], pattern=[[1, 128]], base=0, channel_multiplier=-1,
               allow_small_or_imprecise_dtypes=True)
        g.memset(NEG_A[:], 0.0)
        g.memset(NEG_B[:], 0.0)
        g.affine_select(out=NEG_A[:, 0:128], in_=NEG_A[:, 0:128], pattern=[[-1, 128]], compare_op=ALU.is_ge,
                        fill=NEGV, base=0, channel_multiplier=1)
        g.affine_select(out=NEG_A[:, 128:256], in_=NEG_A[:, 128:256], pattern=[[1, 128]], compare_op=ALU.is_ge,
                        fill=NEGV, base=0, channel_multiplier=-1)
        g.affine_select(out=NEG_B[:, 0:128], in_=NEG_B[:, 0:128], pattern=[[-1, 128]], compare_op=ALU.is_ge,
                        fill=NEGV, base=-1, channel_multiplier=1)
        t_pc = POOL.sig(g.affine_select(out=NEG_B[:, 128:256], in_=NEG_B[:, 128:256], pattern=[[1, 128]],
                                        compare_op=ALU.is_ge, fill=NEGV, base=0, channel_multiplier=-1))
        d_sk = mk_dsem("sk")
        bc_reg = POOL.e.to_reg(NE * CAP - 1)
        t_sk = d_sk.add(SP.e.dma_start(out=sk_t[:], in_=sinks.to_broadcast([128, 16])))
        DVE.wait(t_pc)
        t_idb = DVE.sig(DVE.e.tensor_copy(out=ident_b[:], in_=ident_f[:]))
        ACT.wait(t_sk)
        t_es = ACT.sig(ACT.e.activation(out=es_t[:], in_=sk_t[:], func=AF.Exp))
        t_const = [t_pc, t_idb, t_es]

        def rms_stats(col, src, junk, src_tok):
            ACT.wait(src_tok)
            t1 = ACT.sig(ACT.e.activation(out=junk, in_=src, func=AF.Square, accum_out=ss[:, col:col + 1]))
            ACT.wait(t1)
            t2 = ACT.sig(ACT.e.activation(out=sd[:, col:col + 1], in_=ss[:, col:col + 1], func=AF.Sqrt,
                                          scale=1.0 / D, bias=EPS))
            DVE.wait(t2)
            t3 = DVE.sig(DVE.e.reciprocal(out=rs[:, col:col + 1], in_=sd[:, col:col + 1]))
            return t3

        dbg = None
        if stage < 99:
            dbg = nc.dram_tensor("dbg", [S, D], F32, kind="ExternalOutput").ap()

        st_mixed = ExitStack()
        mixedT = sb("mixedT", [128, 16, S], BF16, st_mixed)
        st_aT = ExitStack()
        aT = sb("aT", [128, 16, S], BF16, st_aT)

        with ExitStack() as s1:
            xt = [sb("xt%d" % i, [128, D], F32, s1) for i in range(2)]
            at = [sb("at%d" % i, [128, D], BF16, s1) for i in range(2)]
            gm = sb("gm", [128, D], F32, s1)
            junk = sb("junk", [128, D], BF16, s1)
            psT = pst("psT1", [128, 2, 1024], BF16, s1)
            dx = [mk_dsem("x%d" % i) for i in range(2)]
            d_g = mk_dsem("gmix")
            t_g = d_g.add(SP.e.dma_start(out=gm[:], in_=g_mix.to_broadcast([128, D])))
            xt_free = [None, None]
            at_free = [None, None]
            psT_free = [None, None]
            p1 = {}

            def pre1(j):
                b = j % 2
                SP.wait(xt_free[b])
                t_x = dx[b].add(SP.e.dma_start(out=xt[b][:], in_=x[128 * j:128 * j + 128, :]))
                t_rs = rms_stats(j, xt[b][:], junk[:], t_x)
                DVE.wait(t_rs, t_x, t_g, at_free[b])
                t_a = DVE.sig(DVE.e.scalar_tensor_tensor(out=at[b][:], in0=xt[b][:], scalar=rs[:, j:j + 1],
                                                         in1=gm[:], op0=ALU.mult, op1=ALU.mult))
                xt_free[b] = t_a
                p1[j] = t_a

            def post1(j):
                b = j % 2
                PE.wait(p1[j], t_idb)
                t_tr = [None, None]
                for c in range(16):
                    if c % 8 == 0:
                        PE.wait(psT_free[c // 8])
                    ins = PE.e.transpose(out=psT[:, c // 8, (c % 8) * 128:(c % 8) * 128 + 128],
                                         in_=at[b][:, c * 128:c * 128 + 128], identity=ident_b[:])
                    if c % 8 == 7:
                        t_tr[c // 8] = PE.sig(ins)
                at_free[b] = t_tr[1]
                ACT.wait(t_tr[0])
                psT_free[0] = ACT.sig(ACT.e.activation(
                    out=aT[:, 0:8, 128 * j:128 * j + 128],
                    in_=psT[:, 0, :].rearrange("p (c t) -> p c t", c=8), func=AF.Copy))
                DVE.wait(t_tr[1])
                psT_free[1] = DVE.sig(DVE.e.tensor_copy(
                    out=aT[:, 8:16, 128 * j:128 * j + 128],
                    in_=psT[:, 1, :].rearrange("p (c t) -> p c t", c=8)))

            pre1(0)
            for j in range(NT):
                if j + 1 < NT:
                    pre1(j + 1)
                post1(j)
            t_aT = [psT_free[0], psT_free[1]]
            barrier()

        if stage == 1:
            with ExitStack() as sd_:
                tmp = sb("dbgtmp", [128, S], F32, sd_)
                dd = mk_dsem("dbg")
                for c in range(16):
                    DVE.wait(dd.tok())
                    t = DVE.sig(DVE.e.tensor_copy(out=tmp[:], in_=aT[:, c, :]))
                    SP.wait(t)
                    dd.add(SP.e.dma_start(out=dbg[128 * c:128 * c + 128, :], in_=tmp[:]))
                SP.wait(dd.tok())
                barrier()
            st_aT.close()
            st_mixed.close()
            return nc

        d_wo2 = mk_dsem("wout_pf")
        with ExitStack() as s2:
            slab = [sb("slab%d" % i, [128, 16, 128], BF16, s2) for i in range(4)]
            dW = [mk_dsem("slab%d" % i) for i in range(4)]
            QT = sb("QT", [128, S], BF16, s2)
            KT = sb("KT", [128, S], BF16, s2)
            VT = sb("VT", [128, S], BF16, s2)
            Vs = [sb("Vs%d" % i, [128, 16, 128], BF16, s2) for i in range(2)]
            ODacc = sb("ODacc", [128, 2, S], F32, s2)
            PT = [sb("PT%d" % i, [128, 512], BF16, s2) for i in range(3)]
            MB2 = [[sb("MB_%d_%d" % (i, r), [128, 512], BF16, s2) for r in range(3)] for i in range(2)]
            btmp = [sb("btmp%d" % i, [128, 2, 128], F32, s2) for i in range(2)]
            psP = pst("psP", [128, 2, 512], F32, s2)
            psT = pst("psT2", [128, 1, 1024], BF16, s2)
            psS = pst("psS", [128, 3, 512], F32, s2)
            psO = pst("psO", [128, 2, 512], F32, s2)

            st = dict(slab_n=0, slab_free=[None] * 4, psP_n=0, psP_free=[None, None],
                      psT_free=None, vs_n=0, vs_free=[None, None], mb_n=0, mb_free=[None, None])
            buf_tok = {}

            def mat_job(pieces, dst, scale, dst_free):
                k = st["slab_n"] % 4
                st["slab_n"] += 1
                POOL.wait(st["slab_free"][k])
                off = 0
                tok = None
                for (c0, n) in pieces:
                    tok = dW[k].add(POOL.e.dma_start(
                        out=slab[k][:, :, off:off + n],
                        in_=w_in[:, c0:c0 + n].rearrange("(c p) n -> p c n", p=128)))
                    off += n
                assert off == 128
                bg_issue(3)
                last = None
                for t4 in range(4):
                    bk = st["psP_n"] % 2
                    st["psP_n"] += 1
                    PE.wait(tok, st["psP_free"][bk], t_aT)
                    for kc in range(16):
                        ins = PE.e.matmul(psP[:, bk, :], lhsT=slab[k][:, kc, :], rhs=aT[:, kc, 512 * t4:512 * t4 + 512],
                                          start=(kc == 0), stop=(kc == 15))
                    t_mm = PE.sig(ins)
                    if bk == 0:
                        ACT.wait(t_mm, dst_free)
                        last = ACT.sig(ACT.e.activation(out=dst[:, 512 * t4:512 * t4 + 512], in_=psP[:, bk, :],
                                                        func=AF.Copy, scale=float(scale)))
                    else:
                        DVE.wait(t_mm, dst_free)
                        last = DVE.sig(DVE.e.tensor_scalar(out=dst[:, 512 * t4:512 * t4 + 512], in0=psP[:, bk, :],
                                                           scalar1=float(scale), scalar2=None, op0=ALU.mult))
                    st["psP_free"][bk] = last
                    if t4 == 2:
                        prev = last
                st["slab_free"][k] = t_mm
                return [prev, last]

            def v_layout(src, src_tok, s):
                vi = st["vs_n"] % 2
                st["vs_n"] += 1
                V = Vs[vi]
                toks = []
                for half in range(2):
                    PE.wait(src_tok, st["psT_free"], t_idb)
                    for i8 in range(8):
                        slot = half * 8 + i8
                        b_, rho = slot // s, slot % s
                        start = s * 128 * b_ + rho
                        ins = PE.e.transpose(out=psT[:, 0, i8 * 128:i8 * 128 + 128],
                                             in_=src[:, start:start + 127 * s + 1:s], identity=ident_b[:])
                    t_tr = PE.sig(ins)
                    buf_tok["vt_free"] = t_tr
                    E = ACT if half == 0 else DVE
                    E.wait(t_tr, st["vs_free"][vi])
                    if half == 0:
                        t_e = ACT.sig(ACT.e.activation(out=V[:, 0:8, :],
                                                       in_=psT[:, 0, :].rearrange("p (c t) -> p c t", c=8),
                                                       func=AF.Copy))
                    else:
                        t_e = DVE.sig(DVE.e.tensor_copy(out=V[:, 8:16, :],
                                                        in_=psT[:, 0, :].rearrange("p (c t) -> p c t", c=8)))
                    st["psT_free"] = t_e
                    toks.append(t_e)
                return vi, toks

            def make_mask(coefs, negt):
                par = st["mb_n"] % 2
                st["mb_n"] += 1
                DVE.wait(st["mb_free"][par], t_const)
                t = None
                for r, c in enumerate(coefs):
                    for u in range(2):
                        t = DVE.sig(DVE.e.scalar_tensor_tensor(out=MB2[par][r][:, 256 * u:256 * u + 256], in0=DT[:],
                                                               scalar=-float(c), in1=negt[:], op0=ALU.mult,
                                                               op1=ALU.add))
                return par, t

            batches = []

            def unit_cols(s, b_, rho):
                start = s * 128 * b_ + rho
                return slice(start, start + 127 * s + 1, s)

            for h in range(8):
                slope = 2.0 ** (-(h + 1))
                hstate = {}

                def pre_head(h=h, hstate=hstate, slope=slope):
                    qk_free = buf_tok.get("qk_free")
                    vt_free = buf_tok.get("vt_free")
                    hstate["q"] = mat_job([(h * 128, 128)], QT, 128.0 ** -0.5, qk_free)
                    hstate["k"] = mat_job([(1024 + h * 128, 128)], KT, 1.0, qk_free)
                    hstate["v"] = mat_job([(2048 + h * 128, 128)], VT, 1.0, vt_free)
                    hstate["mb"] = make_mask([slope * 1, slope * 4, slope * 16], NEG_A)

                for r, s in enumerate((1, 4, 16)):
                    units = [(b_, rho) for b_ in range(16 // s) for rho in range(s)]
                    bstate = {}

                    def pre_branch(s=s, hstate=hstate, bstate=bstate):
                        bstate["v"] = v_layout(VT, hstate["v"], s)

                    for ib in range(8):
                        us = units[2 * ib:2 * ib + 2]
                        pres = []
                        if r == 0 and ib == 0:
                            pres.append(pre_head)
                        if ib == 0:
                            pres.append(pre_branch)
                        batches.append(dict(kind="A", h=h, r=r, s=s, units=us, pres=pres, hstate=hstate,
                                            bstate=bstate, first=(r == 0), last_of_head=(r == 2 and ib == 7),
                                            last_of_branch=(ib == 7), rows=slice(0, 128)))

            kvstates = [{}, {}]
            for c in range(8):
                gkv = c // 4
                hstate = {}
                kvstate = kvstates[gkv]

                def pre_kv(gkv=gkv, kvstate=kvstate):
                    qk_free = buf_tok.get("qk_free")
                    vt_free = buf_tok.get("vt_free")
                    k0 = 4096 + gkv * 64
                    v0 = 4224 + gkv * 64
                    kvstate["k"] = mat_job([(k0, 64), (k0, 64)], KT, 1.0, qk_free)
                    kvstate["vT"] = mat_job([(v0, 64), (v0, 64)], VT, 1.0, vt_free)
                    kvstate["v"] = v_layout(VT, kvstate["vT"], 1)

                def pre_chunk(c=c, hstate=hstate, kvstate=kvstate):
                    qk_free = buf_tok.get("qk_free")
                    hstate["q"] = mat_job([(3072 + c * 128, 128)], QT, 64.0 ** -0.5, qk_free)
                    if c == 7:
                        SP.wait(st["slab_free"][(st["slab_n"] - 1) % 4], d_pre_wo.tok())
                        for kc4 in range(4):
                            buf_tok["t_w"] = d_wo2.add(SP.e.dma_start(out=aT[:, 4 * kc4:4 * kc4 + 4, :],
                                                                       in_=WO[:, 4 * kc4:4 * kc4 + 4, :]))
                    hstate["k"] = kvstate["k"]
                    sl = [2.0 ** (-8.0 * (2 * c + hh + 1) / 16.0) for hh in range(2)]
                    hstate["mb"] = make_mask(sl, NEG_B)

                for hh in range(2):
                    for ib in range(8):
                        us = [(2 * ib, 0), (2 * ib + 1, 0)]
                        pres = []
                        if hh == 0 and ib == 0:
                            if c % 4 == 0:
                                pres.append(pre_kv)
                            pres.append(pre_chunk)
                        batches.append(dict(kind="B", h=2 * c + hh, c=c, hh=hh, r=hh, s=1, units=us, pres=pres,
                                            hstate=hstate, bstate=kvstate, first=True, last_of_head=False,
                                            last_of_branch=(c % 4 == 3 and hh == 1 and ib == 7),
                                            last_q=(hh == 1 and ib == 7),
                                            rows=slice(64 * hh, 64 * hh + 64)))

            if stage == 2:
                batches = [bb for bb in batches if bb["kind"] == "A" and bb["h"] < 1]

            NB = len(batches)
            S_free = [None, None, None]
            PT_free = [None, None, None]
            O_free = [None, None]
            tS = [None] * NB
            tP = [None] * NB

            def emit_qk(n):
                bb = batches[n]
                for f in bb["pres"]:
                    f()
                pb = n % 3
                hs = bb["hstate"]
                rows = bb["rows"]
                par, t_mb = hs["mb"]
                PE.wait(S_free[pb], hs["q"], hs["k"], t_mb, t_idb)
                first = True
                for u, (b_, rho) in enumerate(bb["units"]):
                    qc = unit_cols(bb["s"], b_, rho)
                    if b_ > 0:
                        kc = unit_cols(bb["s"], b_ - 1, rho)
                        PE.e.matmul(psS[:, pb, 256 * u:256 * u + 128], lhsT=KT[rows, kc], rhs=QT[rows, qc],
                                    start=first, stop=False)
                        first = False
                    PE.e.matmul(psS[:, pb, 256 * u + 128:256 * u + 256], lhsT=KT[rows, qc], rhs=QT[rows, qc],
                                start=first, stop=False)
                    first = False
                ins = PE.e.matmul(psS[:, pb, :], lhsT=ident_b[:], rhs=MB2[par][bb["r"]][:], start=False, stop=True)
                tS[n] = PE.sig(ins)
                if bb["last_of_head"] or bb.get("last_q"):
                    buf_tok["qk_free"] = tS[n]
                if bb.get("last_q") or bb["last_of_head"]:
                    st["mb_free"][par] = tS[n]

            def emit_exp(n):
                pb = n % 3
                ACT.wait(tS[n], PT_free[pb])
                tP[n] = ACT.sig(ACT.e.activation(out=PT[pb][:], in_=psS[:, pb, :], func=AF.Exp))
                S_free[pb] = tP[n]

            def emit_pv(n):
                bb = batches[n]
                pb = n % 2
                p3 = n % 3
                vi, vtoks = bb["bstate"]["v"]
                V = Vs[vi]
                s = bb["s"]
                PE.wait(tP[n], O_free[pb], vtoks)
                first = True
                ins = None
                for u, (b_, rho) in enumerate(bb["units"]):
                    slot = b_ * s + rho
                    parts = []
                    if b_ > 0:
                        parts.append(((b_ - 1) * s + rho, PT[p3][:, 256 * u:256 * u + 128]))
                    parts.append((slot, PT[p3][:, 256 * u + 128:256 * u + 256]))
                    for i, (sl, pt) in enumerate(parts):
                        PE.e.matmul(psO[:, pb, 256 * u:256 * u + 128], lhsT=V[:, sl, :], rhs=pt,
                                    start=first, stop=False)
                        first = False
                    for i, (sl, pt) in enumerate(parts):
                        ins = PE.e.matmul(psO[:, pb, 256 * u + 128:256 * u + 256], lhsT=ones_b[:], rhs=pt,
                                          start=False, stop=(u == 1 and i == len(parts) - 1))
                t_o = PE.sig(ins)
                PT_free[p3] = t_o
                if bb["last_of_branch"]:
                    st["vs_free"][vi] = t_o
                return t_o

            def emit_evac(n, t_o):
                bb = batches[n]
                pb = n % 2
                s = bb["s"]
                (b0, r0), (b1, r1) = bb["units"]
                if bb["kind"] == "A":
                    if s == 1:
                        oap = ODacc[:, :, 128 * b0:128 * b0 + 256].rearrange("p o (u q) -> p o u q", u=2)
                    else:
                        lo = s * 128 * b0
                        oap = ODacc[:, :, lo:lo + 128 * s].rearrange("p o (q r) -> p o r q", r=s)[:, :, r0:r0 + 2, :]
                    iap = psO[:, pb, :].rearrange("p (u o q) -> p o u q", u=2, o=2)
                    DVE.wait(t_o, buf_tok.get("od_free"), buf_tok.get("od_last"))
                    if bb["first"]:
                        t_e = DVE.sig(DVE.e.tensor_copy(out=oap, in_=iap))
                    else:
                        t_e = DVE.sig(DVE.e.tensor_tensor(out=oap, in0=iap, in1=oap, op=ALU.add))
                    buf_tok["od_last"] = t_e
                    O_free[pb] = t_e
                    if bb["last_of_head"]:
                        h = bb["h"]
                        ACT.wait(t_e)
                        t0 = ACT.sig(ACT.e.activation(out=ODacc[:, 1, :], in_=ODacc[:, 1, :], func=AF.Ln))
                        ACT.wait(t0)
                        t1 = ACT.sig(ACT.e.activation(out=ODacc[:, 1, :], in_=ODacc[:, 1, :], func=AF.Exp, scale=-1.0))
                        DVE.wait(t1)
                        t2 = DVE.sig(DVE.e.tensor_tensor(out=mixedT[:, h, :], in0=ODacc[:, 0, :], in1=ODacc[:, 1, :],
                                                         op=ALU.mult))
                        buf_tok["od_free"] = t2
                        buf_tok["mixed_last"] = t2
                else:
                    rows = bb["rows"]
                    h = bb["h"]
                    c = bb["c"]
                    iv = psO[rows, pb, :].rearrange("p (u o q) -> p o u q", u=2, o=2)
                    tb = btmp[pb]
                    ACT.wait(t_o, t_es, buf_tok.get("btmp_free%d" % pb))
                    ta = ACT.sig(ACT.e.activation(out=tb[rows, :, :], in_=iv[:, 1, :, :], func=AF.Ln,
                                                  bias=es_t[rows, h:h + 1], scale=1.0))
                    ACT.wait(ta)
                    tb_ = ACT.sig(ACT.e.activation(out=tb[rows, :, :], in_=tb[rows, :, :], func=AF.Exp, scale=-1.0))
                    DVE.wait(tb_)
                    t_e = DVE.sig(DVE.e.tensor_tensor(
                        out=mixedT[rows, 8 + c, 128 * b0:128 * b0 + 256].rearrange("p (u q) -> p u q", u=2),
                        in0=iv[:, 0, :, :], in1=tb[rows, :, :], op=ALU.mult))
                    O_free[pb] = t_e
                    buf_tok["btmp_free%d" % pb] = t_e
                    buf_tok["mixed_last"] = t_e

            emit_qk(0)
            emit_exp(0)
            if NB > 1:
                emit_qk(1)
                emit_exp(1)
            for n in range(NB):
                if n + 2 < NB:
                    emit_qk(n + 2)
                    emit_exp(n + 2)
                t_o = emit_pv(n)
                emit_evac(n, t_o)
            barrier()

        if stage in (2, 22):
            with ExitStack() as sd_:
                tmp = sb("dbgtmp", [128, S], F32, sd_)
                dd = mk_dsem("dbg")
                for c in range(16):
                    DVE.wait(dd.tok())
                    t = DVE.sig(DVE.e.tensor_copy(out=tmp[:], in_=mixedT[:, c, :]))
                    SP.wait(t)
                    dd.add(SP.e.dma_start(out=dbg[128 * c:128 * c + 128, :], in_=tmp[:]))
                SP.wait(dd.tok())
                barrier()
            st_aT.close()
            st_mixed.close()
            return nc

        wout = aT
        with ExitStack() as s3:
            xt = [sb("xt3_%d" % i, [128, D], F32, s3) for i in range(2)]
            h1 = [sb("h1_%d" % i, [128, D], F32, s3) for i in range(2)]
            mf = sb("mf", [128, D], F32, s3)
            mhl = sb("mhl", [128, 2, D], BF16, s3)
            mhlT = sb("mhlT", [128, 2, 16, 128], BF16, s3)
            Whl = sb("Whl", [128, 2, 16, 36], BF16, s3)
            mb1 = sb("mb1", [128, D], BF16, s3)
            mb = [mb1, mb1]
            gmo = sb("gmo", [128, D], F32, s3)
            junk = mb1
            Wr = mf[:, 1024:1024 + 16 * 36].rearrange("p (c n) -> p c n", c=16)
            bias_bc = sb("bias_bc", [128, 36], F32, s3)
            psH = pst("psH", [128, 2, 2, 512], F32, s3)
            psF = pst("psF", [128, 2, 1024], BF16, s3)
            psZ = pst("psZ", [128, 512], F32, s3)
            d_w = mk_dsem("wout")
            dx = [mk_dsem("x3_%d" % i) for i in range(2)]
            dh = [mk_dsem("h1s%d" % i) for i in range(2)]
            dm1 = mk_dsem("ms")
            dm = [dm1, dm1]
            t_w = buf_tok["t_w"]
            t_c3 = d_c.add(SP.e.dma_start(out=gmo[:], in_=g_moe.to_broadcast([128, D])))
            t_c3 = d_c.add(SP.e.dma_start(out=Wr[:, :, 0:4], in_=w_grp.rearrange("(c p) n -> p c n", p=128)))
            t_c3 = d_c.add(SP.e.dma_start(out=Wr[:, :, 4:36], in_=w_exp.rearrange("(c p) n -> p c n", p=128)))
            t_c3 = d_c.add(SP.e.dma_start(out=bias_bc[:, 0:4], in_=b_grp.to_broadcast([128, 4])))
            t_c3 = d_c.add(SP.e.dma_start(out=bias_bc[:, 4:36], in_=b_exp.to_broadcast([128, 32])))

            DVE.wait(t_c3)
            t_ = DVE.sig(DVE.e.tensor_copy(out=Whl[:, 0, :, :], in_=Wr))
            DVE.wait(t_)
            Wtmp = mf[:, 0:16 * 36].rearrange("p (c n) -> p c n", c=16)
            t_ = DVE.sig(DVE.e.tensor_tensor(out=Wtmp, in0=Wr, in1=Whl[:, 0, :, :], op=ALU.subtract))
            DVE.wait(t_)
            t_whl = DVE.sig(DVE.e.tensor_copy(out=Whl[:, 1, :, :], in_=Wtmp))
            mf_free0 = t_whl
            psH_free = [None, None]
            psF_free = [None, None]
            psZ_free = None
            mhl_free = None
            xt_free = [None, None]
            h1_free = [None, None]
            mb_free = [None, None]
            mf_free = t_whl
            mfT_free = None
            t_hmm = {}

            def op_mm(j):
                b = j % 2
                bg_issue(4)
                SP.wait(xt_free[b])
                t_x = dx[b].add(SP.e.dma_start(out=xt[b][:], in_=x[128 * j:128 * j + 128, :]))
                mm = []
                for half in range(2):
                    PE.wait(psH_free[half], t_w)
                    for nn in range(2):
                        for kc in range(16):
                            ins = PE.e.matmul(psH[:, half, nn, :], lhsT=mixedT[:, kc, 128 * j:128 * j + 128],
                                              rhs=wout[:, kc, 1024 * half + 512 * nn:1024 * half + 512 * nn + 512],
                                              start=(kc == 0), stop=(kc == 15))
                    mm.append(PE.sig(ins))
                t_hmm[j] = dict(t_x=t_x, mm=mm)

            def op_add(j):
                b = j % 2
                a = t_hmm[j]
                toks = []
                for half in range(2):
                    DVE.wait(a["mm"][half], a["t_x"], h1_free[b])
                    t_h = DVE.sig(DVE.e.tensor_tensor(out=h1[b][:, 1024 * half:1024 * half + 1024],
                                                      in0=psH[:, half, :, :].rearrange("p n f -> p (n f)"),
                                                      in1=xt[b][:, 1024 * half:1024 * half + 1024], op=ALU.add))
                    psH_free[half] = t_h
                    toks.append(t_h)
                xt_free[b] = toks[1]
                a["toks"] = toks

            def rest_a(j):
                nonlocal mf_free, mfT_free, psZ_free
                b = j % 2
                toks = t_hmm[j]["toks"]
                SP.wait(toks)
                t_hs = dh[b].add(SP.e.dma_start(out=H1[128 * j:128 * j + 128, :], in_=h1[b][:]))
                ACT.wait(mb_free)
                t_rs = rms_stats(16 + j, h1[b][:], junk[:], toks)
                DVE.wait(toks, t_c3, mf_free)
                t_mf = DVE.sig(DVE.e.tensor_tensor(out=mf[:], in0=h1[b][:], in1=gmo[:], op=ALU.mult))
                ACT.wait(t_mf, t_rs, mb_free)
                t_m = ACT.sig(ACT.e.activation(out=mb[b][:], in_=mf[:], func=AF.Copy, scale=rs[:, 16 + j:17 + j]))
                SP.wait(t_m)
                mb_free[b] = dm[b].add(SP.e.dma_start(out=Mscr[128 * j:128 * j + 128, :], in_=mb[b][:]))
                DVE.wait(t_mf, mhl_free)
                t_hi = DVE.sig(DVE.e.tensor_copy(out=mhl[:, 0, :], in_=mf[:]))
                DVE.wait(t_hi)
                t_lo = DVE.sig(DVE.e.tensor_tensor(out=mhl[:, 1, :], in0=mf[:], in1=mhl[:, 0, :], op=ALU.subtract))
                t_hmm[j].update(t_hs=t_hs, t_rs=t_rs, t_mf=t_mf, t_m=t_m, t_lo=t_lo)

            def rest_b(j):
                nonlocal mf_free, mfT_free, psZ_free, mhl_free
                b = j % 2
                a = t_hmm[j]
                t_hs, t_rs, t_mf, t_m, t_lo = a["t_hs"], a["t_rs"], a["t_mf"], a["t_m"], a["t_lo"]
                evs = []
                for q4 in range(4):
                    fb = q4 % 2
                    hl, ch = q4 // 2, q4 % 2
                    PE.wait(t_lo, psF_free[fb], t_idb)
                    for i8 in range(8):
                        c = 8 * ch + i8
                        ins = PE.e.transpose(out=psF[:, fb, 128 * i8:128 * i8 + 128], in_=mhl[:, hl, 128 * c:128 * c + 128],
                                             identity=ident_b[:])
                    t_tr = PE.sig(ins)
                    E = ACT if fb == 0 else DVE
                    E.wait(t_tr, mfT_free)
                    if fb == 0:
                        t_e = ACT.sig(ACT.e.activation(out=mhlT[:, hl, 8 * ch:8 * ch + 8, :],
                                                       in_=psF[:, fb, :].rearrange("p (c t) -> p c t", c=8),
                                                       func=AF.Copy))
                    else:
                        t_e = DVE.sig(DVE.e.tensor_copy(out=mhlT[:, hl, 8 * ch:8 * ch + 8, :],
                                                        in_=psF[:, fb, :].rearrange("p (c t) -> p c t", c=8)))
                    psF_free[fb] = t_e
                    evs.append(t_e)
                mhl_free = t_tr
                mf_free = [t_lo, t_m]
                h1_free[b] = [t_hs, t_mf, t_rs]
                PE.wait(evs, psZ_free, t_whl)
                n_mm = 0
                for (xa, wa_) in ((0, 0), (1, 0), (0, 1)):
                    for c in range(16):
                        ins = PE.e.matmul(psZ[:, 0:36], lhsT=mhlT[:, xa, c, :], rhs=Whl[:, wa_, c, :],
                                          start=(n_mm == 0), stop=(n_mm == 47))
                        n_mm += 1
                t_z = PE.sig(ins)
                mfT_free = t_z
                DVE.wait(t_z, t_rs)
                psZ_free = DVE.sig(DVE.e.scalar_tensor_tensor(out=lg_all[:, j, :], in0=psZ[:, 0:36],
                                                              scalar=rs[:, 16 + j:17 + j], in1=bias_bc[:],
                                                              op0=ALU.mult, op1=ALU.add))

            op_mm(0)
            op_add(0)
            for j in range(NT):
                if j + 1 < NT:
                    op_mm(j + 1)
                rest_a(j)
                if j + 1 < NT:
                    op_add(j + 1)
                rest_b(j)
            barrier()
        st_aT.close()
        st_mixed.close()

        if stage == 3:
            with ExitStack() as sd_:
                tmp = sb("dbgtmp", [128, S], F32, sd_)
                dd = mk_dsem("dbg")
                for c in range(16):
                    SP.wait(dd.tok())
                    dd.add(SP.e.dma_start(out=tmp[:], in_=H1[128 * c:128 * c + 128, :]))
                    SP.wait(dd.tok())
                    dd.add(SP.e.dma_start(out=dbg[128 * c:128 * c + 128, :], in_=tmp[:]))
                SP.wait(dd.tok())
                barrier()
            return nc

        bg_issue(1000)
        st_wpg = ExitStack()
        wpg = sb("wpg", [128, 16, D], BF16, st_wpg)
        d_wpg = mk_dsem("wpg")
        with ExitStack() as s5:
            NMT = 4
            mt = [sb("mt%d" % i, [128, D], BF16, s5) for i in range(NMT)]
            wgu = [sb("wgu%d" % i, [128, 16, 512], BF16, s5) for i in range(2)]
            wdn = [sb("wdn%d" % i, [128, 2, D], BF16, s5) for i in range(2)]
            xs = [[sb("xs%d_%d" % (i, s_), [128, D], BF16, s5) for s_ in range(2)] for i in range(2)]
            XT = [sb("XT%d" % i, [128, 16, 256], BF16, s5) for i in range(2)]
            sg = sb("sg", [128, 2, 256], F32, s5)
            hidT = sb("hidT", [128, 2, 256], BF16, s5)
            yb = [sb("yb%d" % i, [128, D], BF16, s5) for i in range(2)]
            dmt = [mk_dsem("mt%d" % i) for i in range(NMT)]
            dsc = [mk_dsem("sc%d" % i) for i in range(NMT)]
            dwe = [mk_dsem("we%d" % i) for i in range(2)]
            dxs = [mk_dsem("xs%d" % i) for i in range(2)]
            dys = [mk_dsem("ys%d" % i) for i in range(2)]
            mt_tok = {}
            for j in range(NMT):
                mt_tok[j] = dmt[j].add(SP.e.dma_start(out=mt[j][:], in_=Mscr[128 * j:128 * j + 128, :]))

            with ExitStack() as s4:
                def t3(name, shape, dt=F32):
                    return sb(name, shape, dt, s4)
                gmax = t3("gmax", [128, NT]); goh = t3("goh", [128, NT, 4]); tmp4 = t3("tmp4", [128, NT, 4])
                sume = t3("sume", [128, NT]); grpw = t3("grpw", [128, NT]); pen4 = t3("pen4", [128, NT, 4])
                Lm = t3("Lm", [128, NT, 32]); Lm2 = t3("Lm2", [128, NT, 32])
                m1 = t3("m1", [128, NT]); m2 = t3("m2", [128, NT])
                oh = [t3("oh%d" % k, [128, NT, 32]) for k in range(2)]
                Mk = t3("Mk", [128, NT, 32], BF16)
                pos = t3("pos", [128, NT, 32]); tmp32 = t3("tmp32", [128, NT, 32])
                iota3 = t3("iota3", [128, NT, 32])
                pk = t3("pk", [128, NT]); ek = t3("ek", [128, NT]); dk = t3("dk", [128, NT]); ok = t3("ok", [128, NT])
                d21 = t3("d21", [128, NT]); e21 = t3("e21", [128, NT]); wa = t3("wa", [128, NT]); wb_ = t3("wb_", [128, NT])
                psR = pst("psR", [128, 512], F32, s4)
                t_io = POOL.sig(POOL.e.iota(iota3[:], pattern=[[0, NT], [1, 32]], base=0, channel_multiplier=0,
                                            allow_small_or_imprecise_dtypes=True))
                last = [None]

                def dv(inst_fn, extra=None):
                    DVE.wait(last[0], extra)
                    last[0] = DVE.sig(inst_fn())
                    return last[0]

                Lg = lg_all[:, :, 0:4]
                Le4 = lg_all[:, :, 4:36].rearrange("p j (g e) -> p j g e", g=4)
                V = DVE.e
                dv(lambda: V.tensor_reduce(out=gmax[:], in_=Lg, axis=AX.X, op=ALU.max))
                dv(lambda: V.tensor_tensor(out=goh[:], in0=Lg, in1=gmax[:].to_broadcast([128, NT, 4]), op=ALU.is_equal))
                t = dv(lambda: V.tensor_tensor(out=tmp4[:], in0=Lg, in1=gmax[:].to_broadcast([128, NT, 4]), op=ALU.subtract))
                ACT.wait(t)
                t = ACT.sig(ACT.e.activation(out=tmp4[:], in_=tmp4[:], func=AF.Exp))
                dv(lambda: V.tensor_reduce(out=sume[:], in_=tmp4[:], axis=AX.X, op=ALU.add), t)
                dv(lambda: V.reciprocal(out=grpw[:], in_=sume[:]))
                dv(lambda: V.tensor_scalar(out=pen4[:], in0=goh[:], scalar1=BIGV, scalar2=-BIGV, op0=ALU.mult, op1=ALU.add))
                dv(lambda: V.tensor_tensor(out=Lm[:].rearrange("p j (g e) -> p j g e", g=4), in0=Le4,
                                           in1=pen4[:].to_broadcast([128, NT, 4, 8]), op=ALU.add))
                dv(lambda: V.tensor_reduce(out=m1[:], in_=Lm[:], axis=AX.X, op=ALU.max))
                dv(lambda: V.tensor_tensor(out=oh[0][:], in0=Lm[:], in1=m1[:].to_broadcast([128, NT, 32]), op=ALU.is_equal))
                dv(lambda: V.scalar_tensor_tensor(out=Lm2[:], in0=oh[0][:], scalar=-BIGV, in1=Lm[:], op0=ALU.mult, op1=ALU.add))
                dv(lambda: V.tensor_reduce(out=m2[:], in_=Lm2[:], axis=AX.X, op=ALU.max))
                dv(lambda: V.tensor_tensor(out=oh[1][:], in0=Lm2[:], in1=m2[:].to_broadcast([128, NT, 32]), op=ALU.is_equal))
                t_mk = dv(lambda: V.tensor_tensor(out=Mk[:], in0=oh[0][:], in1=oh[1][:], op=ALU.add))
                t = dv(lambda: V.tensor_tensor(out=d21[:], in0=m2[:], in1=m1[:], op=ALU.subtract))
                ACT.wait(t)
                t = ACT.sig(ACT.e.activation(out=e21[:], in_=d21[:], func=AF.Exp))
                dv(lambda: V.tensor_scalar(out=wa[:], in0=e21[:], scalar1=1.0, scalar2=None, op0=ALU.add), t)
                dv(lambda: V.reciprocal(out=wa[:], in_=wa[:]))
                dv(lambda: V.tensor_tensor(out=wb_[:], in0=wa[:], in1=e21[:], op=ALU.mult))
                dv(lambda: V.tensor_tensor(out=wa[:], in0=wa[:], in1=grpw[:], op=ALU.mult))
                dv(lambda: V.tensor_tensor(out=wb_[:], in0=wb_[:], in1=grpw[:], op=ALU.mult))
                PE.wait(t_mk, t_pc)
                for j in range(NT):
                    PE.e.matmul(psR[:, 32 * j:32 * j + 32], lhsT=Utri[:], rhs=Mk[:, j, :], start=(j == 0), stop=False)
                for j in range(NT - 1):
                    for j2 in range(j + 1, NT):
                        ins = PE.e.matmul(psR[:, 32 * j2:32 * j2 + 32], lhsT=ones_b[:], rhs=Mk[:, j, :], start=False,
                                          stop=(j == NT - 2))
                t_pos = PE.sig(ins)
                dv(lambda: V.tensor_copy(out=pos[:].rearrange("p j e -> p (j e)"), in_=psR[:]), t_pos)
                for k in range(2):
                    wk = wa if k == 0 else wb_
                    dv(lambda: V.tensor_tensor(out=tmp32[:], in0=oh[k][:], in1=pos[:], op=ALU.mult))
                    dv(lambda: V.tensor_reduce(out=pk[:], in_=tmp32[:], axis=AX.X, op=ALU.add))
                    dv(lambda: V.tensor_tensor(out=tmp32[:], in0=oh[k][:], in1=iota3[:], op=ALU.mult), t_io)
                    dv(lambda: V.tensor_reduce(out=ek[:], in_=tmp32[:], axis=AX.X, op=ALU.add))
                    dv(lambda: V.scalar_tensor_tensor(out=dk[:], in0=ek[:], scalar=float(CAP), in1=pk[:], op0=ALU.mult, op1=ALU.add))
                    dv(lambda: V.tensor_scalar(out=ok[:], in0=pk[:], scalar1=float(CAP), scalar2=1.0e6, op0=ALU.is_ge, op1=ALU.mult))
                    dv(lambda: V.tensor_tensor(out=dk[:], in0=dk[:], in1=ok[:], op=ALU.add))
                    dv(lambda: V.tensor_copy(out=dst_i[:, k, :], in_=dk[:]))
                    dv(lambda: V.tensor_scalar(out=ok[:], in0=pk[:], scalar1=float(CAP), scalar2=None, op0=ALU.is_lt))
                    dv(lambda: V.tensor_tensor(out=wts[:, k, :], in0=wk[:], in1=ok[:], op=ALU.mult))
                t_route = last[0]


            s5p = ExitStack()
            psT = pst("psT5", [128, 2, 1024], BF16, s5p)
            psG = pst("psG", [128, 2, 2, 256], F32, s5p)
            psY = pst("psY", [128, 2, 2, 512], F32, s5p)

            ACT.wait(d_pre_pg.tok())
            for kc4 in range(4):
                t_wpg = d_wpg.add(ACT.e.dma_start(out=wpg[:, 4 * kc4:4 * kc4 + 4, :], in_=WPG[:, 4 * kc4:4 * kc4 + 4, :]))
            sc_tok = [None] * NMT
            for j in range(NT):
                b = j % NMT
                if j >= NMT:
                    SP.wait(sc_tok[b])
                    mt_tok[j] = dmt[b].add(SP.e.dma_start(out=mt[b][:], in_=Mscr[128 * j:128 * j + 128, :]))
                POOL.wait(mt_tok[j], t_route)
                for k in range(2):
                    sc_tok[b] = dsc[b].add(POOL.e.indirect_dma_start(
                        out=Xg, out_offset=bass.IndirectOffsetOnAxis(ap=dst_i[:, k, j:j + 1], axis=0),
                        in_=mt[b][:], in_offset=None, bounds_check=bc_reg, oob_is_err=False))
            t_scatter = list(sc_tok)

            def load_w(e):
                b = e % 2
                ACT.wait(w_free[b], d_pre_ex[e // 4].tok())
                dwe[b].add(ACT.e.dma_start(out=wgu[b][:].rearrange("p c f -> p (c f)"), in_=WGU[e]))
                return dwe[b].add(ACT.e.dma_start(out=wdn[b][:].rearrange("p c d -> p (c d)"), in_=WDN[e]))

            def load_x(e):
                b = e % 2
                SP.wait(t_scatter, xs_free[b])
                for s_ in range(2):
                    t = dxs[b].add(SP.e.dma_start(out=xs[b][s_][:], in_=Xg[e * CAP + 128 * s_:e * CAP + 128 * s_ + 128, :]))
                return t

            w_free = [None, None]
            xs_free = [None, None]
            psT_free = [None, None]
            psG_free = [None, None]
            psY_free = [None, None]
            yb_free = [None, None]
            XT_free = [None, None]
            hid_free = None
            sg_free = None
            xt_evs = {}
            t_w = {0: load_w(0)}
            t_x = {0: load_x(0)}
            if NE > 1:
                t_x[1] = load_x(1)

            def tr_round(e, r):
                b = e % 2
                s_, ch = r // 2, r % 2
                tb = r % 2
                PE.wait(t_x[e], psT_free[tb], t_idb)
                for i8 in range(8):
                    c = 8 * ch + i8
                    ins = PE.e.transpose(out=psT[:, tb, 128 * i8:128 * i8 + 128],
                                         in_=xs[b][s_][:, 128 * c:128 * c + 128], identity=ident_b[:])
                t_tr = PE.sig(ins)
                E = ACT if tb == 0 else DVE
                E.wait(t_tr, XT_free[b])
                if tb == 0:
                    t_e = ACT.sig(ACT.e.activation(out=XT[b][:, 8 * ch:8 * ch + 8, 128 * s_:128 * s_ + 128],
                                                   in_=psT[:, tb, :].rearrange("p (c t) -> p c t", c=8), func=AF.Copy))
                else:
                    t_e = DVE.sig(DVE.e.tensor_copy(out=XT[b][:, 8 * ch:8 * ch + 8, 128 * s_:128 * s_ + 128],
                                                    in_=psT[:, tb, :].rearrange("p (c t) -> p c t", c=8)))
                psT_free[tb] = t_e
                xt_evs.setdefault(e, []).append(t_e)
                if r == 3:
                    xs_free[b] = t_tr

            for r in range(4):
                tr_round(0, r)
            for e in range(NE):
                b = e % 2
                if e + 1 < NE:
                    t_w[e + 1] = load_w(e + 1)
                if e + 2 < NE:
                    t_x[e + 2] = load_x(e + 2)
                hts = []
                for fc in range(2):
                    PE.wait(xt_evs[e], t_w[e], psG_free[fc])
                    for gu in range(2):
                        for kc in range(16):
                            ins = PE.e.matmul(psG[:, fc, gu, :], lhsT=wgu[b][:, kc, 256 * gu + 128 * fc:256 * gu + 128 * fc + 128],
                                              rhs=XT[b][:, kc, :], start=(kc == 0), stop=(kc == 15))
                        if gu == 1:
                            t_g = PE.sig(ins)
                        if e + 1 < NE:
                            tr_round(e + 1, 2 * fc + gu)
                    ACT.wait(t_g, sg_free)
                    t_s = ACT.sig(ACT.e.activation(out=sg[:, fc, :], in_=psG[:, fc, 0, :], func=AF.Silu))
                    DVE.wait(t_s, hid_free)
                    t_h = DVE.sig(DVE.e.tensor_tensor(out=hidT[:, fc, :], in0=sg[:, fc, :], in1=psG[:, fc, 1, :], op=ALU.mult))
                    psG_free[fc] = t_h
                    hts.append(t_h)
                XT_free[b] = t_g
                sg_free = hts[1]
                ev_s = []
                for u in range(4):
                    s_, half = u // 2, u % 2
                    yi = u % 2
                    PE.wait(hts, psY_free[yi])
                    for nn in range(2):
                        for fc in range(2):
                            ins = PE.e.matmul(psY[:, yi, nn, :], lhsT=hidT[:, fc, 128 * s_:128 * s_ + 128],
                                              rhs=wdn[b][:, fc, 1024 * half + 512 * nn:1024 * half + 512 * nn + 512],
                                              start=(fc == 0), stop=(fc == 1))
                    t_y = PE.sig(ins)
                    if yi == 0:
                        ACT.wait(t_y, yb_free[s_])
                        t_c = ACT.sig(ACT.e.activation(out=yb[s_][:, 1024 * half:1024 * half + 1024],
                                                       in_=psY[:, yi, :, :].rearrange("p n f -> p (n f)"), func=AF.Copy))
                    else:
                        DVE.wait(t_y, yb_free[s_])
                        t_c = DVE.sig(DVE.e.tensor_copy(out=yb[s_][:, 1024 * half:1024 * half + 1024],
                                                        in_=psY[:, yi, :, :].rearrange("p n f -> p (n f)")))
                    psY_free[yi] = t_c
                    ev_s.append(t_c)
                    if half == 1:
                        SP.wait(ev_s)
                        ev_s = []
                        yb_free[s_] = dys[s_].add(SP.e.dma_start(out=Y[e * CAP + 128 * s_:e * CAP + 128 * s_ + 128, :], in_=yb[s_][:]))
                hid_free = t_y
                w_free[b] = t_y
            barrier()
            s5p.close()

        with ExitStack() as s6:
            h2 = [sb("h2_%d" % i, [128, D], F32, s6) for i in range(3)]
            y1 = [sb("y1_%d" % i, [128, D], BF16, s6) for i in range(2)]
            y2 = [sb("y2_%d" % i, [128, D], BF16, s6) for i in range(2)]
            nb = [sb("nb%d" % i, [128, D], BF16, s6) for i in range(2)]
            nT = [sb("nT%d" % i, [128, 16, 128], BF16, s6) for i in range(2)]
            ptl = [sb("ptl%d" % i, [128, 256], F32, s6) for i in range(2)]
            pbf = [sb("pbf%d" % i, [128, 256], BF16, s6) for i in range(2)]
            pT = [sb("pT%d" % i, [128, 2, 128], BF16, s6) for i in range(2)]
            wple = sb("wple", [128, 2, D], BF16, s6)
            gpl = sb("gpl", [128, D], F32, s6)
            gfn = sb("gfn", [128, D], F32, s6)
            sgm = [sb("sgm%d" % i, [128, 512], F32, s6) for i in range(2)]
            ot = [sb("ot%d" % i, [128, D], F32, s6) for i in range(2)]
            junk = sb("junk6", [128, D], BF16, s6)
            psT = pst("psT6", [128, 2, 1024], BF16, s6)
            psGt = pst("psGt", [128, 2, 512], F32, s6)
            psPe = pst("psPe", [128, 2, 512], F32, s6)
            d_c6 = mk_dsem("c6")
            dh2 = [mk_dsem("h2l%d" % i) for i in range(3)]
            dy = [mk_dsem("yg%d" % i) for i in range(2)]
            dp = [mk_dsem("pl%d" % i) for i in range(2)]
            dout = [mk_dsem("out%d" % i) for i in range(2)]
            t_c6 = d_c6.add(POOL.e.dma_start(out=wple[:], in_=w_ple.rearrange("(c p) d -> p c d", p=128)))
            t_c6 = d_c6.add(SP.e.dma_start(out=gpl[:], in_=g_ple.to_broadcast([128, D])))
            t_c6 = d_c6.add(SP.e.dma_start(out=gfn[:], in_=g_final.to_broadcast([128, D])))
            h2_free = [None, None, None]
            y_free = [None, None]
            nb_free = [None, None]
            nT_free = [None, None]
            p_free = [None, None]
            pbf_free = [None, None]
            pT_free = [None, None]
            psT_free = [None, None]
            psQ_free = [None, None]
            sgm_free = [None, None]
            ot_free = [None, None]
            stA = {}

            def A_pre(j):
                b = j % 2
                SP.wait(h2_free[j % 3])
                t_h = dh2[j % 3].add(SP.e.dma_start(out=h2[j % 3][:], in_=H1[128 * j:128 * j + 128, :]))
                SP.wait(p_free[b])
                t_p = dp[b].add(SP.e.dma_start(out=ptl[b][:], in_=p_in[128 * j:128 * j + 128, :]))
                POOL.wait(y_free[b])
                dy[b].add(POOL.e.indirect_dma_start(out=y1[b][:], out_offset=None, in_=Y,
                                                    in_offset=bass.IndirectOffsetOnAxis(ap=dst_i[:, 0, j:j + 1], axis=0),
                                                    bounds_check=bc_reg, oob_is_err=False))
                t_y = dy[b].add(POOL.e.indirect_dma_start(out=y2[b][:], out_offset=None, in_=Y,
                                                          in_offset=bass.IndirectOffsetOnAxis(ap=dst_i[:, 1, j:j + 1], axis=0),
                                                          bounds_check=bc_reg, oob_is_err=False))
                DVE.wait(t_h, t_y)
                t1 = DVE.sig(DVE.e.scalar_tensor_tensor(out=h2[j % 3][:], in0=y1[b][:], scalar=wts[:, 0, j:j + 1], in1=h2[j % 3][:],
                                                        op0=ALU.mult, op1=ALU.add))
                DVE.wait(t1)
                t2 = DVE.sig(DVE.e.scalar_tensor_tensor(out=h2[j % 3][:], in0=y2[b][:], scalar=wts[:, 1, j:j + 1], in1=h2[j % 3][:],
                                                        op0=ALU.mult, op1=ALU.add))
                y_free[b] = t2
                t_rs = rms_stats(32 + j, h2[j % 3][:], junk[:], t2)
                DVE.wait(t_rs, t_c6, nb_free[b])
                t_n = DVE.sig(DVE.e.scalar_tensor_tensor(out=nb[b][:], in0=h2[j % 3][:], scalar=rs[:, 32 + j:33 + j], in1=gpl[:],
                                                         op0=ALU.mult, op1=ALU.mult))
                ACT.wait(t_p, pbf_free[b])
                t_pb = ACT.sig(ACT.e.activation(out=pbf[b][:], in_=ptl[b][:], func=AF.Copy))
                p_free[b] = t_pb
                stA[j] = dict(t_n=t_n, t_pb=t_pb)

            def A_tr(j):
                b = j % 2
                a = stA[j]
                evs = []
                for ch in range(2):
                    PE.wait(a["t_n"], psT_free[ch])
                    for i8 in range(8):
                        c = 8 * ch + i8
                        ins = PE.e.transpose(out=psT[:, ch, 128 * i8:128 * i8 + 128], in_=nb[b][:, 128 * c:128 * c + 128],
                                             identity=ident_b[:])
                    if ch == 0:
                        t_tr = PE.sig(ins)
                        ACT.wait(t_tr, nT_free[b])
                        t_e = ACT.sig(ACT.e.activation(out=nT[b][:, 0:8, :],
                                                       in_=psT[:, 0, :].rearrange("p (c t) -> p c t", c=8), func=AF.Copy))
                        psT_free[0] = t_e
                        evs.append(t_e)
                    else:
                        t_tr = PE.sig(ins)
                        ACT.wait(t_tr, nT_free[b])
                        t_e = ACT.sig(ACT.e.activation(out=nT[b][:, 8:16, :],
                                                       in_=psT[:, 1, :].rearrange("p (c t) -> p c t", c=8), func=AF.Copy))
                        psT_free[1] = t_e
                        evs.append(t_e)
                nb_free[b] = t_tr
                PE.wait(a["t_pb"], psT_free[0])
                for c in range(2):
                    ins = PE.e.transpose(out=psT[:, 0, 128 * c:128 * c + 128], in_=pbf[b][:, 128 * c:128 * c + 128],
                                         identity=ident_b[:])
                t_tr = PE.sig(ins)
                pbf_free[b] = t_tr
                ACT.wait(t_tr, pT_free[b])
                t_e = ACT.sig(ACT.e.activation(out=pT[b][:], in_=psT[:, 0, 0:256].rearrange("p (c t) -> p c t", c=2),
                                               func=AF.Copy))
                psT_free[0] = t_e
                evs.append(t_e)
                a["evs"] = evs

            def B_mm_q(j, q):
                b = j % 2
                a = stA[j]
                qb = q % 2
                PE.wait(a["evs"], t_wpg, t_c6, psQ_free[qb])
                for kc in range(16):
                    PE.e.matmul(psGt[:, qb, :], lhsT=nT[b][:, kc, :], rhs=wpg[:, kc, 512 * q:512 * q + 512],
                                start=(kc == 0), stop=(kc == 15))
                for kc in range(2):
                    ins = PE.e.matmul(psPe[:, qb, :], lhsT=pT[b][:, kc, :], rhs=wple[:, kc, 512 * q:512 * q + 512],
                                      start=(kc == 0), stop=(kc == 1))
                t_mm = PE.sig(ins)
                ACT.wait(t_mm, sgm_free[qb])
                t_s = ACT.sig(ACT.e.activation(out=sgm[qb][:], in_=psGt[:, qb, :], func=AF.Sigmoid))
                DVE.wait(t_s)
                t_a = DVE.sig(DVE.e.tensor_tensor(out=sgm[qb][:], in0=sgm[qb][:], in1=psPe[:, qb, :], op=ALU.mult))
                psQ_free[qb] = t_a
                DVE.wait(t_a)
                tq = DVE.sig(DVE.e.tensor_tensor(out=h2[j % 3][:, 512 * q:512 * q + 512],
                                                 in0=h2[j % 3][:, 512 * q:512 * q + 512], in1=sgm[qb][:], op=ALU.add))
                sgm_free[qb] = tq
                a["tq"] = tq
                if q == 3:
                    nT_free[b] = t_mm
                    pT_free[b] = t_mm

            def B_fin(j):
                b = j % 2
                a = stA[j]
                t_rs = rms_stats(48 + j, h2[j % 3][:], junk[:], a["tq"])
                DVE.wait(t_rs, ot_free[b])
                t_o = DVE.sig(DVE.e.scalar_tensor_tensor(out=ot[b][:], in0=h2[j % 3][:], scalar=rs[:, 48 + j:49 + j], in1=gfn[:],
                                                         op0=ALU.mult, op1=ALU.mult))
                h2_free[j % 3] = t_o
                ACT.wait(t_o)
                ot_free[b] = dout[b].add(ACT.e.dma_start(out=out[128 * j:128 * j + 128, :], in_=ot[b][:]))

            A_pre(0)
            A_tr(0)
            for j in range(NT):
                B_mm_q(j, 0)
                B_mm_q(j, 1)
                if j + 1 < NT:
                    A_pre(j + 1)
                B_mm_q(j, 2)
                B_mm_q(j, 3)
                if j + 1 < NT:
                    A_tr(j + 1)
                B_fin(j)
            barrier()
        st_wpg.close()
    return nc


_IN_NAMES = ["x", "p", "w_in", "w_out", "sinks", "g_mix", "g_moe", "g_ple", "g_final", "w_grp", "b_grp",
             "w_exp", "b_exp", "w_gate", "w_up", "w_down", "w_ple", "w_ple_gate"]


def make_in_maps(inputs, n_cores=8):
    f = lambda a: np.ascontiguousarray(np.asarray(a, dtype=np.float32))
    shared = {
        "w_in": f(inputs["w_in"][0]), "w_out": f(inputs["w_out"][0]),
        "sinks": f(inputs["sinks"]).reshape(1, 16),
        "g_mix": f(inputs["g_mix"]).reshape(1, D), "g_moe": f(inputs["g_moe"]).reshape(1, D),
        "g_ple": f(inputs["g_ple"]).reshape(1, D), "g_final": f(inputs["g_final"]).reshape(1, D),
        "w_grp": f(inputs["w_grp"][0]), "b_grp": f(inputs["b_grp"]).reshape(1, 4),
        "w_exp": f(inputs["w_exp"][0]).reshape(D, 32), "b_exp": f(inputs["b_exp"]).reshape(1, 32),
        "w_gate": f(inputs["w_gate"][0]), "w_up": f(inputs["w_up"][0]), "w_down": f(inputs["w_down"][0]),
        "w_ple": f(inputs["w_ple"][0]), "w_ple_gate": f(inputs["w_ple_gate"][0]),
    }
    xs = f(inputs["x"])
    ps = f(inputs["p"])
    maps = []
    for c in range(n_cores):
        m = dict(shared)
        m["x"] = xs[c]
        m["p"] = ps[0, c]
        maps.append(m)
    return maps


def kernel(**inputs):
    nc = build()
    in_maps = make_in_maps(inputs)
    res = run_bass_kernel_spmd(nc, in_maps, core_ids=list(range(8)))
    return np.stack([r["out"] for r in res.results], axis=0).astype(np.float32)
```
